# Optimizing a Trainium2 kernel written in Bass

```python
import math
import jax, jax.numpy as jnp
from jax import lax
import numpy as np

D_MODEL = 1024
BATCH = 32
SEQ = 2048
DEPTH = 2

HEAD_DIM = 64
A_HEADS = 8
B_HEADS = 8
A_WIDTH = A_HEADS * HEAD_DIM
B_WIDTH = B_HEADS * HEAD_DIM
DECAY_LORA = 64
ICLR_LORA = 64
GATE_LORA = 160
A_COLS = 3 * A_WIDTH + DECAY_LORA + ICLR_LORA + GATE_LORA
B_COLS = 3 * B_WIDTH
EVEN_COLS = A_COLS + B_COLS
SB_BLOCK = 128
GN_EPS = 64e-5
LRU_WIDTH = 1280
LRU_BLOCKS = 10
LRU_BLOCK_DIM = LRU_WIDTH // LRU_BLOCKS
CONV_WIDTH = 4
LRU_C = 8.0
N_GROUPS = 4
EXPERTS_PER_GROUP = 4
N_EXPERTS = N_GROUPS * EXPERTS_PER_GROUP
EXPERT_TOPK = 2
D_EXPERT = 512
RMS_EPS = 1e-6
N_EVEN = (DEPTH + 1) // 2
N_ODD = DEPTH // 2

kernel_name = 'hybrid_rwkv7_stickbreak_rglru_hmoe'


def rmsnorm(x, g):
    xf = x.astype(jnp.float32)
    y = xf * lax.rsqrt(jnp.mean(xf * xf, axis=-1, keepdims=True) + RMS_EPS)
    return (y * g.astype(jnp.float32)).astype(x.dtype)


def token_shift(p, mu):
    prev = jnp.pad(p, ((0, 0), (1, 0), (0, 0)))[:, :-1]
    return p + mu * (prev - p)


def rwkv7_time_mix(p, w0, decay_up, a0, iclr_up, gate_up, k_k, k_a, r_k, gn_w, gn_b):
    f32 = jnp.float32
    bsz, t_len, _ = p.shape
    o1, o2, o3 = A_WIDTH, 2 * A_WIDTH, 3 * A_WIDTH
    r, k, v = p[..., :o1], p[..., o1:o2], p[..., o2:o3]
    xw = p[..., o3:o3 + DECAY_LORA]
    xa = p[..., o3 + DECAY_LORA:o3 + DECAY_LORA + ICLR_LORA]
    xg = p[..., o3 + DECAY_LORA + ICLR_LORA:]
    w_log = -jax.nn.softplus(-(w0 + jnp.tanh(xw) @ decay_up).astype(f32)) - 0.5
    decay = jnp.exp(-jnp.exp(w_log))
    a = jax.nn.sigmoid((a0 + xa @ iclr_up).astype(f32))
    g = (jax.nn.sigmoid(xg) @ gate_up).astype(f32)

    def heads(t):
        return t.astype(f32).reshape(bsz, t_len, A_HEADS, HEAD_DIM)

    kk = heads(k * k_k)
    kk = kk / jnp.maximum(jnp.sqrt(jnp.sum(kk * kk, axis=-1, keepdims=True)), 1e-12)
    k_mod = k.astype(f32) * (1.0 + (a - 1.0) * k_a.astype(f32))
    r_h, k_h, v_h, w_h, a_h = heads(r), heads(k_mod), heads(v), heads(decay), heads(a)
    a_vec = -kk
    b_vec = kk * a_h

    def step(S, inp):
        r_t, w_t, k_t, v_t, a_t, b_t = inp
        sa = jnp.einsum('bhvk,bhk->bhv', S, a_t)
        S = S * w_t[:, :, None, :] + sa[..., None] * b_t[:, :, None, :] + v_t[..., None] * k_t[:, :, None, :]
        return S, jnp.einsum('bhvk,bhk->bhv', S, r_t)

    s0 = jnp.zeros((bsz, A_HEADS, HEAD_DIM, HEAD_DIM), f32)
    xs = tuple(jnp.swapaxes(t, 0, 1) for t in (r_h, w_h, k_h, v_h, a_vec, b_vec))
    _, y = lax.scan(step, s0, xs)
    y = jnp.swapaxes(y, 0, 1)
    mean = jnp.mean(y, axis=-1, keepdims=True)
    var = jnp.mean(jnp.square(y - mean), axis=-1, keepdims=True)
    y = ((y - mean) * lax.rsqrt(var + GN_EPS)).reshape(bsz, t_len, A_WIDTH)
    y = y * gn_w.astype(f32) + gn_b.astype(f32)
    bonus = jnp.sum(r_h * k_h * r_k.astype(f32), axis=-1, keepdims=True) * v_h
    y = y + bonus.reshape(bsz, t_len, A_WIDTH)
    return (y * g).astype(p.dtype)


def stick_breaking_attention(p, q_norm_g, k_norm_g):
    bsz, t_len, _ = p.shape

    def heads(t):
        return t.reshape(bsz, t_len, B_HEADS, HEAD_DIM)

    q = rmsnorm(heads(p[..., :B_WIDTH]), q_norm_g)
    k = rmsnorm(heads(p[..., B_WIDTH:2 * B_WIDTH]), k_norm_g)
    v = heads(p[..., 2 * B_WIDTH:])
    q, k, v = (jnp.swapaxes(t, 1, 2) for t in (q, k, v))
    scale = 1.0 / math.sqrt(HEAD_DIM)
    outs = []
    for blk in range(t_len // SB_BLOCK):
        q0 = blk * SB_BLOCK
        end = q0 + SB_BLOCK
        z = jnp.einsum('bhqd,bhkd->bhqk', q[:, :, q0:end], k[:, :, :end]).astype(jnp.float32) * scale
        t_idx = q0 + jnp.arange(SB_BLOCK)[:, None]
        s_idx = jnp.arange(end)[None, :]
        causal = s_idx < t_idx
        log_1m_beta = jnp.where(causal, jax.nn.log_sigmoid(-z), 0.0)
        after = lax.cumsum(log_1m_beta, axis=3, reverse=True) - log_1m_beta
        att = jnp.where(causal, jnp.exp(jax.nn.log_sigmoid(z) + after), 0.0)
        outs.append(jnp.einsum('bhqk,bhkd->bhqd', att.astype(v.dtype), v[:, :, :end]))
    o = jnp.concatenate(outs, axis=2)
    return jnp.swapaxes(o, 1, 2).reshape(bsz, t_len, B_WIDTH)


def causal_depthwise_conv(u, conv_w, conv_b):
    out = lax.conv_general_dilated(
        u, conv_w[:, None, :], window_strides=(1,), padding=((CONV_WIDTH - 1, 0),),
        dimension_numbers=('NWC', 'WIO', 'NWC'), feature_group_count=u.shape[-1])
    return out + conv_b


def rg_lru(u, w_rgate, b_rgate, w_igate, b_igate, lru_lambda):
    bsz, t_len, _ = u.shape
    ub = u.reshape(bsz, t_len, LRU_BLOCKS, LRU_BLOCK_DIM)
    r = jax.nn.sigmoid((jnp.einsum('btgi,gij->btgj', ub, w_rgate).reshape(bsz, t_len, LRU_WIDTH) + b_rgate).astype(jnp.float32))
    i = jax.nn.sigmoid((jnp.einsum('btgi,gij->btgj', ub, w_igate).reshape(bsz, t_len, LRU_WIDTH) + b_igate).astype(jnp.float32))
    log_a = -LRU_C * r * jax.nn.softplus(-lru_lambda.astype(jnp.float32))
    a = jnp.exp(log_a)
    b = jnp.sqrt(-jnp.expm1(2.0 * log_a)) * (i * u.astype(jnp.float32))

    def combine(c1, c2):
        a1, b1 = c1
        a2, b2 = c2
        return a1 * a2, a2 * b1 + b2

    _, h = lax.associative_scan(combine, (a, b), axis=1)
    return h.astype(u.dtype)


def hier_moe(h, w_group, b_group, w_erouter, b_erouter, exp_w_gate, exp_w_up, exp_w_down):
    f32 = jnp.float32
    bsz, t_len, d = h.shape
    hf = h.reshape(bsz * t_len, d)
    g_logits = (hf @ w_group).astype(f32) + b_group.astype(f32)
    g_prob = jax.nn.softmax(g_logits, axis=-1)
    g_top, g_idx = lax.top_k(g_prob, 1)
    e_logits = ((hf @ w_erouter).astype(f32) + b_erouter.astype(f32)).reshape(-1, N_GROUPS, EXPERTS_PER_GROUP)
    e_sel = jnp.take_along_axis(e_logits, g_idx[:, :, None], axis=1)[:, 0]
    e_prob = jax.nn.softmax(e_sel, axis=-1)
    e_top, e_idx = lax.top_k(e_prob, EXPERT_TOPK)
    gates = g_top * e_top / jnp.sum(e_top, axis=-1, keepdims=True)
    expert_id = g_idx * EXPERTS_PER_GROUP + e_idx
    dense_gate = jnp.sum(jax.nn.one_hot(expert_id, N_EXPERTS, dtype=f32) * gates[..., None], axis=1)
    dense_gate = dense_gate.astype(hf.dtype)
    y = jnp.zeros_like(hf)
    for e in range(N_EXPERTS):
        hid = jax.nn.silu(hf @ exp_w_gate[e]) * (hf @ exp_w_up[e])
        y = y + dense_gate[:, e:e + 1] * (hid @ exp_w_down[e])
    return y.reshape(bsz, t_len, d)


def setup_inputs(seed: int = 0) -> dict:
    key = jax.random.key(seed)
    ks = iter(jax.random.split(key, 48))
    f32 = jnp.float32

    def nrm(shape, scale):
        return jax.random.normal(next(ks), shape, f32) * scale

    def uni(shape, lo, hi):
        return jax.random.uniform(next(ks), shape, f32, minval=lo, maxval=hi)

    x = nrm((BATCH, SEQ, D_MODEL), 1.0)
    norm_mix = 1.0 + nrm((DEPTH, D_MODEL), 0.05)
    norm_ffn = 1.0 + nrm((DEPTH, D_MODEL), 0.05)
    w_in_even = nrm((N_EVEN, D_MODEL, EVEN_COLS), D_MODEL ** -0.5)
    mu_a = uni((N_EVEN, A_COLS), 0.0, 1.0)
    w0 = uni((N_EVEN, A_WIDTH), -6.0, -1.0)
    decay_up = nrm((N_EVEN, DECAY_LORA, A_WIDTH), 0.5 * DECAY_LORA ** -0.5)
    a0 = nrm((N_EVEN, A_WIDTH), 0.1)
    iclr_up = nrm((N_EVEN, ICLR_LORA, A_WIDTH), ICLR_LORA ** -0.5)
    gate_up = nrm((N_EVEN, GATE_LORA, A_WIDTH), GATE_LORA ** -0.5)
    k_k = 0.85 + nrm((N_EVEN, A_WIDTH), 0.05)
    k_a = 1.0 + nrm((N_EVEN, A_WIDTH), 0.05)
    r_k = nrm((N_EVEN, A_HEADS, HEAD_DIM), 0.1)
    gn_w = 1.0 + nrm((N_EVEN, A_WIDTH), 0.05)
    gn_b = nrm((N_EVEN, A_WIDTH), 0.02)
    q_norm_g = 1.0 + nrm((N_EVEN, HEAD_DIM), 0.05)
    k_norm_g = 1.0 + nrm((N_EVEN, HEAD_DIM), 0.05)
    w_out_even = nrm((N_EVEN, A_WIDTH + B_WIDTH, D_MODEL), (A_WIDTH + B_WIDTH) ** -0.5)
    w_in_odd = nrm((N_ODD, D_MODEL, 2 * LRU_WIDTH), D_MODEL ** -0.5)
    conv_w = nrm((N_ODD, CONV_WIDTH, LRU_WIDTH), CONV_WIDTH ** -0.5)
    conv_b = nrm((N_ODD, LRU_WIDTH), 0.02)
    w_rgate = nrm((N_ODD, LRU_BLOCKS, LRU_BLOCK_DIM, LRU_BLOCK_DIM), LRU_BLOCK_DIM ** -0.5)
    b_rgate = nrm((N_ODD, LRU_WIDTH), 0.02)
    w_igate = nrm((N_ODD, LRU_BLOCKS, LRU_BLOCK_DIM, LRU_BLOCK_DIM), LRU_BLOCK_DIM ** -0.5)
    b_igate = nrm((N_ODD, LRU_WIDTH), 0.02)
    a_pow_c = uni((N_ODD, LRU_WIDTH), 0.9, 0.999)
    s = a_pow_c ** (1.0 / LRU_C)
    lru_lambda = jnp.log(s) - jnp.log1p(-s)
    w_out_odd = nrm((N_ODD, LRU_WIDTH, D_MODEL), LRU_WIDTH ** -0.5)
    w_group = nrm((DEPTH, D_MODEL, N_GROUPS), D_MODEL ** -0.5)
    b_group = nrm((DEPTH, N_GROUPS), 0.01)
    w_erouter = nrm((DEPTH, D_MODEL, N_EXPERTS), D_MODEL ** -0.5)
    b_erouter = nrm((DEPTH, N_EXPERTS), 0.01)
    exp_w_gate = nrm((DEPTH, N_EXPERTS, D_MODEL, D_EXPERT), D_MODEL ** -0.5)
    exp_w_up = nrm((DEPTH, N_EXPERTS, D_MODEL, D_EXPERT), D_MODEL ** -0.5)
    exp_w_down = nrm((DEPTH, N_EXPERTS, D_EXPERT, D_MODEL), D_EXPERT ** -0.5)
    return {'x': x, 'norm_mix': norm_mix, 'norm_ffn': norm_ffn,
            'w_in_even': w_in_even, 'mu_a': mu_a, 'w0': w0, 'decay_up': decay_up,
            'a0': a0, 'iclr_up': iclr_up, 'gate_up': gate_up, 'k_k': k_k, 'k_a': k_a,
            'r_k': r_k, 'gn_w': gn_w, 'gn_b': gn_b, 'q_norm_g': q_norm_g, 'k_norm_g': k_norm_g,
            'w_out_even': w_out_even, 'w_in_odd': w_in_odd, 'conv_w': conv_w, 'conv_b': conv_b,
            'w_rgate': w_rgate, 'b_rgate': b_rgate, 'w_igate': w_igate, 'b_igate': b_igate,
            'lru_lambda': lru_lambda, 'w_out_odd': w_out_odd, 'w_group': w_group,
            'b_group': b_group, 'w_erouter': w_erouter, 'b_erouter': b_erouter,
            'exp_w_gate': exp_w_gate, 'exp_w_up': exp_w_up, 'exp_w_down': exp_w_down}


def reference(x, norm_mix, norm_ffn, w_in_even, mu_a, w0, decay_up, a0, iclr_up, gate_up,
              k_k, k_a, r_k, gn_w, gn_b, q_norm_g, k_norm_g, w_out_even, w_in_odd, conv_w,
              conv_b, w_rgate, b_rgate, w_igate, b_igate, lru_lambda, w_out_odd, w_group,
              b_group, w_erouter, b_erouter, exp_w_gate, exp_w_up, exp_w_down):
    for layer in range(DEPTH):
        h = rmsnorm(x, norm_mix[layer])
        if layer % 2 == 0:
            i = layer // 2
            proj = h @ w_in_even[i]
            pa = token_shift(proj[..., :A_COLS], mu_a[i])
            ya = rwkv7_time_mix(pa, w0[i], decay_up[i], a0[i], iclr_up[i], gate_up[i],
                                k_k[i], k_a[i], r_k[i], gn_w[i], gn_b[i])
            yb = stick_breaking_attention(proj[..., A_COLS:], q_norm_g[i], k_norm_g[i])
            x = x + jnp.concatenate([ya, yb], axis=-1) @ w_out_even[i]
        else:
            j = layer // 2
            u = h @ w_in_odd[j]
            gate_branch = jax.nn.gelu(u[..., :LRU_WIDTH])
            rec = causal_depthwise_conv(u[..., LRU_WIDTH:], conv_w[j], conv_b[j])
            rec = rg_lru(rec, w_rgate[j], b_rgate[j], w_igate[j], b_igate[j], lru_lambda[j])
            x = x + (gate_branch * rec) @ w_out_odd[j]
        x = x + hier_moe(rmsnorm(x, norm_ffn[layer]), w_group[layer], b_group[layer],
                         w_erouter[layer], b_erouter[layer], exp_w_gate[layer],
                         exp_w_up[layer], exp_w_down[layer])
    return x
```

```python
import os
import numpy as np
from contextlib import ExitStack
import concourse.bass as bass
import concourse.mybir as mybir
from concourse.bass_utils import run_bass_kernel_spmd

F32 = mybir.dt.float32
BF16 = mybir.dt.bfloat16
AF = mybir.ActivationFunctionType
ALU = mybir.AluOpType
AX = mybir.AxisListType

D = 1024
NCORES = 8
A_COLS = 1824
EVEN_COLS = 3360
LRU_W = 1280
NEXP = 16
DEXP = 512
DECAY_C = 0.6065306597126334
RMS_EPS = 1e-6
GN_EPS = 64e-5
SEM_LIMIT = 30000


class Buf:
    __slots__ = ("t", "w", "r")

    def __init__(self, t):
        self.t = t
        self.w = None
        self.r = {}


class MBuf:
    __slots__ = ("t", "ch")

    def __init__(self, t, n=2):
        self.t = t
        self.ch = [Buf(t) for _ in range(n)]


def _flat(bs):
    out = []
    for b in bs:
        if isinstance(b, MBuf):
            out.extend(b.ch)
        else:
            out.append(b)
    return out


class K:
    def __init__(self, nc, es):
        self.nc = nc
        self.es = es
        self.eng = {"pe": nc.tensor, "act": nc.scalar, "dve": nc.vector, "pool": nc.gpsimd, "sp": nc.sync}
        self.sems = {}
        self.epoch = {e: 0 for e in self.eng}
        self.cnt = {}
        self.waited = {e: {} for e in self.eng}
        for e in self.eng:
            self._new_epoch(e, first=True)
        self.ndslots = {"sp": 6, "pool": 6, "act": 2, "pre": 4}
        self.qeng = {"sp": "sp", "pool": "pool", "act": "act", "pre": "pool"}
        self.dslot = {q: 0 for q in self.ndslots}
        for q, n in self.ndslots.items():
            for i in range(n):
                key = ("d", q, i)
                self.sems[key] = es.enter_context(nc.semaphore("d_%s_%d" % (q, i)))
                self.cnt[key] = 0
        self.n_inst = 0

    def _new_epoch(self, e, first=False):
        if not first:
            self.epoch[e] += 1
        key = (e, self.epoch[e])
        self.sems[key] = self.es.enter_context(self.nc.semaphore("c_%s_%d" % (e, self.epoch[e])))
        self.cnt[key] = 0

    def _wait(self, e, deps):
        for key, val in deps.items():
            if self.waited[e].get(key, 0) < val:
                self.eng[e].wait_ge(self.sems[key], val)
                self.waited[e][key] = val

    def _deps(self, e, reads, writes):
        deps = {}

        def add(kv):
            if kv is None:
                return
            key, val = kv
            if deps.get(key, 0) < val:
                deps[key] = val

        for b in reads:
            add(b.w)
        for b in writes:
            if not (e == "pe" and b.w is not None and b.w[0][0] == "pe" and not b.r):
                add(b.w)
            for key, val in b.r.items():
                add((key, val))
        return deps

    def op(self, e, fn, reads=(), writes=()):
        reads, writes = _flat(reads), _flat(writes)
        self._wait(e, self._deps(e, reads, writes))
        inst = fn(self.eng[e])
        key = (e, self.epoch[e])
        self.cnt[key] += 1
        val = self.cnt[key]
        inst.then_inc(self.sems[key], 1)
        self.n_inst += 1
        for b in reads:
            if b.r.get(key, 0) < val:
                b.r[key] = val
        for b in writes:
            b.w = (key, val)
            b.r = {}
        if val >= SEM_LIMIT:
            self._new_epoch(e)

    def dma(self, qc, out, in_, reads=(), writes=()):
        reads, writes = _flat(reads), _flat(writes)
        q = self.qeng[qc]
        self._wait(q, self._deps(q, reads, writes))
        i = self.dslot[qc]
        self.dslot[qc] = (i + 1) % self.ndslots[qc]
        key = ("d", qc, i)
        if self.cnt[key] > 0 and self.waited[q].get(key, 0) < self.cnt[key]:
            self.eng[q].wait_ge(self.sems[key], self.cnt[key])
            self.waited[q][key] = self.cnt[key]
        self.eng[q].dma_start(out=out, in_=in_).then_inc(self.sems[key], 16)
        self.cnt[key] += 16
        val = self.cnt[key]
        self.n_inst += 1
        for b in reads:
            if b.r.get(key, 0) < val:
                b.r[key] = val
        for b in writes:
            b.w = (key, val)
            b.r = {}

    def barrier(self, engines=None, skip_pre=True):
        engines = engines or list(self.eng)
        deps = {key: val for key, val in self.cnt.items() if val > 0 and not (skip_pre and key[0] == "d" and key[1] == "pre")}
        for e in engines:
            self._wait(e, deps)

    def mm(self, out, lhsT, rhs, start, stop, reads, writes):
        self.op("pe", lambda g: g.matmul(out, lhsT, rhs, start=start, stop=stop), reads, writes)

    def tr(self, out, in_, ident, reads, writes):
        self.op("pe", lambda g: g.transpose(out, in_, ident), reads, writes)

    def act(self, out, in_, func, reads, writes, bias=None, scale=None, e="act"):
        kw = {}
        if bias is not None:
            kw["bias"] = bias
        if scale is not None:
            kw["scale"] = scale
        self.op(e, lambda g: g.activation(out=out, in_=in_, func=func, **kw), reads, writes)

    def tt(self, out, in0, in1, op, reads, writes, e="dve"):
        self.op(e, lambda g: g.tensor_tensor(out=out, in0=in0, in1=in1, op=op), reads, writes)

    def ts(self, out, in0, s1, s2, op0, op1, reads, writes, e="dve"):
        if op1 is None:
            self.op(e, lambda g: g.tensor_scalar(out, in0, s1, None, op0), reads, writes)
        else:
            self.op(e, lambda g: g.tensor_scalar(out, in0, s1, s2, op0, op1), reads, writes)

    def stt(self, out, in0, scalar, in1, op0, op1, reads, writes, e="dve"):
        self.op(e, lambda g: g.scalar_tensor_tensor(out=out, in0=in0, scalar=scalar, in1=in1, op0=op0, op1=op1), reads, writes)

    def rsqrt(self, out, in_, scale, bias, reads, writes, floor=None):
        kw = {"scale": scale}
        if bias is not None:
            kw["bias"] = bias
        self.op("act", lambda g: g.activation(out=out, in_=in_, func=AF.Sqrt, **kw), reads, writes)
        if floor is not None:
            self.op("dve", lambda g: g.tensor_scalar(out, out, floor, None, ALU.max), writes, writes)
        self.op("dve", lambda g: g.reciprocal(out, out), writes, writes)

    def copy(self, out, in_, reads, writes, e="dve"):
        if e == "act":
            self.op(e, lambda g: g.activation(out=out, in_=in_, func=AF.Copy), reads, writes)
        else:
            self.op(e, lambda g: g.tensor_copy(out, in_), reads, writes)

    def memset(self, ap, val, writes, e="dve"):
        self.op(e, lambda g: g.memset(ap, val), (), writes)


IN_CHUNKS = [(i * 128, 128) for i in range(12)] + [(1536, 128), (1664, 128), (1792, 32)] + \
            [(A_COLS + i * 128, 128) for i in range(12)]


def _pcols(inp):
    cols = {}
    arrs = []

    def add(name, v):
        v = np.asarray(v, np.float32).reshape(-1)
        col = np.zeros(128, np.float32)
        col[: v.shape[0]] = v
        cols[name] = len(arrs)
        arrs.append(col)

    mu = inp["mu_a"][0]
    for ci, (c0, m) in enumerate(IN_CHUNKS[:15]):
        add("mu%d" % ci, mu[c0:c0 + m])
    for p in range(4):
        sl = slice(p * 128, (p + 1) * 128)
        add("w0_%d" % p, inp["w0"][0][sl])
        add("a0_%d" % p, inp["a0"][0][sl])
        add("kk_%d" % p, inp["k_k"][0][sl])
        add("ka_%d" % p, inp["k_a"][0][sl])
        add("rk_%d" % p, inp["r_k"][0].reshape(-1)[sl])
        add("gnw_%d" % p, inp["gn_w"][0][sl])
        add("gnb_%d" % p, inp["gn_b"][0][sl])
    add("qg", np.tile(inp["q_norm_g"][0], 2))
    add("kg", np.tile(inp["k_norm_g"][0], 2))
    for g in range(10):
        sl = slice(g * 128, (g + 1) * 128)
        for t in range(4):
            add("cw%d_%d" % (t, g), inp["conv_w"][0][t][sl])
        add("cb_%d" % g, inp["conv_b"][0][sl])
        add("br_%d" % g, inp["b_rgate"][0][sl])
        add("bi_%d" % g, inp["b_igate"][0][sl])
        add("lam_%d" % g, inp["lru_lambda"][0][sl])
    return cols, np.stack(arrs, axis=1).copy()


def _consts():
    j = np.arange(128)
    c = {}
    c["ident"] = np.eye(128, dtype=np.float32)
    bo = np.zeros((128, 128), np.float32)
    bo[:64, :64] = 1.0
    bo[64:, 64:] = 1.0
    c["blockones"] = bo
    c["ones"] = np.ones((128, 128), np.float32)
    c["triu"] = (j[:, None] >= j[None, :]).astype(np.float32)
    t = np.arange(512)
    c["maskrel"] = np.stack([((r * 128 + j[:, None]) < t[None, :]).astype(np.float32) for r in range(4)], axis=1)
    jj = np.arange(64)
    m = np.zeros((64, 4, 64), np.float32)
    m[:, 0] = (jj[:, None] < jj[None, :])
    m[:, 1] = (jj[:, None] <= jj[None, :])
    m[:, 2] = (jj[None, :] < jj[:, None])
    m[:, 3] = np.eye(64)
    c["m64"] = m
    return c


def build(NSEQ, T, pcol_idx, npc, phases=("l0in", "rwkv", "attn", "l0out", "l1in", "lru", "l1out"), debug=False):
    NTOK = NSEQ * T
    NT512 = NTOK // 512
    nc = bass.Bass("TRN2", target_bir_lowering=False)
    dbgkind = "ExternalOutput" if debug else "Internal"

    def din(name, shape, dt=F32):
        return nc.dram_tensor(name, list(shape), dt, kind="ExternalInput").ap()

    x_in = din("x", [NTOK, D])
    out_d = nc.dram_tensor("out", [NTOK, D], F32, kind="ExternalOutput").ap()
    gbc_d = din("gbc", [4, 128, D])
    pc_d = din("pc", [128, npc])
    w_in_even = din("w_in_even", [D, EVEN_COLS])
    lora_w_d = din("lora_w", [128, 512])
    gate_up_d = din("gate_up", [160, 512])
    w_out_even = din("w_out_even", [D, D])
    w_in_odd = din("w_in_odd", [D, 2 * LRU_W])
    w_rgate = din("w_rgate", [10, 128, 128])
    w_igate = din("w_igate", [10, 128, 128])
    w_out_odd = din("w_out_odd", [LRU_W, D])
    w_router = din("w_router", [2, D, 20])
    b_router = din("b_router", [2, 128, 20])
    exp_w_gate = din("exp_w_gate", [2, NEXP, D, DEXP])
    exp_w_up = din("exp_w_up", [2, NEXP, D, DEXP])
    exp_w_down = din("exp_w_down", [2, NEXP, DEXP, D])
    c_ident = din("ident", [128, 128])
    c_blockones = din("blockones", [128, 128])
    c_ones = din("ones", [128, 128])
    c_triu = din("triu", [128, 128])
    c_maskrel = din("maskrel", [128, 4, 512])
    c_m64 = din("m64", [64, 4, 64])
    c_sel = din("sel", [16, 16, 128])

    projT = nc.dram_tensor("projT", [EVEN_COLS, NTOK], F32, kind=dbgkind).ap()
    catT = nc.dram_tensor("catT", [D, NTOK], BF16, kind="Internal").ap()
    x1 = nc.dram_tensor("x1", [NTOK, D], F32, kind=dbgkind).ap()
    x2 = nc.dram_tensor("x2", [NTOK, D], F32, kind=dbgkind).ap()
    x3 = nc.dram_tensor("x3", [NTOK, D], F32, kind=dbgkind).ap()
    gateT = nc.dram_tensor("gateT", [LRU_W, NTOK], F32, kind="Internal").ap()
    recT = nc.dram_tensor("recT", [LRU_W, NTOK], F32, kind="Internal").ap()
    cat2T = nc.dram_tensor("cat2T", [LRU_W, NTOK], BF16, kind="Internal").ap()
    if debug:
        catT_dbg = nc.dram_tensor("catT_dbg", [D, NTOK], F32, kind="ExternalOutput").ap()
        cat2T_dbg = nc.dram_tensor("cat2T_dbg", [LRU_W, NTOK], F32, kind="ExternalOutput").ap()

    wgu_bf = nc.dram_tensor("wgu_bf", [2, NEXP, 128, 2, 8, DEXP], BF16, kind="Internal").ap()
    wdn_bf = nc.dram_tensor("wdn_bf", [2, NEXP, 128, 4, D], BF16, kind="Internal").ap()
    PC = pcol_idx
    dbufs = {}
    dump_list = []

    def dump(k, name, buf, ap, shape):
        if not debug:
            return
        d = nc.dram_tensor("dump_" + name, list(shape), F32, kind="ExternalOutput").ap()
        k.dma("sp", d, ap, [buf], [])

    def dbuf(name, idx):
        b = dbufs.get((name, idx))
        if b is None:
            b = Buf(None)
            dbufs[(name, idx)] = b
        return b

    with ExitStack() as es:
        k = K(nc, es)

        def sb(es_, name, shape, dt=F32):
            return Buf(es_.enter_context(nc.sbuf_tensor("sb_" + name, list(shape), dt)))

        def ps(es_, name, shape, dt=F32):
            return Buf(es_.enter_context(nc.psum_tensor("ps_" + name, list(shape), dt)))

        pc = sb(es, "pc", [128, npc])
        ident = sb(es, "ident", [128, 128])
        identb = sb(es, "identb", [128, 128], BF16)
        blockones = sb(es, "blockones", [128, 128])
        k.dma("sp", pc.t[:], pc_d[:, :], (), [pc])
        k.dma("sp", ident.t[:], c_ident[:, :], (), [ident])
        k.dma("pool", identb.t[:], c_ident[:, :], (), [identb])
        k.dma("sp", blockones.t[:], c_blockones[:, :], (), [blockones])

        epsc = sb(es, "epsc", [128, 4])
        k.memset(epsc.t[:, 0:1], RMS_EPS, [epsc])
        k.memset(epsc.t[:, 1:2], GN_EPS, [epsc])
        k.memset(epsc.t[:, 2:3], 1.0, [epsc])

        def pcol(name, rows=128):
            i = PC[name]
            return pc.t[0:rows, i:i + 1]

        def rmsnorm_T(xt, gb, hT, j, sq, ssb, hb, ptr):
            k.tt(sq.t[:], xt.t[:], xt.t[:], ALU.mult, [xt], [sq])
            k.op("dve", lambda g: g.reduce_sum(out=ssb.t[:, 0:1], in_=sq.t[:], axis=AX.X), [sq], [ssb])
            k.rsqrt(ssb.t[:, 2:3], ssb.t[:, 0:1], 1.0 / D, epsc.t[:, 0:1], [ssb, epsc], [ssb])
            k.stt(hb.t[:], xt.t[:], ssb.t[:, 2:3], gb.t[:], ALU.mult, ALU.mult, [xt, ssb, gb], [hb])
            for kc in range(8):
                k.tr(ptr.t[:, kc * 128:(kc + 1) * 128], hb.t[:, kc * 128:(kc + 1) * 128], identb.t[:], [hb, identb], [ptr])
            k.copy(hT.t[:, :, j * 128:(j + 1) * 128], ptr.t[:].rearrange("p (c t) -> p c t", t=128), [ptr], [hT], e="act")

        def precast(layer):
            for e in range(NEXP):
                wb = dbuf("wbf", (layer, e))
                k.dma("pre", wgu_bf[layer, e, :, 0, :, :], exp_w_gate[layer, e].rearrange("(c p) f -> p c f", p=128), (), [wb])
                k.dma("pre", wgu_bf[layer, e, :, 1, :, :], exp_w_up[layer, e].rearrange("(c p) f -> p c f", p=128), (), [wb])
                k.dma("pre", wdn_bf[layer, e, :, :, :], exp_w_down[layer, e].rearrange("(c p) d -> p c d", p=128), (), [wb])

        def phase_in(x_src, x_name, gidx, w_d, ncols, chunks, dst_fn, tagp, after_loads=None):
            with ExitStack() as pes:
                wsb = sb(pes, tagp + "w", [128, 8, ncols], BF16)
                gb = sb(pes, tagp + "gb", [128, D])
                k.dma("sp", gb.t[:], gbc_d[gidx], (), [gb])
                for kc in range(8):
                    k.dma("pool", wsb.t[:, kc, :], w_d[kc * 128:(kc + 1) * 128, :], (), [wsb])
                if after_loads is not None:
                    after_loads()
                xts = [sb(pes, tagp + "xt%d" % i, [128, D]) for i in range(2)]
                sq = sb(pes, tagp + "sq", [128, D])
                ssb = sb(pes, tagp + "ss", [128, 4])
                hb = sb(pes, tagp + "hb", [128, D], BF16)
                hTs = [sb(pes, tagp + "hT%d" % i, [128, 8, 512], BF16) for i in range(2)]
                ptr = ps(pes, tagp + "ptr", [128, 1024], BF16)
                pps = [ps(pes, tagp + "pp%d" % i, [128, 512]) for i in range(3)]
                stg = [sb(pes, tagp + "stg%d" % i, [128, 512]) for i in range(3)]
                n = 0
                for tt_ in range(NT512):
                    hT = hTs[tt_ % 2]
                    for j in range(4):
                        xt = xts[(tt_ * 4 + j) % 2]
                        r0 = tt_ * 512 + j * 128
                        k.dma("sp", xt.t[:], x_src[r0:r0 + 128, :], [dbuf(x_name, r0 // 128)], [xt])
                        rmsnorm_T(xt, gb, hT, j, sq, ssb, hb, ptr)
                    for ci, (c0, m) in enumerate(chunks):
                        pp = pps[n % 3]
                        st = stg[n % 3]
                        n += 1
                        for kc in range(8):
                            k.mm(pp.t[0:m, :], wsb.t[:, kc, c0:c0 + m], hT.t[:, kc, :], kc == 0, kc == 7, [wsb, hT], [pp])
                        dst_fn(ci, c0, m, tt_, pp, st)
            k.barrier()

        def l0_dst(ci, c0, m, tt_, pp, st):
            k.copy(st.t[0:m, :], pp.t[0:m, :], [pp], [st], e=("act" if ci % 2 else "dve"))
            k.dma("sp", projT[c0:c0 + m, tt_ * 512:(tt_ + 1) * 512], st.t[0:m, :], [st], [dbuf("projT", tt_ * 512 // T)])

        if "l0in" in phases:
            phase_in(x_in, "x", 0, w_in_even, EVEN_COLS, IN_CHUNKS, l0_dst, "a_", after_loads=(lambda: (precast(0), precast(1) if "l1out" in phases else None)) if "l0out" in phases else None)

        def phase_rwkv():
            with ExitStack() as pes:
                SEG = 256
                lw_t = sb(pes, "r_lw", [128, 512])
                gu1 = sb(pes, "r_gu1", [128, 512])
                gu2 = sb(pes, "r_gu2", [32, 512])
                m64 = sb(pes, "r_m64", [64, 4, 64])
                onesf = sb(pes, "r_ones", [128, 64])
                k.dma("sp", lw_t.t[:], lora_w_d[:, :], (), [lw_t])
                k.dma("sp", gu1.t[:], gate_up_d[0:128, :], (), [gu1])
                k.dma("sp", gu2.t[:], gate_up_d[128:160, :], (), [gu2])
                k.dma("sp", m64.t[:], c_m64[:, :, :], (), [m64])
                k.memset(onesf.t[:], 1.0, [onesf])
                raw = {nm: sb(pes, "r_raw_" + nm, [128, 4, SEG + 1]) for nm in ("r", "k", "v")}
                rawl = sb(pes, "r_rawl", [128, 3, SEG + 1])
                sh = {nm: sb(pes, "r_sh_" + nm, [128, 4, SEG]) for nm in ("r", "k", "v")}
                shl = sb(pes, "r_shl", [128, 3, SEG])
                tmpA = sb(pes, "r_tmpA", [128, 4, SEG])
                tmpB = sb(pes, "r_tmpB", [128, 4, SEG])
                sg = sb(pes, "r_sg", [128, 4, SEG])
                aic = sb(pes, "r_aic", [128, 4, SEG])
                gg = sb(pes, "r_g", [128, 4, SEG])
                kk = sb(pes, "r_kk", [128, 4, SEG])
                kmod = sb(pes, "r_kmod", [128, 4, SEG])
                bonus = sb(pes, "r_bonus", [128, 4, SEG])
                cs = sb(pes, "r_cs", [128, 4, SEG])
                Ep = sb(pes, "r_Ep", [128, 4, SEG])
                En = sb(pes, "r_En", [128, 4, SEG])
                Em = sb(pes, "r_Em", [128, 4, SEG])
                MD = BF16
                rt = sb(pes, "r_rt", [128, 4, SEG], MD)
                kt = sb(pes, "r_kt", [128, 4, SEG], MD)
                bt = sb(pes, "r_bt", [128, 4, SEG], MD)
                at = sb(pes, "r_at", [128, 4, SEG], MD)
                vb16 = sb(pes, "r_vb16", [128, 4, SEG], MD)
                PCp = sb(pes, "r_PCp", [128, 4, SEG // 64])
                PCH = sb(pes, "r_PCH", [64, 8, SEG // 64])
                yseg = sb(pes, "r_y", [128, 4, SEG])
                yab = sb(pes, "r_yab", [128, 4, SEG], BF16)
                def sbm(name, shape, dt=F32):
                    return MBuf(pes.enter_context(nc.sbuf_tensor("sb_" + name, list(shape), dt)))

                def psm(name, shape, dt=F32):
                    return MBuf(pes.enter_context(nc.psum_tensor("ps_" + name, list(shape), dt)))

                S = sbm("r_S", [64, 8, 64])
                Stmp = sbm("r_Stmp", [64, 8, 64])
                Sb = sbm("r_Sb", [64, 8, 64], MD)
                X = [sbm("r_X%d" % i, [64, 8, 64], MD) for i in range(2)]
                Y = [sbm("r_Y%d" % i, [64, 8, 64], MD) for i in range(2)]
                Z = [sbm("r_Z%d" % i, [64, 8, 64], MD) for i in range(2)]
                LakT = sbm("r_LakT", [64, 8, 64], MD)
                MrbT = sbm("r_MrbT", [64, 8, 64], MD)
                MrkT = sbm("r_MrkT", [64, 8, 64], MD)
                Vt = sbm("r_Vt", [64, 8, 64], MD)
                Btk = sbm("r_Btk", [64, 8, 64], MD)
                Ktk = sbm("r_Ktk", [64, 8, 64], MD)
                Wsb = sbm("r_Wsb", [64, 8, 64], MD)
                Usb = sbm("r_Usb", [64, 8, 64], MD)
                atH = sb(pes, "r_atH", [64, 8, SEG], MD)
                btH = sb(pes, "r_btH", [64, 8, SEG], MD)
                ktH = sb(pes, "r_ktH", [64, 8, SEG], MD)
                rtH = sb(pes, "r_rtH", [64, 8, SEG], MD)
                yH = sbm("r_yH", [64, 8, SEG])
                QG = [[ps(pes, "r_Q%d%d" % (g_, i), [128, 512]) for i in range(4)] for g_ in range(2)]
                P1, P2, P3, P4 = QG[0]

                def v864(b):
                    return b.t[0:64, :].rearrange("p (h j) -> p h j", j=64)

                def v8128(b):
                    return b.t[0:64, :].rearrange("p (h j) -> p h j", j=128)

                def v4128(b, rows=128):
                    return b.t[0:rows, 0:512].rearrange("p (h j) -> p h j", j=128)

                mU = m64.t[:, 0:1, :].to_broadcast([64, 4, 64])
                mUI = m64.t[:, 1:2, :].to_broadcast([64, 4, 64])
                mL = m64.t[:, 2:3, :].to_broadcast([64, 4, 64])
                mI = m64.t[:, 3:4, :].to_broadcast([64, 4, 64])

                for s in range(NSEQ):
                    k.memset(S.t[:], 0.0, [S])
                    k.memset(Sb.t[:], 0.0, [Sb])
                    for seg in range(T // SEG):
                        tok0 = s * T + seg * SEG
                        dep = [dbuf("projT", s)]
                        def load(dst_ap, dstbuf, r0, m):
                            if seg == 0:
                                k.memset(dst_ap[0:m, 0:1], 0.0, [dstbuf])
                                k.dma("sp", dst_ap[0:m, 1:SEG + 1], projT[r0:r0 + m, tok0:tok0 + SEG], dep, [dstbuf])
                            else:
                                k.dma("sp", dst_ap[0:m, 0:SEG + 1], projT[r0:r0 + m, tok0 - 1:tok0 + SEG], dep, [dstbuf])
                        for i, nm in enumerate(("r", "k", "v")):
                            for p in range(4):
                                load(raw[nm].t[:, p, :], raw[nm], i * 512 + p * 128, 128)
                        load(rawl.t[:, 0, :], rawl, 1536, 128)
                        load(rawl.t[:, 1, :], rawl, 1664, 128)
                        load(rawl.t[:, 2, :], rawl, 1792, 32)
                        def shift(dst, dbuf_, src, sbuf_, m, mucol):
                            k.tt(tmpA.t[0:m, 0, :], src[0:m, 0:SEG], src[0:m, 1:SEG + 1], ALU.subtract, [sbuf_], [tmpA])
                            k.stt(dst[0:m, :], tmpA.t[0:m, 0, :], pcol(mucol, m), src[0:m, 1:SEG + 1], ALU.mult, ALU.add, [tmpA, sbuf_, pc], [dbuf_])
                        for i, nm in enumerate(("r", "k", "v")):
                            for p in range(4):
                                shift(sh[nm].t[:, p, :], sh[nm], raw[nm].t[:, p, :], raw[nm], 128, "mu%d" % (i * 4 + p))
                        shift(shl.t[:, 0, :], shl, rawl.t[:, 0, :], rawl, 128, "mu12")
                        shift(shl.t[:, 1, :], shl, rawl.t[:, 1, :], rawl, 128, "mu13")
                        shift(shl.t[:, 2, :], shl, rawl.t[:, 2, :], rawl, 32, "mu14")
                        STOP = int(os.environ.get("RWKV_STOP", "99"))
                        if STOP <= 0:
                            continue
                        k.act(shl.t[0:64, 0, :], shl.t[0:64, 0, :], AF.Tanh, [shl], [shl])
                        k.act(shl.t[:, 1, :], shl.t[:, 1, :], AF.Sigmoid, [shl], [shl])
                        k.act(shl.t[0:32, 2, :], shl.t[0:32, 2, :], AF.Sigmoid, [shl], [shl])
                        for p in range(4):
                            pch = slice(p * 128, (p + 1) * 128)
                            k.mm(P1.t[:, 0:SEG], lw_t.t[0:64, pch], shl.t[0:64, 0, :], True, True, [lw_t, shl], [P1])
                            k.act(sg.t[:, p, :], P1.t[:, 0:SEG], AF.Sigmoid, [P1, pc], [sg], bias=pcol("w0_%d" % p))
                            k.mm(P2.t[:, 0:SEG], lw_t.t[64:128, pch], shl.t[64:128, 0, :], True, True, [lw_t, shl], [P2])
                            k.act(aic.t[:, p, :], P2.t[:, 0:SEG], AF.Sigmoid, [P2, pc], [aic], bias=pcol("a0_%d" % p))
                            k.mm(P3.t[:, 0:SEG], gu1.t[:, pch], shl.t[:, 1, :], True, False, [gu1, shl], [P3])
                            k.mm(P3.t[:, 0:SEG], gu2.t[0:32, pch], shl.t[0:32, 2, :], False, True, [gu2, shl], [P3])
                            k.copy(gg.t[:, p, :], P3.t[:, 0:SEG], [P3], [gg], e="act")
                            k.ts(tmpA.t[:, p, :], sh["k"].t[:, p, :], pcol("kk_%d" % p), None, ALU.mult, None, [sh["k"], pc], [tmpA])
                            k.tt(tmpB.t[:, p, :], tmpA.t[:, p, :], tmpA.t[:, p, :], ALU.mult, [tmpA], [tmpB])
                            k.mm(P4.t[:, 0:SEG], blockones.t[:], tmpB.t[:, p, :], True, True, [blockones, tmpB], [P4])
                            k.rsqrt(tmpB.t[:, p, :], P4.t[:, 0:SEG], 1.0, None, [P4], [tmpB], floor=1e-12)
                            k.tt(kk.t[:, p, :], tmpA.t[:, p, :], tmpB.t[:, p, :], ALU.mult, [tmpA, tmpB], [kk])
                            k.ts(tmpA.t[:, p, :], aic.t[:, p, :], -1.0, pcol("ka_%d" % p), ALU.add, ALU.mult, [aic, pc], [tmpA])
                            k.stt(kmod.t[:, p, :], tmpA.t[:, p, :], 1.0, sh["k"].t[:, p, :], ALU.add, ALU.mult, [tmpA, sh["k"]], [kmod])
                            k.stt(tmpB.t[:, p, :], sh["r"].t[:, p, :], pcol("rk_%d" % p), kmod.t[:, p, :], ALU.mult, ALU.mult, [sh["r"], kmod, pc], [tmpB])
                            k.mm(P1.t[:, 0:SEG], blockones.t[:], tmpB.t[:, p, :], True, True, [blockones, tmpB], [P1])
                            k.tt(bonus.t[:, p, :], P1.t[:, 0:SEG], sh["v"].t[:, p, :], ALU.mult, [P1, sh["v"]], [bonus])
                            for c in range(SEG // 64):
                                ch = slice(c * 64, (c + 1) * 64)
                                k.op("dve", lambda g, p=p, ch=ch: g.tensor_tensor_scan(cs.t[:, p, ch], onesf.t[:, :], sg.t[:, p, ch], 0.0, ALU.mult, ALU.add), [sg, onesf], [cs])
                        if STOP <= 1:
                            continue
                        k.tt(tmpA.t[:], cs.t[:], sg.t[:], ALU.subtract, [cs, sg], [tmpA])
                        k.act(Ep.t[:], cs.t[:], AF.Exp, [cs], [Ep], scale=-DECAY_C)
                        k.act(En.t[:], cs.t[:], AF.Exp, [cs], [En], scale=DECAY_C)
                        k.act(Em.t[:], tmpA.t[:], AF.Exp, [tmpA], [Em], scale=-DECAY_C)
                        k.tt(rt.t[:], sh["r"].t[:], Ep.t[:], ALU.mult, [sh["r"], Ep], [rt])
                        k.tt(kt.t[:], kmod.t[:], En.t[:], ALU.mult, [kmod, En], [kt])
                        k.tt(tmpB.t[:], kk.t[:], aic.t[:], ALU.mult, [kk, aic], [tmpB])
                        k.tt(bt.t[:], tmpB.t[:], En.t[:], ALU.mult, [tmpB, En], [bt])
                        k.stt(at.t[:], kk.t[:], -1.0, Em.t[:], ALU.mult, ALU.mult, [kk, Em], [at])
                        k.copy(PCp.t[:], Ep.t[:].rearrange("p a (c j) -> p a c j", j=64)[:, :, :, 63], [Ep], [PCp], e="dve")
                        k.copy(vb16.t[:], sh["v"].t[:], [sh["v"]], [vb16], e="act")
                        if s == 0 and seg == 0:
                            for nm_, b_ in (("shr", sh["r"]), ("shk", sh["k"]), ("shv", sh["v"]), ("sg", sg), ("aic", aic), ("gg", gg), ("kk", kk),
                                            ("kmod", kmod), ("bonus", bonus), ("cs", cs), ("Ep", Ep), ("En", En), ("Em", Em),
                                            ):
                                dump(k, nm_, b_, b_.t[:], [128, 4, SEG])
                        if STOP <= 2:
                            continue
                        def to_head(dstb, srcb):
                            dv = dstb.t[:].rearrange("q (p b) t -> q p b t", b=2)
                            k.dma("sp", dv[:, :, 0, :], srcb.t[0:64, :, :], [srcb], [dstb])
                            k.dma("sp", dv[:, :, 1, :], srcb.t[64:128, :, :], [srcb], [dstb])
                        to_head(atH, at)
                        to_head(btH, bt)
                        to_head(ktH, kt)
                        to_head(rtH, rt)
                        to_head(PCH, PCp)
                        identm = identb if MD == BF16 else ident

                        def chunk_gen(c, g):
                            ch = slice(c * 64, (c + 1) * 64)
                            H = slice(4 * g, 4 * g + 4)
                            hl = list(range(4 * g, 4 * g + 4))
                            bmap = {1: 0, 2: 1, 3: 2, 5: 0, 6: 1, 7: 2, 8: 3}
                            pb = {i: QG[g][bi] for i, bi in bmap.items()}
                            pv = {i: pb[i].t[0:64, 0:256].rearrange("q (h j) -> q h j", j=64) for i in bmap}

                            def T_(mb):
                                return mb.t[:, H, :]
                            Xg = [X[0].ch[g], X[1].ch[g]]
                            Yg = [Y[0].ch[g], Y[1].ch[g]]
                            Zg = [Z[0].ch[g], Z[1].ch[g]]
                            for j_, h in enumerate(hl):
                                k.mm(pv[1][:, j_, :], btH.t[:, h, ch], atH.t[:, h, ch], True, True, [btH, atH], [pb[1]])
                                k.mm(pv[2][:, j_, :], atH.t[:, h, ch], btH.t[:, h, ch], True, True, [btH, atH], [pb[2]])
                                k.mm(pv[3][:, j_, :], ktH.t[:, h, ch], atH.t[:, h, ch], True, True, [ktH, atH], [pb[3]])
                            k.tt(T_(X[0]), pv[1], mU, ALU.mult, [pb[1], m64], [Xg[0]])
                            k.tt(T_(Y[0]), pv[2], mL, ALU.mult, [pb[2], m64], [Yg[0]])
                            k.tt(T_(LakT), pv[3], mU, ALU.mult, [pb[3], m64], [LakT.ch[g]])
                            k.tt(T_(Z[0]), T_(X[0]), mI, ALU.add, [Xg[0], m64], [Zg[0]])
                            yield
                            for j_, h in enumerate(hl):
                                k.mm(pv[1][:, j_, :], Y[0].t[:, h, :], X[0].t[:, h, :], True, True, [Xg[0], Yg[0]], [pb[1]])
                                k.mm(pv[2][:, j_, :], X[0].t[:, h, :], Y[0].t[:, h, :], True, True, [Xg[0], Yg[0]], [pb[2]])
                            k.copy(T_(X[1]), pv[1], [pb[1]], [Xg[1]], e="act")
                            k.copy(T_(Y[1]), pv[2], [pb[2]], [Yg[1]], e="dve")
                            yield
                            xi, zi = 1, 0
                            for r in range(1, 6):
                                for j_, h in enumerate(hl):
                                    k.mm(pv[3][:, j_, :], Y[xi].t[:, h, :], Z[zi].t[:, h, :], True, True, [Yg[xi], Zg[zi]], [pb[3]])
                                    if r < 5:
                                        k.mm(pv[2][:, j_, :], X[xi].t[:, h, :], Y[xi].t[:, h, :], True, True, [Xg[xi], Yg[xi]], [pb[2]])
                                    if r < 4:
                                        k.mm(pv[1][:, j_, :], Y[xi].t[:, h, :], X[xi].t[:, h, :], True, True, [Xg[xi], Yg[xi]], [pb[1]])
                                k.tt(T_(Z[1 - zi]), pv[3], T_(Z[zi]), ALU.add, [pb[3], Zg[zi]], [Zg[1 - zi]])
                                if r < 5:
                                    k.copy(T_(Y[1 - xi]), pv[2], [pb[2]], [Yg[1 - xi]], e="act")
                                if r < 4:
                                    k.copy(T_(X[1 - xi]), pv[1], [pb[1]], [Xg[1 - xi]], e="dve")
                                xi, zi = 1 - xi, 1 - zi
                                yield
                            Zf, Zfb = Z[zi], Zg[zi]
                            for j_, h in enumerate(hl):
                                k.mm(pv[1][:, j_, :], btH.t[:, h, ch], rtH.t[:, h, ch], True, True, [btH, rtH], [pb[1]])
                                k.mm(pv[2][:, j_, :], ktH.t[:, h, ch], rtH.t[:, h, ch], True, True, [ktH, rtH], [pb[2]])
                            k.tt(T_(MrbT), pv[1], mUI, ALU.mult, [pb[1], m64], [MrbT.ch[g]])
                            k.tt(T_(MrkT), pv[2], mUI, ALU.mult, [pb[2], m64], [MrkT.ch[g]])
                            yield
                            tvs = [P.t[:].bitcast(BF16) if MD == BF16 else P.t[:] for P in (pb[1], pb[2], pb[3])]
                            for pp in range(2):
                                p = 2 * g + pp
                                cs_ = slice(pp * 128, (pp + 1) * 128)
                                k.tr(tvs[0][0:64, cs_], vb16.t[:, p, ch], identm.t[:], [vb16, identm], [pb[1]])
                                k.tr(tvs[1][0:64, cs_], bt.t[:, p, ch], identm.t[:], [bt, identm], [pb[2]])
                                k.tr(tvs[2][0:64, cs_], kt.t[:, p, ch], identm.t[:], [kt, identm], [pb[3]])
                            gv = [tv[0:64, 0:256].rearrange("q (h j) -> q h j", j=64) for tv in tvs]
                            k.copy(T_(Vt), gv[0], [pb[1]], [Vt.ch[g]], e="act")
                            k.copy(T_(Btk), gv[1], [pb[2]], [Btk.ch[g]], e="dve")
                            k.copy(T_(Ktk), gv[2], [pb[3]], [Ktk.ch[g]], e="act")
                            yield
                            for j_, h in enumerate(hl):
                                k.mm(pv[5][:, j_, :], atH.t[:, h, ch], Sb.t[:, h, :], True, False, [atH, Sb.ch[g]], [pb[5]])
                                k.mm(pv[5][:, j_, :], LakT.t[:, h, :], Vt.t[:, h, :], False, True, [LakT.ch[g], Vt.ch[g]], [pb[5]])
                            k.copy(T_(Wsb), pv[5], [pb[5]], [Wsb.ch[g]], e="act")
                            yield
                            for j_, h in enumerate(hl):
                                k.mm(pv[6][:, j_, :], Zf.t[:, h, :], Wsb.t[:, h, :], True, True, [Zfb, Wsb.ch[g]], [pb[6]])
                            k.copy(T_(Usb), pv[6], [pb[6]], [Usb.ch[g]], e="dve")
                            yield
                            for j_, h in enumerate(hl):
                                k.mm(pv[7][:, j_, :], Sb.t[:, h, :], rtH.t[:, h, ch], True, False, [Sb.ch[g], rtH], [pb[7]])
                                k.mm(pv[7][:, j_, :], Usb.t[:, h, :], MrbT.t[:, h, :], False, False, [Usb.ch[g], MrbT.ch[g]], [pb[7]])
                                k.mm(pv[7][:, j_, :], Vt.t[:, h, :], MrkT.t[:, h, :], False, True, [Vt.ch[g], MrkT.ch[g]], [pb[7]])
                            for j_, h in enumerate(hl):
                                k.mm(pv[8][:, j_, :], Btk.t[:, h, :], Usb.t[:, h, :], True, False, [Btk.ch[g], Usb.ch[g]], [pb[8]])
                                k.mm(pv[8][:, j_, :], Ktk.t[:, h, :], Vt.t[:, h, :], False, True, [Ktk.ch[g], Vt.ch[g]], [pb[8]])
                            k.copy(yH.t[:, H, ch], pv[7], [pb[7]], [yH.ch[g]], e="act")
                            pcc = PCH.t[:, H, c:c + 1].to_broadcast([64, 4, 64])
                            k.tt(T_(Stmp), T_(S), pv[8], ALU.add, [S.ch[g], pb[8]], [Stmp.ch[g]])
                            k.tt(T_(S), T_(Stmp), pcc, ALU.mult, [Stmp.ch[g], PCH], [S.ch[g]])
                            k.copy(T_(Sb), T_(S), [S.ch[g]], [Sb.ch[g]], e="act")
                            yield

                        for c in range(SEG // 64):
                            gens = [chunk_gen(c, 0), chunk_gen(c, 1)]
                            alive = [True, True]
                            while any(alive):
                                for gi in range(2):
                                    if alive[gi]:
                                        try:
                                            next(gens[gi])
                                        except StopIteration:
                                            alive[gi] = False
                        yv = yH.t[:].rearrange("q (p b) t -> q p b t", b=2)
                        k.dma("sp", yseg.t[0:64, :, :], yv[:, :, 0, :], [yH], [yseg])
                        k.dma("sp", yseg.t[64:128, :, :], yv[:, :, 1, :], [yH], [yseg])
                        if s == 0 and seg == 0:
                            dump(k, "yseg", yseg, yseg.t[:], [128, 4, SEG])
                        for p in range(4):
                            k.mm(P1.t[:, 0:SEG], blockones.t[:], yseg.t[:, p, :], True, True, [blockones, yseg], [P1])
                            k.stt(tmpA.t[:, p, :], P1.t[:, 0:SEG], -1.0 / 64, yseg.t[:, p, :], ALU.mult, ALU.add, [P1, yseg], [tmpA])
                            k.tt(tmpB.t[:, p, :], tmpA.t[:, p, :], tmpA.t[:, p, :], ALU.mult, [tmpA], [tmpB])
                            k.mm(P2.t[:, 0:SEG], blockones.t[:], tmpB.t[:, p, :], True, True, [blockones, tmpB], [P2])
                            k.rsqrt(tmpB.t[:, p, :], P2.t[:, 0:SEG], 1.0 / 64, epsc.t[:, 1:2], [P2, epsc], [tmpB])
                            k.tt(tmpA.t[:, p, :], tmpA.t[:, p, :], tmpB.t[:, p, :], ALU.mult, [tmpA, tmpB], [tmpA])
                            k.ts(tmpA.t[:, p, :], tmpA.t[:, p, :], pcol("gnw_%d" % p), pcol("gnb_%d" % p), ALU.mult, ALU.add, [tmpA, pc], [tmpA])
                            k.tt(tmpA.t[:, p, :], tmpA.t[:, p, :], bonus.t[:, p, :], ALU.add, [tmpA, bonus], [tmpA])
                            k.tt(yab.t[:, p, :], tmpA.t[:, p, :], gg.t[:, p, :], ALU.mult, [tmpA, gg], [yab])
                            k.dma("sp", catT[p * 128:(p + 1) * 128, tok0:tok0 + SEG], yab.t[:, p, :], [yab], [dbuf("catT", s)])
            k.barrier()

        if "rwkv" in phases:
            phase_rwkv()

        def phase_attn():
            with ExitStack() as pes:
                triu = sb(pes, "s_triu", [128, 128], BF16)
                onesb = sb(pes, "s_ones", [128, 128], BF16)
                maskb = sb(pes, "s_maskb", [128, 4, 512], BF16)
                k.dma("pool", triu.t[:], c_triu[:, :], (), [triu])
                k.dma("pool", onesb.t[:], c_ones[:, :], (), [onesb])
                k.dma("pool", maskb.t[:], c_maskrel[:, :, :], (), [maskb])
                qraw = sb(pes, "s_qraw", [128, T])
                kraw = sb(pes, "s_kraw", [128, T])
                vb = sb(pes, "s_vb", [128, T], BF16)
                sq = sb(pes, "s_sq", [128, T])
                rs = sb(pes, "s_rs", [128, T])
                qn2 = [sb(pes, "s_qn%d" % i, [128, T], BF16) for i in range(2)]
                kn2 = [sb(pes, "s_kn%d" % i, [128, T], BF16) for i in range(2)]
                vtok2 = [sb(pes, "s_vtok%d" % i, [128, T // 128, 128], BF16) for i in range(2)]
                ND = 6
                spb = [sb(pes, "s_sp%d" % i, [128, 512], BF16) for i in range(ND)]
                zsb = [sb(pes, "s_zs%d" % i, [128, 512]) for i in range(ND)]
                t1 = [sb(pes, "s_t1%d" % i, [128, 512]) for i in range(ND)]
                att = [sb(pes, "s_att%d" % i, [128, 512], BF16) for i in range(ND)]
                ob = [sb(pes, "s_ob%d" % i, [64, 512], BF16) for i in range(2)]
                PZ = [ps(pes, "s_PZ%d" % i, [128, 512]) for i in range(2)]
                PT = [ps(pes, "s_PT%d" % i, [128, 512]) for i in range(2)]
                PR = [ps(pes, "s_PR%d" % i, [128, 512]) for i in range(2)]
                PV = [ps(pes, "s_PV%d" % i, [128, 512]) for i in range(2)]
                Rb = sb(pes, "s_R", [128, 512])
                qnn2 = [sb(pes, "s_qnn%d" % i, [128, T], BF16) for i in range(2)]

                def prep(s, p, par):
                    dep = [dbuf("projT", s)]
                    ts0 = s * T
                    qn, kn, vtok = qn2[par], kn2[par], vtok2[par]
                    k.dma("sp", qraw.t[:], projT[A_COLS + p * 128:A_COLS + (p + 1) * 128, ts0:ts0 + T], dep, [qraw])
                    k.dma("sp", kraw.t[:], projT[A_COLS + 512 + p * 128:A_COLS + 512 + (p + 1) * 128, ts0:ts0 + T], dep, [kraw])
                    k.dma("pool", vb.t[:], projT[A_COLS + 1024 + p * 128:A_COLS + 1024 + (p + 1) * 128, ts0:ts0 + T], dep, [vb])
                    for (rawb, outb, gname) in ((qraw, qn, "qg"), (kraw, kn, "kg")):
                        k.tt(sq.t[:], rawb.t[:], rawb.t[:], ALU.mult, [rawb], [sq])
                        for f in range(T // 512):
                            fs = slice(f * 512, (f + 1) * 512)
                            pz = PZ[f % 2]
                            k.mm(pz.t[:], blockones.t[:], sq.t[:, fs], True, True, [blockones, sq], [pz])
                            k.rsqrt(rs.t[:, fs], pz.t[:], 1.0 / 64, epsc.t[:, 0:1], [pz, epsc], [rs])
                        k.stt(outb.t[:], rawb.t[:], pcol(gname), rs.t[:], ALU.mult, ALU.mult, [rawb, rs, pc], [outb])
                    k.ts(qnn2[par].t[:], qn.t[:], -0.125, None, ALU.mult, None, [qn], [qnn2[par]])
                    for kb in range(T // 128):
                        pz = PT[kb % 2]
                        pzb = pz.t[:].bitcast(BF16)
                        k.tr(pzb[:, 0:128], vb.t[:, kb * 128:(kb + 1) * 128], identb.t[:], [vb, identb], [pz])
                        k.copy(vtok.t[:, kb, :], pzb[:, 0:128], [pz], [vtok], e="act")

                pairs = [(s, p) for s in range(NSEQ) for p in range(4)]
                steps = []
                for pi, (s, p) in enumerate(pairs):
                    for hh in range(2):
                        for QT in range(T // 512):
                            kb_hi = QT * 4 + 3
                            for kb in range(kb_hi, -1, -1):
                                steps.append((pi, s, p, hh, QT, kb, kb_hi))
                qtidx = {}
                for st in steps:
                    key = (st[0], st[3], st[4])
                    if key not in qtidx:
                        qtidx[key] = len(qtidx)

                def stepA(n):
                    pi, s, p, hh, QT, kb, kb_hi = steps[n]
                    R = slice(hh * 64, hh * 64 + 64)
                    qs = slice(QT * 512, (QT + 1) * 512)
                    qn, kn = qn2[pi % 2], kn2[pi % 2]
                    pz, sp_, zs_ = PZ[n % 2], spb[n % ND], zsb[n % ND]
                    rel = kb - QT * 4
                    k.mm(pz.t[:], kn.t[R, kb * 128:(kb + 1) * 128], qn.t[R, qs], True, True, [kn, qn], [pz])
                    k.act(zs_.t[:], pz.t[:], AF.Exp, [pz], [zs_], scale=0.125)
                    k.act(sp_.t[:], zs_.t[:], AF.Ln, [zs_, epsc], [sp_], bias=epsc.t[:, 2:3])
                    if rel >= 0:
                        k.tt(sp_.t[:], sp_.t[:], maskb.t[:, rel, :], ALU.mult, [sp_, maskb], [sp_])

                def stepB(n):
                    pi, s, p, hh, QT, kb, kb_hi = steps[n]
                    R = slice(hh * 64, hh * 64 + 64)
                    qs = slice(QT * 512, (QT + 1) * 512)
                    kn, qnn = kn2[pi % 2], qnn2[pi % 2]
                    pt, po = PT[n % 2], PR[n % 2]
                    sp_, t1_, at_ = spb[n % ND], t1[n % ND], att[n % ND]
                    first = kb == kb_hi
                    k.mm(pt.t[:], kn.t[R, kb * 128:(kb + 1) * 128], qnn.t[R, qs], True, False, [kn, qnn], [pt])
                    k.mm(pt.t[:], triu.t[:], sp_.t[:], False, True, [triu, sp_], [pt])
                    if kb > 0:
                        k.mm(po.t[:], onesb.t[:], sp_.t[:], True, True, [onesb, sp_], [po])
                    if first:
                        k.act(at_.t[:], pt.t[:], AF.Exp, [pt], [at_], scale=-1.0)
                    else:
                        k.tt(t1_.t[:], pt.t[:], Rb.t[:], ALU.add, [pt, Rb], [t1_])
                        k.act(at_.t[:], t1_.t[:], AF.Exp, [t1_], [at_], scale=-1.0)
                    if kb > 0:
                        if first:
                            k.copy(Rb.t[:], po.t[:], [po], [Rb], e="dve")
                        else:
                            k.tt(Rb.t[:], Rb.t[:], po.t[:], ALU.add, [Rb, po], [Rb])

                def stepC(n):
                    pi, s, p, hh, QT, kb, kb_hi = steps[n]
                    R = slice(hh * 64, hh * 64 + 64)
                    h = 2 * p + hh
                    q = qtidx[(pi, hh, QT)] % 2
                    vtok = vtok2[pi % 2]
                    at_ = att[n % ND]
                    pv, obb = PV[q], ob[q]
                    rel = kb - QT * 4
                    if rel >= 0:
                        k.tt(at_.t[:], at_.t[:], maskb.t[:, rel, :], ALU.mult, [at_, maskb], [at_])
                    k.mm(pv.t[0:64, :], vtok.t[:, kb, R], at_.t[:], kb == kb_hi, kb == 0, [vtok, at_], [pv])
                    if kb == 0:
                        ts0 = s * T
                        k.copy(obb.t[:], pv.t[0:64, :], [pv], [obb], e="act")
                        k.dma("sp", catT[512 + h * 64:512 + (h + 1) * 64, ts0 + QT * 512:ts0 + (QT + 1) * 512], obb.t[:], [obb], [dbuf("catT", s)])

                NS_ = len(steps)
                per_pair = NS_ // len(pairs)
                prep(pairs[0][0], pairs[0][1], 0)
                DA, DB = 4, 2
                for n in range(-DA, NS_):
                    if n >= 0 and n % per_pair == 8:
                        pi = n // per_pair
                        if pi + 1 < len(pairs):
                            prep(pairs[pi + 1][0], pairs[pi + 1][1], (pi + 1) % 2)
                    if n + DA < NS_:
                        stepA(n + DA)
                    if 0 <= n + DB < NS_:
                        stepB(n + DB)
                    if n >= 0:
                        stepC(n)
            k.barrier()

        if "attn" in phases:
            phase_attn()

        def phase_out_moe(layer, cat_src, cat_name, nkc, w_out_d, x_src, x_name, gidx, x_mid, x_mid_name, x_dst, x_dst_name, tagp):
            TT = min(1024, NTOK)
            NSUB = TT // 128
            NH = TT // 512
            with ExitStack() as pes:
                wo = sb(pes, tagp + "wo", [128, nkc, D], BF16)
                for kc in range(nkc):
                    k.dma("pool", wo.t[:, kc, :], w_out_d[kc * 128:(kc + 1) * 128, :], (), [wo])
                gb = sb(pes, tagp + "gb", [128, D])
                k.dma("sp", gb.t[:], gbc_d[gidx], (), [gb])
                wr = sb(pes, tagp + "wr", [128, 8, 20])
                k.dma("sp", wr.t[:], w_router[layer].rearrange("(c p) n -> p c n", p=128), (), [wr])
                br = sb(pes, tagp + "br", [128, 20])
                k.dma("sp", br.t[:], b_router[layer], (), [br])
                sel = sb(pes, tagp + "sel", [16, 16, 128], BF16)
                k.dma("pool", sel.t[:], c_sel[:, :, :], (), [sel])
                if layer == 0 and "l1out" in phases and "l0in" not in phases:
                    precast(1)
                if layer == 1 and "l0out" not in phases:
                    precast(1)
                if layer == 0 and "l0in" not in phases:
                    precast(0)
                catb = [sb(pes, tagp + "cat%d" % i, [128, nkc, 128], BF16) for i in range(2)]
                xt = [sb(pes, tagp + "xt%d" % i, [128, D]) for i in range(2)]
                ssb = sb(pes, tagp + "ss", [128, 4])
                hb = sb(pes, tagp + "hb", [128, D], BF16)
                hf = sb(pes, tagp + "hf", [128, D])
                sq = hf
                hTf = sb(pes, tagp + "hTf", [128, 8, 128])
                hT2 = [sb(pes, tagp + "hT%d" % i, [128, 8, TT], BF16) for i in range(2)]
                yacc = sb(pes, tagp + "yacc", [128, NSUB, D])
                rt_ = sb(pes, tagp + "rt", [128, NSUB, 64])
                LG = sb(pes, tagp + "LG", [128, NSUB, 20])
                dg = sb(pes, tagp + "dg", [128, NSUB, 16], BF16)
                gT2 = [sb(pes, tagp + "gT%d" % i, [16, TT], BF16) for i in range(2)]
                gbc_e = [sb(pes, tagp + "gbe%d" % i, [128, TT], BF16) for i in range(2)]
                wgu = [sb(pes, tagp + "wgu%d" % i, [128, 2, 8, DEXP], BF16) for i in range(2)]
                wdn = [sb(pes, tagp + "wdn%d" % i, [128, 4, D], BF16) for i in range(2)]
                silu = [sb(pes, tagp + "silu%d" % i, [128, 512]) for i in range(2)]
                hid = [sb(pes, tagp + "hid%d" % i, [128, 4, TT], BF16) for i in range(2)]
                ptr = ps(pes, tagp + "ptr", [128, 1024], BF16)
                pg = [ps(pes, tagp + "pg%d" % i, [128, 512]) for i in range(2)]
                pu = [ps(pes, tagp + "pu%d" % i, [128, 512]) for i in range(2)]
                py = [ps(pes, tagp + "py%d" % i, [128, 512]) for i in range(3)]
                cnt_ = {"nf": 0, "ny": 0, "nx": 0}
                nx = 0
                ne = 0
                nf = 0
                ny = 0
                def make_front(tile):
                    t0 = tile * TT
                    hT = hT2[tile % 2]
                    gT = gT2[tile % 2]
                    cl = []
                    for j in range(NSUB):
                        def sub1(j=j):
                            r0 = t0 + j * 128
                            cb = catb[cnt_["nx"] % 2]
                            xb = xt[cnt_["nx"] % 2]
                            cnt_["nx"] += 1
                            k.dma("sp", cb.t[:], cat_src[:, r0:r0 + 128].rearrange("(c p) t -> p c t", p=128), [dbuf(cat_name, r0 // T)], [cb])
                            k.dma("pool", xb.t[:], x_src[r0:r0 + 128, :], [dbuf(x_name, r0 // 128)], [xb])
                            for half in range(2):
                                pp = py[half]
                                for kc in range(nkc):
                                    k.mm(pp.t[:], cb.t[:, kc, :], wo.t[:, kc, half * 512:(half + 1) * 512], kc == 0, kc == nkc - 1, [cb, wo], [pp])
                                k.tt(xb.t[:, half * 512:(half + 1) * 512], xb.t[:, half * 512:(half + 1) * 512], pp.t[:], ALU.add, [xb, pp], [xb])
                            k.dma("pool", x_mid[r0:r0 + 128, :], xb.t[:], [xb], [dbuf(x_mid_name, r0 // 128)])
                            k.tt(sq.t[:], xb.t[:], xb.t[:], ALU.mult, [xb], [sq])
                            k.op("dve", lambda g: g.reduce_sum(out=ssb.t[:, 0:1], in_=sq.t[:], axis=AX.X), [sq], [ssb])
                            k.rsqrt(ssb.t[:, 2:3], ssb.t[:, 0:1], 1.0 / D, epsc.t[:, 0:1], [ssb, epsc], [ssb])
                            k.stt(hf.t[:], xb.t[:], ssb.t[:, 2:3], gb.t[:], ALU.mult, ALU.mult, [xb, ssb, gb], [hf])
                            k.copy(hb.t[:], hf.t[:], [hf], [hb], e="act")
                        def sub2(j=j):
                            for kc in range(8):
                                k.tr(ptr.t[:, kc * 128:(kc + 1) * 128], hb.t[:, kc * 128:(kc + 1) * 128], identb.t[:], [hb, identb], [ptr])
                                pf_ = py[kc // 4]
                                k.tr(pf_.t[:, (kc % 4) * 128:(kc % 4 + 1) * 128], hf.t[:, kc * 128:(kc + 1) * 128], ident.t[:], [hf, ident], [pf_])
                            k.copy(hT.t[:, :, j * 128:(j + 1) * 128], ptr.t[:].rearrange("p (c t) -> p c t", t=128), [ptr], [hT], e="act")
                            for q_ in range(2):
                                k.copy(hTf.t[:, q_ * 4:(q_ + 1) * 4, :], py[q_].t[:].rearrange("p (c t) -> p c t", t=128), [py[q_]], [hTf], e="dve")
                        def sub3(j=j):
                            pr = pg[0]
                            for kc in range(8):
                                k.mm(pr.t[:, 0:20], hTf.t[:, kc, :], wr.t[:, kc, :], kc == 0, kc == 7, [hTf, wr], [pr])
                            k.tt(LG.t[:, j, :], pr.t[:, 0:20], br.t[:], ALU.add, [pr, br], [LG])
                        cl += [sub1, None, None, sub2, None, None, sub3, None, None]

                    def routing():
                        Rr = rt_.t
                        NS = NSUB

                        def col(a_, b_=None):
                            return Rr[:, :, a_:(a_ + 1 if b_ is None else b_)]

                        def bc(ap1, n_):
                            return ap1.to_broadcast([128, NS, n_])

                        gl = LG.t[:, :, 0:4]
                        k.op("dve", lambda g: g.reduce_max(out=col(44), in_=gl, axis=AX.X), [LG], [rt_])
                        k.tt(col(0, 4), gl, bc(col(44), 4), ALU.is_equal, [LG, rt_], [rt_])
                        k.tt(col(4, 8), gl, bc(col(44), 4), ALU.subtract, [LG, rt_], [rt_])
                        k.act(col(4, 8), col(4, 8), AF.Exp, [rt_], [rt_])
                        k.op("dve", lambda g: g.reduce_sum(out=col(45), in_=col(4, 8), axis=AX.X), [rt_], [rt_])
                        k.op("dve", lambda g: g.reciprocal(col(45), col(45)), [rt_], [rt_])
                        el4 = LG.t[:, :, 4:20].rearrange("p s (g e) -> p s g e", e=4)
                        t16 = Rr[:, :, 28:44].rearrange("p s (g e) -> p s g e", e=4)
                        k.tt(t16, el4, Rr[:, :, 0:4].unsqueeze(3).to_broadcast([128, NS, 4, 4]), ALU.mult, [LG, rt_], [rt_])
                        k.op("dve", lambda g: g.reduce_sum(out=col(8, 12), in_=Rr[:, :, 28:44].rearrange("p s (g e) -> p s e g", e=4), axis=AX.X), [rt_], [rt_])
                        k.op("dve", lambda g: g.reduce_max(out=col(46), in_=col(8, 12), axis=AX.X), [rt_], [rt_])
                        k.tt(col(12, 16), col(8, 12), bc(col(46), 4), ALU.is_equal, [rt_], [rt_])
                        k.stt(col(16, 20), col(12, 16), -1e30, col(8, 12), ALU.mult, ALU.add, [rt_], [rt_])
                        k.op("dve", lambda g: g.reduce_max(out=col(47), in_=col(16, 20), axis=AX.X), [rt_], [rt_])
                        k.tt(col(20, 24), col(16, 20), bc(col(47), 4), ALU.is_equal, [rt_], [rt_])
                        k.tt(col(48), col(47), col(46), ALU.subtract, [rt_], [rt_])
                        k.act(col(49), col(48), AF.Exp, [rt_], [rt_])
                        k.ts(col(50), col(49), 1.0, None, ALU.add, None, [rt_], [rt_])
                        k.op("dve", lambda g: g.reciprocal(col(50), col(50)), [rt_], [rt_])
                        k.tt(col(51), col(50), col(45), ALU.mult, [rt_], [rt_])
                        k.tt(col(52), col(51), col(49), ALU.mult, [rt_], [rt_])
                        k.tt(col(24, 28), col(12, 16), bc(col(51), 4), ALU.mult, [rt_], [rt_])
                        k.tt(col(4, 8), col(20, 24), bc(col(52), 4), ALU.mult, [rt_], [rt_])
                        k.tt(col(24, 28), col(24, 28), col(4, 8), ALU.add, [rt_], [rt_])
                        k.tt(dg.t[:].rearrange("p s (g e) -> p s g e", e=4), Rr[:, :, 0:4].unsqueeze(3).to_broadcast([128, NS, 4, 4]),
                             Rr[:, :, 24:28].unsqueeze(2).to_broadcast([128, NS, 4, 4]), ALU.mult, [rt_], [dg])
                        for j in range(NSUB):
                            ptb = ptr.t[:]
                            k.tr(ptb[0:16, j * 128:(j + 1) * 128], dg.t[:, j, :], identb.t[:], [dg, identb], [ptr])
                        k.copy(gT.t[:, :], ptr.t[0:16, 0:TT], [ptr], [gT], e="act")

                    cl.append(routing)
                    return cl

                ntiles = NTOK // TT
                AQ = "pool"

                def make_expert(tile, e, wg_, wd_, ge_, hid_, hT, gT):
                    def pro():
                        for hlf in range(NH):
                            pp = py[hlf % 2]
                            k.mm(pp.t[:], sel.t[:, e, :], gT.t[:, hlf * 512:(hlf + 1) * 512], True, True, [sel, gT], [pp])
                            k.copy(ge_.t[:, hlf * 512:(hlf + 1) * 512], pp.t[:], [pp], [ge_], e="act")
                    G = []
                    for fc in range(4):
                        for hlf in range(NH):
                            def g(fc=fc, hlf=hlf):
                                hs_ = slice(hlf * 512, (hlf + 1) * 512)
                                i_ = cnt_["nf"] % 2
                                cnt_["nf"] += 1
                                pgg, puu, sl_ = pg[i_], pu[i_], silu[i_]
                                for kc in range(8):
                                    k.mm(pgg.t[:], wg_.t[:, 0, kc, fc * 128:(fc + 1) * 128], hT.t[:, kc, hs_], kc == 0, kc == 7, [wg_, hT], [pgg])
                                for kc in range(8):
                                    k.mm(puu.t[:], wg_.t[:, 1, kc, fc * 128:(fc + 1) * 128], hT.t[:, kc, hs_], kc == 0, kc == 7, [wg_, hT], [puu])
                                k.act(sl_.t[:], pgg.t[:], AF.Silu, [pgg], [sl_])
                                k.tt(sl_.t[:], sl_.t[:], puu.t[:], ALU.mult, [sl_, puu], [sl_])
                                k.tt(hid_.t[:, fc, hs_], sl_.t[:], ge_.t[:, hs_], ALU.mult, [sl_, ge_], [hid_])
                            G.append(g)
                    Dn = []
                    for j in range(NSUB):
                        for half in range(2):
                            def d(j=j, half=half):
                                pp = py[cnt_["ny"] % 3]
                                cnt_["ny"] += 1
                                for fc in range(4):
                                    k.mm(pp.t[:], hid_.t[:, fc, j * 128:(j + 1) * 128], wd_.t[:, fc, half * 512:(half + 1) * 512], fc == 0, fc == 3, [hid_, wd_], [pp])
                                ya = yacc.t[:, j, half * 512:(half + 1) * 512]
                                if e == 0:
                                    k.copy(ya, pp.t[:], [pp], [yacc], e="dve")
                                else:
                                    k.tt(ya, ya, pp.t[:], ALU.add, [yacc, pp], [yacc])
                            Dn.append(d)
                    return pro, G, Dn

                def tail(tile):
                    for j in range(NSUB):
                        r0 = tile * TT + j * 128
                        xb = xt[cnt_["nx"] % 2]
                        cnt_["nx"] += 1
                        k.dma(AQ, xb.t[:], x_mid[r0:r0 + 128, :], [dbuf(x_mid_name, r0 // 128)], [xb])
                        k.tt(xb.t[:], xb.t[:], yacc.t[:, j, :], ALU.add, [xb, yacc], [xb], e="pool")
                        k.dma(AQ, x_dst[r0:r0 + 128, :], xb.t[:], [xb], [dbuf(x_dst_name, r0 // 128)])

                inst = [(tile, e) for tile in range(ntiles) for e in range(NEXP)]

                def load_wg(n):
                    e_ = inst[n][1]
                    k.dma("sp", wgu[n % 2].t[:], wgu_bf[layer, e_], [dbuf("wbf", (layer, e_))], [wgu[n % 2]])

                def load_wd(n):
                    e_ = inst[n][1]
                    k.dma("sp", wdn[n % 2].t[:], wdn_bf[layer, e_], [dbuf("wbf", (layer, e_))], [wdn[n % 2]])

                load_wg(0)
                load_wd(0)
                for f_ in make_front(0):
                    if f_ is not None:
                        f_()
                prevD = None
                pending = []
                for n, (tile, e) in enumerate(inst):
                    if e == 0:
                        pending = make_front(tile + 1) if tile + 1 < ntiles else []
                    if n + 1 < len(inst):
                        load_wg(n + 1)
                    pro, G, Dn = make_expert(tile, e, wgu[n % 2], wdn[n % 2], gbc_e[n % 2], hid[n % 2], hT2[tile % 2], gT2[tile % 2])
                    pro()

                    def inject():
                        if pending:
                            it = pending.pop(0)
                            if it is not None:
                                it()
                    if prevD is None:
                        for g in G:
                            g()
                            inject()
                    else:
                        per = (len(prevD) + len(G) - 1) // len(G)
                        di = 0
                        for g in G:
                            g()
                            for _ in range(per):
                                if di < len(prevD):
                                    prevD[di]()
                                    di += 1
                            inject()
                        while di < len(prevD):
                            prevD[di]()
                            di += 1
                    if n + 1 < len(inst):
                        load_wd(n + 1)
                    if e == 0 and tile > 0:
                        tail(tile - 1)
                    if e == NEXP - 1:
                        while pending:
                            it = pending.pop(0)
                            if it is not None:
                                it()
                    prevD = Dn
                for d in prevD:
                    d()
                tail(ntiles - 1)
            k.barrier()

        if "l0out" in phases:
            phase_out_moe(0, catT, "catT", 8, w_out_even, x_in, "x", 1, x1, "x1", x2, "x2", "m0_")

        L1_CHUNKS = [(i * 128, 128) for i in range(20)]

        def l1_dst(ci, c0, m, tt_, pp, st):
            if ci < 10:
                k.act(st.t[:], pp.t[:], AF.Gelu_apprx_tanh, [pp], [st])
                k.dma("sp", gateT[c0:c0 + 128, tt_ * 512:(tt_ + 1) * 512], st.t[:], [st], [dbuf("gateT", tt_ * 512 // T)])
            else:
                k.copy(st.t[:], pp.t[:], [pp], [st], e="dve")
                k.dma("sp", recT[c0 - LRU_W:c0 - LRU_W + 128, tt_ * 512:(tt_ + 1) * 512], st.t[:], [st], [dbuf("recT", tt_ * 512 // T)])

        if "l1in" in phases:
            phase_in(x2, "x2", 2, w_in_odd, 2 * LRU_W, L1_CHUNKS, l1_dst, "b_")

        def phase_lru():
            with ExitStack() as pes:
                wrg = sb(pes, "u_wr", [128, 10, 128], BF16)
                wig = sb(pes, "u_wi", [128, 10, 128], BF16)
                k.dma("pool", wrg.t[:], w_rgate.rearrange("g i j -> i g j"), (), [wrg])
                k.dma("pool", wig.t[:], w_igate.rearrange("g i j -> i g j"), (), [wig])
                lam = sb(pes, "u_lam", [128, 10])
                for g_ in range(10):
                    k.act(lam.t[:, g_:g_ + 1], pcol("lam_%d" % g_), AF.Softplus, [pc], [lam], scale=-1.0)
                k.ts(lam.t[:], lam.t[:], -8.0, None, ALU.mult, None, [lam], [lam])
                u = [sb(pes, "u_u%d" % i, [128, T + 3]) for i in range(2)]
                gt = [sb(pes, "u_gt%d" % i, [128, T]) for i in range(2)]
                cv = sb(pes, "u_cv", [128, T])
                cvb = sb(pes, "u_cvb", [128, T], BF16)
                rg = sb(pes, "u_rg", [128, T])
                ig = sb(pes, "u_ig", [128, T])
                aa = sb(pes, "u_aa", [128, T])
                bb = sb(pes, "u_bb", [128, T])
                hh_ = sb(pes, "u_hh", [128, T])
                ob = [sb(pes, "u_ob%d" % i, [128, T], BF16) for i in range(2)]
                pr = [ps(pes, "u_pr%d" % i, [128, 512]) for i in range(2)]
                pi = [ps(pes, "u_pi%d" % i, [128, 512]) for i in range(2)]
                n = 0
                for s in range(NSEQ):
                    ts0 = s * T
                    for g_ in range(10):
                        ub, gtb, obb = u[n % 2], gt[n % 2], ob[n % 2]
                        n += 1
                        k.memset(ub.t[:, 0:3], 0.0, [ub])
                        k.dma("sp", ub.t[:, 3:T + 3], recT[g_ * 128:(g_ + 1) * 128, ts0:ts0 + T], [dbuf("recT", s)], [ub])
                        k.dma("sp", gtb.t[:], gateT[g_ * 128:(g_ + 1) * 128, ts0:ts0 + T], [dbuf("gateT", s)], [gtb])
                        k.ts(cv.t[:], ub.t[:, 0:T], pcol("cw0_%d" % g_), pcol("cb_%d" % g_), ALU.mult, ALU.add, [ub, pc], [cv])
                        for tp in range(1, 4):
                            k.stt(cv.t[:], ub.t[:, tp:T + tp], pcol("cw%d_%d" % (tp, g_)), cv.t[:], ALU.mult, ALU.add, [ub, cv, pc], [cv])
                        k.copy(cvb.t[:], cv.t[:], [cv], [cvb], e="act")
                        for f in range(T // 512):
                            fs = slice(f * 512, (f + 1) * 512)
                            k.mm(pr[f % 2].t[:], wrg.t[:, g_, :], cvb.t[:, fs], True, True, [wrg, cvb], [pr[f % 2]])
                            k.act(rg.t[:, fs], pr[f % 2].t[:], AF.Sigmoid, [pr[f % 2], pc], [rg], bias=pcol("br_%d" % g_))
                            k.mm(pi[f % 2].t[:], wig.t[:, g_, :], cvb.t[:, fs], True, True, [wig, cvb], [pi[f % 2]])
                            k.act(ig.t[:, fs], pi[f % 2].t[:], AF.Sigmoid, [pi[f % 2], pc], [ig], bias=pcol("bi_%d" % g_))
                        k.act(aa.t[:], rg.t[:], AF.Exp, [rg, lam], [aa], scale=lam.t[:, g_:g_ + 1])
                        k.tt(bb.t[:], aa.t[:], aa.t[:], ALU.mult, [aa], [bb])
                        k.act(bb.t[:], bb.t[:], AF.Sqrt, [bb, epsc], [bb], scale=-1.0, bias=epsc.t[:, 2:3])
                        k.tt(ig.t[:], ig.t[:], cv.t[:], ALU.mult, [ig, cv], [ig])
                        k.tt(bb.t[:], bb.t[:], ig.t[:], ALU.mult, [bb, ig], [bb])
                        k.op("dve", lambda g, : g.tensor_tensor_scan(hh_.t[:], aa.t[:], bb.t[:], 0.0, ALU.mult, ALU.add), [aa, bb], [hh_])
                        k.tt(obb.t[:], hh_.t[:], gtb.t[:], ALU.mult, [hh_, gtb], [obb])
                        k.dma("sp", cat2T[g_ * 128:(g_ + 1) * 128, ts0:ts0 + T], obb.t[:], [obb], [dbuf("cat2T", s)])
            k.barrier()

        if "lru" in phases:
            phase_lru()

        if "l1out" in phases:
            phase_out_moe(1, cat2T, "cat2T", 10, w_out_odd, x2, "x2", 3, x3, "x3", out_d, "out", "m1_")

        if debug:
            with ExitStack() as pes:
                a = sb(pes, "dbg_a", [128, 512], BF16)
                b = sb(pes, "dbg_b", [128, 512])
                for (src, dst, rows) in ((catT, catT_dbg, D), (cat2T, cat2T_dbg, LRU_W)):
                    for r in range(rows // 128):
                        for c in range(NTOK // 512):
                            k.dma("sp", a.t[:], src[r * 128:(r + 1) * 128, c * 512:(c + 1) * 512], (), [a])
                            k.copy(b.t[:], a.t[:], [a], [b], e="dve")
                            k.dma("sp", dst[r * 128:(r + 1) * 128, c * 512:(c + 1) * 512], b.t[:], [b], [a])
        k.barrier(skip_pre=False)
        print("kernel build: %d instructions" % k.n_inst)
    return nc


def host_inputs(inp):
    cols, pcarr = _pcols(inp)
    c = _consts()
    sel = np.zeros((16, 16, 128), np.float32)
    for e in range(16):
        sel[e, e, :] = 1.0
    f = lambda a: np.ascontiguousarray(np.asarray(a, np.float32))
    shared = {
        "gbc": f(np.stack([np.broadcast_to(inp["norm_mix"][0], (128, D)), np.broadcast_to(inp["norm_ffn"][0], (128, D)),
                           np.broadcast_to(inp["norm_mix"][1], (128, D)), np.broadcast_to(inp["norm_ffn"][1], (128, D))])),
        "pc": f(pcarr),
        "w_in_even": f(inp["w_in_even"][0]),
        "lora_w": f(np.concatenate([inp["decay_up"][0], inp["iclr_up"][0]], axis=0)),
        "gate_up": f(inp["gate_up"][0]),
        "w_out_even": f(inp["w_out_even"][0]),
        "w_in_odd": f(inp["w_in_odd"][0]),
        "w_rgate": f(inp["w_rgate"][0]),
        "w_igate": f(inp["w_igate"][0]),
        "w_out_odd": f(inp["w_out_odd"][0]),
        "w_router": f(np.concatenate([inp["w_group"], inp["w_erouter"]], axis=2)),
        "b_router": f(np.broadcast_to(np.concatenate([inp["b_group"], inp["b_erouter"]], axis=1)[:, None, :], (2, 128, 20))),
        "exp_w_gate": f(inp["exp_w_gate"]),
        "exp_w_up": f(inp["exp_w_up"]),
        "exp_w_down": f(inp["exp_w_down"]),
        "ident": c["ident"], "blockones": c["blockones"], "ones": c["ones"], "triu": c["triu"],
        "maskrel": f(c["maskrel"]), "m64": f(c["m64"]), "sel": sel,
    }
    return cols, pcarr.shape[1], shared


def kernel(**inputs):
    inp = {k_: np.asarray(v) for k_, v in inputs.items()}
    x = inp["x"]
    B, T, _ = x.shape
    nseq = B // NCORES
    cols, npc, shared = host_inputs(inp)
    nc = build(nseq, T, cols, npc)
    in_maps = []
    for c in range(NCORES):
        m = dict(shared)
        m["x"] = np.ascontiguousarray(x[c * nseq:(c + 1) * nseq].reshape(nseq * T, D), dtype=np.float32)
        in_maps.append(m)
    res = run_bass_kernel_spmd(nc, in_maps, core_ids=list(range(NCORES)))
    out = np.concatenate([np.asarray(r["out"]).reshape(nseq, T, D) for r in res.results], axis=0)
    return out.astype(np.float32)
```

```python
import os
import numpy as np
from contextlib import ExitStack
import concourse.bass as bass
import concourse.mybir as mybir
from concourse.bass_utils import run_bass_kernel_spmd

F32 = mybir.dt.float32
BF16 = mybir.dt.bfloat16
AF = mybir.ActivationFunctionType
ALU = mybir.AluOpType
AX = mybir.AxisListType

D = 1024
NCORES = 8
A_COLS = 1824
EVEN_COLS = 3360
LRU_W = 1280
NEXP = 16
DEXP = 512
DECAY_C = 0.6065306597126334
RMS_EPS = 1e-6
GN_EPS = 64e-5
SEM_LIMIT = 30000


class Buf:
    __slots__ = ("t", "w", "r")

    def __init__(self, t):
        self.t = t
        self.w = None
        self.r = {}


class MBuf:
    __slots__ = ("t", "ch")

    def __init__(self, t, n=2):
        self.t = t
        self.ch = [Buf(t) for _ in range(n)]


def _flat(bs):
    out = []
    for b in bs:
        if isinstance(b, MBuf):
            out.extend(b.ch)
        else:
            out.append(b)
    return out


class K:
    def __init__(self, nc, es):
        self.nc = nc
        self.es = es
        self.eng = {"pe": nc.tensor, "act": nc.scalar, "dve": nc.vector, "pool": nc.gpsimd, "sp": nc.sync}
        self.sems = {}
        self.epoch = {e: 0 for e in self.eng}
        self.cnt = {}
        self.waited = {e: {} for e in self.eng}
        for e in self.eng:
            self._new_epoch(e, first=True)
        self.ndslots = {"sp": 6, "pool": 6, "act": 2, "pre": 4}
        self.qeng = {"sp": "sp", "pool": "pool", "act": "act", "pre": "pool"}
        self.dslot = {q: 0 for q in self.ndslots}
        for q, n in self.ndslots.items():
            for i in range(n):
                key = ("d", q, i)
                self.sems[key] = es.enter_context(nc.semaphore("d_%s_%d" % (q, i)))
                self.cnt[key] = 0
        self.n_inst = 0

    def _new_epoch(self, e, first=False):
        if not first:
            self.epoch[e] += 1
        key = (e, self.epoch[e])
        self.sems[key] = self.es.enter_context(self.nc.semaphore("c_%s_%d" % (e, self.epoch[e])))
        self.cnt[key] = 0

    def _wait(self, e, deps):
        for key, val in deps.items():
            if self.waited[e].get(key, 0) < val:
                self.eng[e].wait_ge(self.sems[key], val)
                self.waited[e][key] = val

    def _deps(self, e, reads, writes):
        deps = {}

        def add(kv):
            if kv is None:
                return
            key, val = kv
            if deps.get(key, 0) < val:
                deps[key] = val

        for b in reads:
            add(b.w)
        for b in writes:
            if not (e == "pe" and b.w is not None and b.w[0][0] == "pe" and not b.r):
                add(b.w)
            for key, val in b.r.items():
                add((key, val))
        return deps

    def op(self, e, fn, reads=(), writes=()):
        reads, writes = _flat(reads), _flat(writes)
        self._wait(e, self._deps(e, reads, writes))
        inst = fn(self.eng[e])
        key = (e, self.epoch[e])
        self.cnt[key] += 1
        val = self.cnt[key]
        inst.then_inc(self.sems[key], 1)
        self.n_inst += 1
        for b in reads:
            if b.r.get(key, 0) < val:
                b.r[key] = val
        for b in writes:
            b.w = (key, val)
            b.r = {}
        if val >= SEM_LIMIT:
            self._new_epoch(e)

    def dma(self, qc, out, in_, reads=(), writes=()):
        reads, writes = _flat(reads), _flat(writes)
        q = self.qeng[qc]
        self._wait(q, self._deps(q, reads, writes))
        i = self.dslot[qc]
        self.dslot[qc] = (i + 1) % self.ndslots[qc]
        key = ("d", qc, i)
        if self.cnt[key] > 0 and self.waited[q].get(key, 0) < self.cnt[key]:
            self.eng[q].wait_ge(self.sems[key], self.cnt[key])
            self.waited[q][key] = self.cnt[key]
        self.eng[q].dma_start(out=out, in_=in_).then_inc(self.sems[key], 16)
        self.cnt[key] += 16
        val = self.cnt[key]
        self.n_inst += 1
        for b in reads:
            if b.r.get(key, 0) < val:
                b.r[key] = val
        for b in writes:
            b.w = (key, val)
            b.r = {}

    def barrier(self, engines=None, skip_pre=True):
        engines = engines or list(self.eng)
        deps = {key: val for key, val in self.cnt.items() if val > 0 and not (skip_pre and key[0] == "d" and key[1] == "pre")}
        for e in engines:
            self._wait(e, deps)

    def mm(self, out, lhsT, rhs, start, stop, reads, writes):
        self.op("pe", lambda g: g.matmul(out, lhsT, rhs, start=start, stop=stop), reads, writes)

    def tr(self, out, in_, ident, reads, writes):
        self.op("pe", lambda g: g.transpose(out, in_, ident), reads, writes)

    def act(self, out, in_, func, reads, writes, bias=None, scale=None, e="act"):
        kw = {}
        if bias is not None:
            kw["bias"] = bias
        if scale is not None:
            kw["scale"] = scale
        self.op(e, lambda g: g.activation(out=out, in_=in_, func=func, **kw), reads, writes)

    def tt(self, out, in0, in1, op, reads, writes, e="dve"):
        self.op(e, lambda g: g.tensor_tensor(out=out, in0=in0, in1=in1, op=op), reads, writes)

    def ts(self, out, in0, s1, s2, op0, op1, reads, writes, e="dve"):
        if op1 is None:
            self.op(e, lambda g: g.tensor_scalar(out, in0, s1, None, op0), reads, writes)
        else:
            self.op(e, lambda g: g.tensor_scalar(out, in0, s1, s2, op0, op1), reads, writes)

    def stt(self, out, in0, scalar, in1, op0, op1, reads, writes, e="dve"):
        self.op(e, lambda g: g.scalar_tensor_tensor(out=out, in0=in0, scalar=scalar, in1=in1, op0=op0, op1=op1), reads, writes)

    def rsqrt(self, out, in_, scale, bias, reads, writes, floor=None):
        kw = {"scale": scale}
        if bias is not None:
            kw["bias"] = bias
        self.op("act", lambda g: g.activation(out=out, in_=in_, func=AF.Sqrt, **kw), reads, writes)
        if floor is not None:
            self.op("dve", lambda g: g.tensor_scalar(out, out, floor, None, ALU.max), writes, writes)
        self.op("dve", lambda g: g.reciprocal(out, out), writes, writes)

    def copy(self, out, in_, reads, writes, e="dve"):
        if e == "act":
            self.op(e, lambda g: g.activation(out=out, in_=in_, func=AF.Copy), reads, writes)
        else:
            self.op(e, lambda g: g.tensor_copy(out, in_), reads, writes)

    def memset(self, ap, val, writes, e="dve"):
        self.op(e, lambda g: g.memset(ap, val), (), writes)


IN_CHUNKS = [(i * 128, 128) for i in range(12)] + [(1536, 128), (1664, 128), (1792, 32)] + \
            [(A_COLS + i * 128, 128) for i in range(12)]


def _pcols(inp):
    cols = {}
    arrs = []

    def add(name, v):
        v = np.asarray(v, np.float32).reshape(-1)
        col = np.zeros(128, np.float32)
        col[: v.shape[0]] = v
        cols[name] = len(arrs)
        arrs.append(col)

    mu = inp["mu_a"][0]
    for ci, (c0, m) in enumerate(IN_CHUNKS[:15]):
        add("mu%d" % ci, mu[c0:c0 + m])
    for p in range(4):
        sl = slice(p * 128, (p + 1) * 128)
        add("w0_%d" % p, inp["w0"][0][sl])
        add("a0_%d" % p, inp["a0"][0][sl])
        add("kk_%d" % p, inp["k_k"][0][sl])
        add("ka_%d" % p, inp["k_a"][0][sl])
        add("rk_%d" % p, inp["r_k"][0].reshape(-1)[sl])
        add("gnw_%d" % p, inp["gn_w"][0][sl])
        add("gnb_%d" % p, inp["gn_b"][0][sl])
    add("qg", np.tile(inp["q_norm_g"][0], 2))
    add("kg", np.tile(inp["k_norm_g"][0], 2))
    for g in range(10):
        sl = slice(g * 128, (g + 1) * 128)
        for t in range(4):
            add("cw%d_%d" % (t, g), inp["conv_w"][0][t][sl])
        add("cb_%d" % g, inp["conv_b"][0][sl])
        add("br_%d" % g, inp["b_rgate"][0][sl])
        add("bi_%d" % g, inp["b_igate"][0][sl])
        add("lam_%d" % g, inp["lru_lambda"][0][sl])
    return cols, np.stack(arrs, axis=1).copy()


def _consts():
    j = np.arange(128)
    c = {}
    c["ident"] = np.eye(128, dtype=np.float32)
    bo = np.zeros((128, 128), np.float32)
    bo[:64, :64] = 1.0
    bo[64:, 64:] = 1.0
    c["blockones"] = bo
    c["ones"] = np.ones((128, 128), np.float32)
    c["triu"] = (j[:, None] >= j[None, :]).astype(np.float32)
    t = np.arange(512)
    c["maskrel"] = np.stack([((r * 128 + j[:, None]) < t[None, :]).astype(np.float32) for r in range(4)], axis=1)
    jj = np.arange(64)
    m = np.zeros((64, 4, 64), np.float32)
    m[:, 0] = (jj[:, None] < jj[None, :])
    m[:, 1] = (jj[:, None] <= jj[None, :])
    m[:, 2] = (jj[None, :] < jj[:, None])
    m[:, 3] = np.eye(64)
    c["m64"] = m
    return c


def build(NSEQ, T, pcol_idx, npc, phases=("l0in", "rwkv", "attn", "l0out", "l1in", "lru", "l1out"), debug=False):
    NTOK = NSEQ * T
    NT512 = NTOK // 512
    nc = bass.Bass("TRN2", target_bir_lowering=False)
    dbgkind = "ExternalOutput" if debug else "Internal"

    def din(name, shape, dt=F32):
        return nc.dram_tensor(name, list(shape), dt, kind="ExternalInput").ap()

    x_in = din("x", [NTOK, D])
    out_d = nc.dram_tensor("out", [NTOK, D], F32, kind="ExternalOutput").ap()
    gbc_d = din("gbc", [4, 128, D])
    pc_d = din("pc", [128, npc])
    w_in_even = din("w_in_even", [D, EVEN_COLS])
    lora_w_d = din("lora_w", [128, 512])
    gate_up_d = din("gate_up", [160, 512])
    w_out_even = din("w_out_even", [D, D])
    w_in_odd = din("w_in_odd", [D, 2 * LRU_W])
    w_rgate = din("w_rgate", [10, 128, 128])
    w_igate = din("w_igate", [10, 128, 128])
    w_out_odd = din("w_out_odd", [LRU_W, D])
    w_router = din("w_router", [2, D, 20])
    b_router = din("b_router", [2, 128, 20])
    exp_w_gate = din("exp_w_gate", [2, NEXP, D, DEXP])
    exp_w_up = din("exp_w_up", [2, NEXP, D, DEXP])
    exp_w_down = din("exp_w_down", [2, NEXP, DEXP, D])
    c_ident = din("ident", [128, 128])
    c_blockones = din("blockones", [128, 128])
    c_ones = din("ones", [128, 128])
    c_triu = din("triu", [128, 128])
    c_maskrel = din("maskrel", [128, 4, 512])
    c_m64 = din("m64", [64, 4, 64])
    c_sel = din("sel", [16, 16, 128])

    projT = nc.dram_tensor("projT", [EVEN_COLS, NTOK], F32, kind=dbgkind).ap()
    catT = nc.dram_tensor("catT", [D, NTOK], BF16, kind="Internal").ap()
    x1 = nc.dram_tensor("x1", [NTOK, D], F32, kind=dbgkind).ap()
    x2 = nc.dram_tensor("x2", [NTOK, D], F32, kind=dbgkind).ap()
    x3 = nc.dram_tensor("x3", [NTOK, D], F32, kind=dbgkind).ap()
    gateT = nc.dram_tensor("gateT", [LRU_W, NTOK], F32, kind="Internal").ap()
    recT = nc.dram_tensor("recT", [LRU_W, NTOK], F32, kind="Internal").ap()
    cat2T = nc.dram_tensor("cat2T", [LRU_W, NTOK], BF16, kind="Internal").ap()
    if debug:
        catT_dbg = nc.dram_tensor("catT_dbg", [D, NTOK], F32, kind="ExternalOutput").ap()
        cat2T_dbg = nc.dram_tensor("cat2T_dbg", [LRU_W, NTOK], F32, kind="ExternalOutput").ap()

    wgu_bf = nc.dram_tensor("wgu_bf", [2, NEXP, 128, 2, 8, DEXP], BF16, kind="Internal").ap()
    wdn_bf = nc.dram_tensor("wdn_bf", [2, NEXP, 128, 4, D], BF16, kind="Internal").ap()
    PC = pcol_idx
    dbufs = {}
    dump_list = []

    def dump(k, name, buf, ap, shape):
        if not debug:
            return
        d = nc.dram_tensor("dump_" + name, list(shape), F32, kind="ExternalOutput").ap()
        k.dma("sp", d, ap, [buf], [])

    def dbuf(name, idx):
        b = dbufs.get((name, idx))
        if b is None:
            b = Buf(None)
            dbufs[(name, idx)] = b
        return b

    with ExitStack() as es:
        k = K(nc, es)

        def sb(es_, name, shape, dt=F32):
            return Buf(es_.enter_context(nc.sbuf_tensor("sb_" + name, list(shape), dt)))

        def ps(es_, name, shape, dt=F32):
            return Buf(es_.enter_context(nc.psum_tensor("ps_" + name, list(shape), dt)))

        pc = sb(es, "pc", [128, npc])
        ident = sb(es, "ident", [128, 128])
        identb = sb(es, "identb", [128, 128], BF16)
        blockones = sb(es, "blockones", [128, 128])
        k.dma("sp", pc.t[:], pc_d[:, :], (), [pc])
        k.dma("sp", ident.t[:], c_ident[:, :], (), [ident])
        k.dma("pool", identb.t[:], c_ident[:, :], (), [identb])
        k.dma("sp", blockones.t[:], c_blockones[:, :], (), [blockones])

        epsc = sb(es, "epsc", [128, 4])
        k.memset(epsc.t[:, 0:1], RMS_EPS, [epsc])
        k.memset(epsc.t[:, 1:2], GN_EPS, [epsc])
        k.memset(epsc.t[:, 2:3], 1.0, [epsc])

        def pcol(name, rows=128):
            i = PC[name]
            return pc.t[0:rows, i:i + 1]

        def rmsnorm_T(xt, gb, hT, j, sq, ssb, hb, ptr):
            k.tt(sq.t[:], xt.t[:], xt.t[:], ALU.mult, [xt], [sq])
            k.op("dve", lambda g: g.reduce_sum(out=ssb.t[:, 0:1], in_=sq.t[:], axis=AX.X), [sq], [ssb])
            k.rsqrt(ssb.t[:, 2:3], ssb.t[:, 0:1], 1.0 / D, epsc.t[:, 0:1], [ssb, epsc], [ssb])
            k.stt(hb.t[:], xt.t[:], ssb.t[:, 2:3], gb.t[:], ALU.mult, ALU.mult, [xt, ssb, gb], [hb])
            for kc in range(8):
                k.tr(ptr.t[:, kc * 128:(kc + 1) * 128], hb.t[:, kc * 128:(kc + 1) * 128], identb.t[:], [hb, identb], [ptr])
            k.copy(hT.t[:, :, j * 128:(j + 1) * 128], ptr.t[:].rearrange("p (c t) -> p c t", t=128), [ptr], [hT], e="act")

        def precast(layer):
            for e in range(NEXP):
                wb = dbuf("wbf", (layer, e))
                k.dma("pre", wgu_bf[layer, e, :, 0, :, :], exp_w_gate[layer, e].rearrange("(c p) f -> p c f", p=128), (), [wb])
                k.dma("pre", wgu_bf[layer, e, :, 1, :, :], exp_w_up[layer, e].rearrange("(c p) f -> p c f", p=128), (), [wb])
                k.dma("pre", wdn_bf[layer, e, :, :, :], exp_w_down[layer, e].rearrange("(c p) d -> p c d", p=128), (), [wb])

        def phase_in(x_src, x_name, gidx, w_d, ncols, chunks, dst_fn, tagp, after_loads=None):
            with ExitStack() as pes:
                wsb = sb(pes, tagp + "w", [128, 8, ncols], BF16)
                gb = sb(pes, tagp + "gb", [128, D])
                k.dma("sp", gb.t[:], gbc_d[gidx], (), [gb])
                for kc in range(8):
                    k.dma("pool", wsb.t[:, kc, :], w_d[kc * 128:(kc + 1) * 128, :], (), [wsb])
                if after_loads is not None:
                    after_loads()
                xts = [sb(pes, tagp + "xt%d" % i, [128, D]) for i in range(2)]
                sq = sb(pes, tagp + "sq", [128, D])
                ssb = sb(pes, tagp + "ss", [128, 4])
                hb = sb(pes, tagp + "hb", [128, D], BF16)
                hTs = [sb(pes, tagp + "hT%d" % i, [128, 8, 512], BF16) for i in range(2)]
                ptr = ps(pes, tagp + "ptr", [128, 1024], BF16)
                pps = [ps(pes, tagp + "pp%d" % i, [128, 512]) for i in range(3)]
                stg = [sb(pes, tagp + "stg%d" % i, [128, 512]) for i in range(3)]
                n = 0
                for tt_ in range(NT512):
                    hT = hTs[tt_ % 2]
                    for j in range(4):
                        xt = xts[(tt_ * 4 + j) % 2]
                        r0 = tt_ * 512 + j * 128
                        k.dma("sp", xt.t[:], x_src[r0:r0 + 128, :], [dbuf(x_name, r0 // 128)], [xt])
                        rmsnorm_T(xt, gb, hT, j, sq, ssb, hb, ptr)
                    for ci, (c0, m) in enumerate(chunks):
                        pp = pps[n % 3]
                        st = stg[n % 3]
                        n += 1
                        for kc in range(8):
                            k.mm(pp.t[0:m, :], wsb.t[:, kc, c0:c0 + m], hT.t[:, kc, :], kc == 0, kc == 7, [wsb, hT], [pp])
                        dst_fn(ci, c0, m, tt_, pp, st)
            k.barrier()

        def l0_dst(ci, c0, m, tt_, pp, st):
            k.copy(st.t[0:m, :], pp.t[0:m, :], [pp], [st], e=("act" if ci % 2 else "dve"))
            k.dma("sp", projT[c0:c0 + m, tt_ * 512:(tt_ + 1) * 512], st.t[0:m, :], [st], [dbuf("projT", tt_ * 512 // T)])

        if "l0in" in phases:
            phase_in(x_in, "x", 0, w_in_even, EVEN_COLS, IN_CHUNKS, l0_dst, "a_", after_loads=(lambda: (precast(0), precast(1) if "l1out" in phases else None)) if "l0out" in phases else None)

        def phase_rwkv():
            with ExitStack() as pes:
                SEG = 256
                lw_t = sb(pes, "r_lw", [128, 512])
                gu1 = sb(pes, "r_gu1", [128, 512])
                gu2 = sb(pes, "r_gu2", [32, 512])
                m64 = sb(pes, "r_m64", [64, 4, 64])
                onesf = sb(pes, "r_ones", [128, 64])
                k.dma("sp", lw_t.t[:], lora_w_d[:, :], (), [lw_t])
                k.dma("sp", gu1.t[:], gate_up_d[0:128, :], (), [gu1])
                k.dma("sp", gu2.t[:], gate_up_d[128:160, :], (), [gu2])
                k.dma("sp", m64.t[:], c_m64[:, :, :], (), [m64])
                k.memset(onesf.t[:], 1.0, [onesf])
                raw = {nm: sb(pes, "r_raw_" + nm, [128, 4, SEG + 1]) for nm in ("r", "k", "v")}
                rawl = sb(pes, "r_rawl", [128, 3, SEG + 1])
                sh = {nm: sb(pes, "r_sh_" + nm, [128, 4, SEG]) for nm in ("r", "k", "v")}
                shl = sb(pes, "r_shl", [128, 3, SEG])
                tmpA = sb(pes, "r_tmpA", [128, 4, SEG])
                tmpB = sb(pes, "r_tmpB", [128, 4, SEG])
                sg = sb(pes, "r_sg", [128, 4, SEG])
                aic = sb(pes, "r_aic", [128, 4, SEG])
                gg = sb(pes, "r_g", [128, 4, SEG])
                kk = sb(pes, "r_kk", [128, 4, SEG])
                kmod = sb(pes, "r_kmod", [128, 4, SEG])
                bonus = sb(pes, "r_bonus", [128, 4, SEG])
                cs = sb(pes, "r_cs", [128, 4, SEG])
                Ep = sb(pes, "r_Ep", [128, 4, SEG])
                En = sb(pes, "r_En", [128, 4, SEG])
                Em = sb(pes, "r_Em", [128, 4, SEG])
                MD = BF16
                rt = sb(pes, "r_rt", [128, 4, SEG], MD)
                kt = sb(pes, "r_kt", [128, 4, SEG], MD)
                bt = sb(pes, "r_bt", [128, 4, SEG], MD)
                at = sb(pes, "r_at", [128, 4, SEG], MD)
                vb16 = sb(pes, "r_vb16", [128, 4, SEG], MD)
                PCp = sb(pes, "r_PCp", [128, 4, SEG // 64])
                PCH = sb(pes, "r_PCH", [64, 8, SEG // 64])
                yseg = sb(pes, "r_y", [128, 4, SEG])
                yab = sb(pes, "r_yab", [128, 4, SEG], BF16)
                def sbm(name, shape, dt=F32):
                    return MBuf(pes.enter_context(nc.sbuf_tensor("sb_" + name, list(shape), dt)))

                def psm(name, shape, dt=F32):
                    return MBuf(pes.enter_context(nc.psum_tensor("ps_" + name, list(shape), dt)))

                S = sbm("r_S", [64, 8, 64])
                Stmp = sbm("r_Stmp", [64, 8, 64])
                Sb = sbm("r_Sb", [64, 8, 64], MD)
                X = [sbm("r_X%d" % i, [64, 8, 64], MD) for i in range(2)]
                Y = [sbm("r_Y%d" % i, [64, 8, 64], MD) for i in range(2)]
                Z = [sbm("r_Z%d" % i, [64, 8, 64], MD) for i in range(2)]
                LakT = sbm("r_LakT", [64, 8, 64], MD)
                MrbT = sbm("r_MrbT", [64, 8, 64], MD)
                MrkT = sbm("r_MrkT", [64, 8, 64], MD)
                Vt = sbm("r_Vt", [64, 8, 64], MD)
                Btk = sbm("r_Btk", [64, 8, 64], MD)
                Ktk = sbm("r_Ktk", [64, 8, 64], MD)
                Wsb = sbm("r_Wsb", [64, 8, 64], MD)
                Usb = sbm("r_Usb", [64, 8, 64], MD)
                atH = sb(pes, "r_atH", [64, 8, SEG], MD)
                btH = sb(pes, "r_btH", [64, 8, SEG], MD)
                ktH = sb(pes, "r_ktH", [64, 8, SEG], MD)
                rtH = sb(pes, "r_rtH", [64, 8, SEG], MD)
                yH = sbm("r_yH", [64, 8, SEG])
                QG = [[ps(pes, "r_Q%d%d" % (g_, i), [128, 512]) for i in range(4)] for g_ in range(2)]
                P1, P2, P3, P4 = QG[0]

                def v864(b):
                    return b.t[0:64, :].rearrange("p (h j) -> p h j", j=64)

                def v8128(b):
                    return b.t[0:64, :].rearrange("p (h j) -> p h j", j=128)

                def v4128(b, rows=128):
                    return b.t[0:rows, 0:512].rearrange("p (h j) -> p h j", j=128)

                mU = m64.t[:, 0:1, :].to_broadcast([64, 4, 64])
                mUI = m64.t[:, 1:2, :].to_broadcast([64, 4, 64])
                mL = m64.t[:, 2:3, :].to_broadcast([64, 4, 64])
                mI = m64.t[:, 3:4, :].to_broadcast([64, 4, 64])

                def loads_shift(s, seg):
                    tok0 = s * T + seg * SEG
                    dep = [dbuf("projT", s)]
                    def load(dst_ap, dstbuf, r0, m):
                        if seg == 0:
                            k.memset(dst_ap[0:m, 0:1], 0.0, [dstbuf])
                            k.dma("sp", dst_ap[0:m, 1:SEG + 1], projT[r0:r0 + m, tok0:tok0 + SEG], dep, [dstbuf])
                        else:
                            k.dma("sp", dst_ap[0:m, 0:SEG + 1], projT[r0:r0 + m, tok0 - 1:tok0 + SEG], dep, [dstbuf])
                    for i, nm in enumerate(("r", "k", "v")):
                        for p in range(4):
                            load(raw[nm].t[:, p, :], raw[nm], i * 512 + p * 128, 128)
                    load(rawl.t[:, 0, :], rawl, 1536, 128)
                    load(rawl.t[:, 1, :], rawl, 1664, 128)
                    load(rawl.t[:, 2, :], rawl, 1792, 32)
                    def shift(dst, dbuf_, src, sbuf_, m, mucol):
                        k.tt(tmpA.t[0:m, 0, :], src[0:m, 0:SEG], src[0:m, 1:SEG + 1], ALU.subtract, [sbuf_], [tmpA])
                        k.stt(dst[0:m, :], tmpA.t[0:m, 0, :], pcol(mucol, m), src[0:m, 1:SEG + 1], ALU.mult, ALU.add, [tmpA, sbuf_, pc], [dbuf_])
                    for i, nm in enumerate(("r", "k", "v")):
                        for p in range(4):
                            shift(sh[nm].t[:, p, :], sh[nm], raw[nm].t[:, p, :], raw[nm], 128, "mu%d" % (i * 4 + p))
                    shift(shl.t[:, 0, :], shl, rawl.t[:, 0, :], rawl, 128, "mu12")
                    shift(shl.t[:, 1, :], shl, rawl.t[:, 1, :], rawl, 128, "mu13")
                    shift(shl.t[:, 2, :], shl, rawl.t[:, 2, :], rawl, 32, "mu14")

                seglist = [(s_, g_) for s_ in range(NSEQ) for g_ in range(T // SEG)]
                loads_shift(*seglist[0])
                for s in range(NSEQ):
                    k.memset(S.t[:], 0.0, [S])
                    k.memset(Sb.t[:], 0.0, [Sb])
                    for seg in range(T // SEG):
                        tok0 = s * T + seg * SEG
                        seg_idx = s * (T // SEG) + seg
                        STOP = int(os.environ.get("RWKV_STOP", "99"))
                        if STOP <= 0:
                            continue
                        k.act(shl.t[0:64, 0, :], shl.t[0:64, 0, :], AF.Tanh, [shl], [shl])
                        k.act(shl.t[:, 1, :], shl.t[:, 1, :], AF.Sigmoid, [shl], [shl])
                        k.act(shl.t[0:32, 2, :], shl.t[0:32, 2, :], AF.Sigmoid, [shl], [shl])
                        for p in range(4):
                            pch = slice(p * 128, (p + 1) * 128)
                            k.mm(P1.t[:, 0:SEG], lw_t.t[0:64, pch], shl.t[0:64, 0, :], True, True, [lw_t, shl], [P1])
                            k.act(sg.t[:, p, :], P1.t[:, 0:SEG], AF.Sigmoid, [P1, pc], [sg], bias=pcol("w0_%d" % p))
                            k.mm(P2.t[:, 0:SEG], lw_t.t[64:128, pch], shl.t[64:128, 0, :], True, True, [lw_t, shl], [P2])
                            k.act(aic.t[:, p, :], P2.t[:, 0:SEG], AF.Sigmoid, [P2, pc], [aic], bias=pcol("a0_%d" % p))
                            k.mm(P3.t[:, 0:SEG], gu1.t[:, pch], shl.t[:, 1, :], True, False, [gu1, shl], [P3])
                            k.mm(P3.t[:, 0:SEG], gu2.t[0:32, pch], shl.t[0:32, 2, :], False, True, [gu2, shl], [P3])
                            k.copy(gg.t[:, p, :], P3.t[:, 0:SEG], [P3], [gg], e="act")
                            k.ts(tmpA.t[:, p, :], sh["k"].t[:, p, :], pcol("kk_%d" % p), None, ALU.mult, None, [sh["k"], pc], [tmpA])
                            k.tt(tmpB.t[:, p, :], tmpA.t[:, p, :], tmpA.t[:, p, :], ALU.mult, [tmpA], [tmpB])
                            k.mm(P4.t[:, 0:SEG], blockones.t[:], tmpB.t[:, p, :], True, True, [blockones, tmpB], [P4])
                            k.rsqrt(tmpB.t[:, p, :], P4.t[:, 0:SEG], 1.0, None, [P4], [tmpB], floor=1e-12)
                            k.tt(kk.t[:, p, :], tmpA.t[:, p, :], tmpB.t[:, p, :], ALU.mult, [tmpA, tmpB], [kk])
                            k.ts(tmpA.t[:, p, :], aic.t[:, p, :], -1.0, pcol("ka_%d" % p), ALU.add, ALU.mult, [aic, pc], [tmpA])
                            k.stt(kmod.t[:, p, :], tmpA.t[:, p, :], 1.0, sh["k"].t[:, p, :], ALU.add, ALU.mult, [tmpA, sh["k"]], [kmod])
                            k.stt(tmpB.t[:, p, :], sh["r"].t[:, p, :], pcol("rk_%d" % p), kmod.t[:, p, :], ALU.mult, ALU.mult, [sh["r"], kmod, pc], [tmpB])
                            k.mm(P1.t[:, 0:SEG], blockones.t[:], tmpB.t[:, p, :], True, True, [blockones, tmpB], [P1])
                            k.tt(bonus.t[:, p, :], P1.t[:, 0:SEG], sh["v"].t[:, p, :], ALU.mult, [P1, sh["v"]], [bonus])
                            for c in range(SEG // 64):
                                ch = slice(c * 64, (c + 1) * 64)
                                k.op("dve", lambda g, p=p, ch=ch: g.tensor_tensor_scan(cs.t[:, p, ch], onesf.t[:, :], sg.t[:, p, ch], 0.0, ALU.mult, ALU.add), [sg, onesf], [cs])
                        if STOP <= 1:
                            continue
                        k.tt(tmpA.t[:], cs.t[:], sg.t[:], ALU.subtract, [cs, sg], [tmpA])
                        k.act(Ep.t[:], cs.t[:], AF.Exp, [cs], [Ep], scale=-DECAY_C)
                        k.act(En.t[:], cs.t[:], AF.Exp, [cs], [En], scale=DECAY_C)
                        k.act(Em.t[:], tmpA.t[:], AF.Exp, [tmpA], [Em], scale=-DECAY_C)
                        k.tt(rt.t[:], sh["r"].t[:], Ep.t[:], ALU.mult, [sh["r"], Ep], [rt])
                        k.tt(kt.t[:], kmod.t[:], En.t[:], ALU.mult, [kmod, En], [kt])
                        k.tt(tmpB.t[:], kk.t[:], aic.t[:], ALU.mult, [kk, aic], [tmpB])
                        k.tt(bt.t[:], tmpB.t[:], En.t[:], ALU.mult, [tmpB, En], [bt])
                        k.stt(at.t[:], kk.t[:], -1.0, Em.t[:], ALU.mult, ALU.mult, [kk, Em], [at])
                        k.copy(PCp.t[:], Ep.t[:].rearrange("p a (c j) -> p a c j", j=64)[:, :, :, 63], [Ep], [PCp], e="dve")
                        k.copy(vb16.t[:], sh["v"].t[:], [sh["v"]], [vb16], e="act")
                        if s == 0 and seg == 0:
                            for nm_, b_ in (("shr", sh["r"]), ("shk", sh["k"]), ("shv", sh["v"]), ("sg", sg), ("aic", aic), ("gg", gg), ("kk", kk),
                                            ("kmod", kmod), ("bonus", bonus), ("cs", cs), ("Ep", Ep), ("En", En), ("Em", Em),
                                            ):
                                dump(k, nm_, b_, b_.t[:], [128, 4, SEG])
                        if STOP <= 2:
                            continue
                        if seg_idx + 1 < len(seglist):
                            loads_shift(*seglist[seg_idx + 1])
                        def to_head(dstb, srcb):
                            dv = dstb.t[:].rearrange("q (p b) t -> q p b t", b=2)
                            k.dma("sp", dv[:, :, 0, :], srcb.t[0:64, :, :], [srcb], [dstb])
                            k.dma("sp", dv[:, :, 1, :], srcb.t[64:128, :, :], [srcb], [dstb])
                        to_head(atH, at)
                        to_head(btH, bt)
                        to_head(ktH, kt)
                        to_head(rtH, rt)
                        to_head(PCH, PCp)
                        identm = identb if MD == BF16 else ident

                        def chunk_gen(c, g):
                            ch = slice(c * 64, (c + 1) * 64)
                            H = slice(4 * g, 4 * g + 4)
                            hl = list(range(4 * g, 4 * g + 4))
                            bmap = {1: 0, 2: 1, 3: 2, 5: 0, 6: 1, 7: 2, 8: 3}
                            pb = {i: QG[g][bi] for i, bi in bmap.items()}
                            pv = {i: pb[i].t[0:64, 0:256].rearrange("q (h j) -> q h j", j=64) for i in bmap}

                            def T_(mb):
                                return mb.t[:, H, :]
                            Xg = [X[0].ch[g], X[1].ch[g]]
                            Yg = [Y[0].ch[g], Y[1].ch[g]]
                            Zg = [Z[0].ch[g], Z[1].ch[g]]
                            for j_, h in enumerate(hl):
                                k.mm(pv[1][:, j_, :], btH.t[:, h, ch], atH.t[:, h, ch], True, True, [btH, atH], [pb[1]])
                                k.mm(pv[2][:, j_, :], atH.t[:, h, ch], btH.t[:, h, ch], True, True, [btH, atH], [pb[2]])
                                k.mm(pv[3][:, j_, :], ktH.t[:, h, ch], atH.t[:, h, ch], True, True, [ktH, atH], [pb[3]])
                            k.tt(T_(X[0]), pv[1], mU, ALU.mult, [pb[1], m64], [Xg[0]])
                            k.tt(T_(Y[0]), pv[2], mL, ALU.mult, [pb[2], m64], [Yg[0]])
                            k.tt(T_(LakT), pv[3], mU, ALU.mult, [pb[3], m64], [LakT.ch[g]])
                            k.tt(T_(Z[0]), T_(X[0]), mI, ALU.add, [Xg[0], m64], [Zg[0]])
                            yield
                            for j_, h in enumerate(hl):
                                k.mm(pv[1][:, j_, :], Y[0].t[:, h, :], X[0].t[:, h, :], True, True, [Xg[0], Yg[0]], [pb[1]])
                                k.mm(pv[2][:, j_, :], X[0].t[:, h, :], Y[0].t[:, h, :], True, True, [Xg[0], Yg[0]], [pb[2]])
                            k.copy(T_(X[1]), pv[1], [pb[1]], [Xg[1]], e="act")
                            k.copy(T_(Y[1]), pv[2], [pb[2]], [Yg[1]], e="dve")
                            yield
                            xi, zi = 1, 0
                            for r in range(1, 6):
                                for j_, h in enumerate(hl):
                                    k.mm(pv[3][:, j_, :], Y[xi].t[:, h, :], Z[zi].t[:, h, :], True, True, [Yg[xi], Zg[zi]], [pb[3]])
                                    if r < 5:
                                        k.mm(pv[2][:, j_, :], X[xi].t[:, h, :], Y[xi].t[:, h, :], True, True, [Xg[xi], Yg[xi]], [pb[2]])
                                    if r < 4:
                                        k.mm(pv[1][:, j_, :], Y[xi].t[:, h, :], X[xi].t[:, h, :], True, True, [Xg[xi], Yg[xi]], [pb[1]])
                                k.tt(T_(Z[1 - zi]), pv[3], T_(Z[zi]), ALU.add, [pb[3], Zg[zi]], [Zg[1 - zi]])
                                if r < 5:
                                    k.copy(T_(Y[1 - xi]), pv[2], [pb[2]], [Yg[1 - xi]], e="act")
                                if r < 4:
                                    k.copy(T_(X[1 - xi]), pv[1], [pb[1]], [Xg[1 - xi]], e="dve")
                                xi, zi = 1 - xi, 1 - zi
                                yield
                            Zf, Zfb = Z[zi], Zg[zi]
                            for j_, h in enumerate(hl):
                                k.mm(pv[1][:, j_, :], btH.t[:, h, ch], rtH.t[:, h, ch], True, True, [btH, rtH], [pb[1]])
                                k.mm(pv[2][:, j_, :], ktH.t[:, h, ch], rtH.t[:, h, ch], True, True, [ktH, rtH], [pb[2]])
                            k.tt(T_(MrbT), pv[1], mUI, ALU.mult, [pb[1], m64], [MrbT.ch[g]])
                            k.tt(T_(MrkT), pv[2], mUI, ALU.mult, [pb[2], m64], [MrkT.ch[g]])
                            yield
                            tvs = [P.t[:].bitcast(BF16) if MD == BF16 else P.t[:] for P in (pb[1], pb[2], pb[3])]
                            for pp in range(2):
                                p = 2 * g + pp
                                cs_ = slice(pp * 128, (pp + 1) * 128)
                                k.tr(tvs[0][0:64, cs_], vb16.t[:, p, ch], identm.t[:], [vb16, identm], [pb[1]])
                                k.tr(tvs[1][0:64, cs_], bt.t[:, p, ch], identm.t[:], [bt, identm], [pb[2]])
                                k.tr(tvs[2][0:64, cs_], kt.t[:, p, ch], identm.t[:], [kt, identm], [pb[3]])
                            gv = [tv[0:64, 0:256].rearrange("q (h j) -> q h j", j=64) for tv in tvs]
                            k.copy(T_(Vt), gv[0], [pb[1]], [Vt.ch[g]], e="act")
                            k.copy(T_(Btk), gv[1], [pb[2]], [Btk.ch[g]], e="dve")
                            k.copy(T_(Ktk), gv[2], [pb[3]], [Ktk.ch[g]], e="act")
                            yield
                            for j_, h in enumerate(hl):
                                k.mm(pv[5][:, j_, :], atH.t[:, h, ch], Sb.t[:, h, :], True, False, [atH, Sb.ch[g]], [pb[5]])
                                k.mm(pv[5][:, j_, :], LakT.t[:, h, :], Vt.t[:, h, :], False, True, [LakT.ch[g], Vt.ch[g]], [pb[5]])
                            k.copy(T_(Wsb), pv[5], [pb[5]], [Wsb.ch[g]], e="act")
                            yield
                            for j_, h in enumerate(hl):
                                k.mm(pv[6][:, j_, :], Zf.t[:, h, :], Wsb.t[:, h, :], True, True, [Zfb, Wsb.ch[g]], [pb[6]])
                            k.copy(T_(Usb), pv[6], [pb[6]], [Usb.ch[g]], e="dve")
                            yield
                            for j_, h in enumerate(hl):
                                k.mm(pv[7][:, j_, :], Sb.t[:, h, :], rtH.t[:, h, ch], True, False, [Sb.ch[g], rtH], [pb[7]])
                                k.mm(pv[7][:, j_, :], Usb.t[:, h, :], MrbT.t[:, h, :], False, False, [Usb.ch[g], MrbT.ch[g]], [pb[7]])
                                k.mm(pv[7][:, j_, :], Vt.t[:, h, :], MrkT.t[:, h, :], False, True, [Vt.ch[g], MrkT.ch[g]], [pb[7]])
                            for j_, h in enumerate(hl):
                                k.mm(pv[8][:, j_, :], Btk.t[:, h, :], Usb.t[:, h, :], True, False, [Btk.ch[g], Usb.ch[g]], [pb[8]])
                                k.mm(pv[8][:, j_, :], Ktk.t[:, h, :], Vt.t[:, h, :], False, True, [Ktk.ch[g], Vt.ch[g]], [pb[8]])
                            k.copy(yH.t[:, H, ch], pv[7], [pb[7]], [yH.ch[g]], e="act")
                            pcc = PCH.t[:, H, c:c + 1].to_broadcast([64, 4, 64])
                            k.tt(T_(Stmp), T_(S), pv[8], ALU.add, [S.ch[g], pb[8]], [Stmp.ch[g]])
                            k.tt(T_(S), T_(Stmp), pcc, ALU.mult, [Stmp.ch[g], PCH], [S.ch[g]])
                            k.copy(T_(Sb), T_(S), [S.ch[g]], [Sb.ch[g]], e="act")
                            yield

                        for c in range(SEG // 64):
                            gens = [chunk_gen(c, 0), chunk_gen(c, 1)]
                            alive = [True, True]
                            while any(alive):
                                for gi in range(2):
                                    if alive[gi]:
                                        try:
                                            next(gens[gi])
                                        except StopIteration:
                                            alive[gi] = False
                        yv = yH.t[:].rearrange("q (p b) t -> q p b t", b=2)
                        k.dma("sp", yseg.t[0:64, :, :], yv[:, :, 0, :], [yH], [yseg])
                        k.dma("sp", yseg.t[64:128, :, :], yv[:, :, 1, :], [yH], [yseg])
                        if s == 0 and seg == 0:
                            dump(k, "yseg", yseg, yseg.t[:], [128, 4, SEG])
                        for p in range(4):
                            k.mm(P1.t[:, 0:SEG], blockones.t[:], yseg.t[:, p, :], True, True, [blockones, yseg], [P1])
                            k.stt(tmpA.t[:, p, :], P1.t[:, 0:SEG], -1.0 / 64, yseg.t[:, p, :], ALU.mult, ALU.add, [P1, yseg], [tmpA])
                            k.tt(tmpB.t[:, p, :], tmpA.t[:, p, :], tmpA.t[:, p, :], ALU.mult, [tmpA], [tmpB])
                            k.mm(P2.t[:, 0:SEG], blockones.t[:], tmpB.t[:, p, :], True, True, [blockones, tmpB], [P2])
                            k.rsqrt(tmpB.t[:, p, :], P2.t[:, 0:SEG], 1.0 / 64, epsc.t[:, 1:2], [P2, epsc], [tmpB])
                            k.tt(tmpA.t[:, p, :], tmpA.t[:, p, :], tmpB.t[:, p, :], ALU.mult, [tmpA, tmpB], [tmpA])
                            k.ts(tmpA.t[:, p, :], tmpA.t[:, p, :], pcol("gnw_%d" % p), pcol("gnb_%d" % p), ALU.mult, ALU.add, [tmpA, pc], [tmpA])
                            k.tt(tmpA.t[:, p, :], tmpA.t[:, p, :], bonus.t[:, p, :], ALU.add, [tmpA, bonus], [tmpA])
                            k.tt(yab.t[:, p, :], tmpA.t[:, p, :], gg.t[:, p, :], ALU.mult, [tmpA, gg], [yab])
                            k.dma("sp", catT[p * 128:(p + 1) * 128, tok0:tok0 + SEG], yab.t[:, p, :], [yab], [dbuf("catT", s)])
            k.barrier()

        if "rwkv" in phases:
            phase_rwkv()

        def phase_attn():
            with ExitStack() as pes:
                triu = sb(pes, "s_triu", [128, 128], BF16)
                onesb = sb(pes, "s_ones", [128, 128], BF16)
                maskb = sb(pes, "s_maskb", [128, 4, 512], BF16)
                k.dma("pool", triu.t[:], c_triu[:, :], (), [triu])
                k.dma("pool", onesb.t[:], c_ones[:, :], (), [onesb])
                k.dma("pool", maskb.t[:], c_maskrel[:, :, :], (), [maskb])
                qraw = sb(pes, "s_qraw", [128, T])
                kraw = sb(pes, "s_kraw", [128, T])
                vb = sb(pes, "s_vb", [128, T], BF16)
                sq = sb(pes, "s_sq", [128, T])
                rs = sb(pes, "s_rs", [128, T])
                qn2 = [sb(pes, "s_qn%d" % i, [128, T], BF16) for i in range(2)]
                kn2 = [sb(pes, "s_kn%d" % i, [128, T], BF16) for i in range(2)]
                vtok2 = [sb(pes, "s_vtok%d" % i, [128, T // 128, 128], BF16) for i in range(2)]
                ND = 6
                spb = [sb(pes, "s_sp%d" % i, [128, 512], BF16) for i in range(ND)]
                zsb = [sb(pes, "s_zs%d" % i, [128, 512]) for i in range(ND)]
                t1 = [sb(pes, "s_t1%d" % i, [128, 512]) for i in range(ND)]
                att = [sb(pes, "s_att%d" % i, [128, 512], BF16) for i in range(ND)]
                ob = [sb(pes, "s_ob%d" % i, [64, 512], BF16) for i in range(2)]
                PZ = [ps(pes, "s_PZ%d" % i, [128, 512]) for i in range(2)]
                PT = [ps(pes, "s_PT%d" % i, [128, 512]) for i in range(2)]
                PR = [ps(pes, "s_PR%d" % i, [128, 512]) for i in range(2)]
                PV = [ps(pes, "s_PV%d" % i, [128, 512]) for i in range(2)]
                Rb = sb(pes, "s_R", [128, 512])
                qnn2 = [sb(pes, "s_qnn%d" % i, [128, T], BF16) for i in range(2)]

                def prep(s, p, par):
                    dep = [dbuf("projT", s)]
                    ts0 = s * T
                    qn, kn, vtok = qn2[par], kn2[par], vtok2[par]
                    k.dma("sp", qraw.t[:], projT[A_COLS + p * 128:A_COLS + (p + 1) * 128, ts0:ts0 + T], dep, [qraw])
                    k.dma("sp", kraw.t[:], projT[A_COLS + 512 + p * 128:A_COLS + 512 + (p + 1) * 128, ts0:ts0 + T], dep, [kraw])
                    k.dma("pool", vb.t[:], projT[A_COLS + 1024 + p * 128:A_COLS + 1024 + (p + 1) * 128, ts0:ts0 + T], dep, [vb])
                    for (rawb, outb, gname) in ((qraw, qn, "qg"), (kraw, kn, "kg")):
                        k.tt(sq.t[:], rawb.t[:], rawb.t[:], ALU.mult, [rawb], [sq])
                        for f in range(T // 512):
                            fs = slice(f * 512, (f + 1) * 512)
                            pz = PZ[f % 2]
                            k.mm(pz.t[:], blockones.t[:], sq.t[:, fs], True, True, [blockones, sq], [pz])
                            k.rsqrt(rs.t[:, fs], pz.t[:], 1.0 / 64, epsc.t[:, 0:1], [pz, epsc], [rs])
                        k.stt(outb.t[:], rawb.t[:], pcol(gname), rs.t[:], ALU.mult, ALU.mult, [rawb, rs, pc], [outb])
                    k.ts(qnn2[par].t[:], qn.t[:], -0.125, None, ALU.mult, None, [qn], [qnn2[par]])
                    for kb in range(T // 128):
                        pz = PT[kb % 2]
                        pzb = pz.t[:].bitcast(BF16)
                        k.tr(pzb[:, 0:128], vb.t[:, kb * 128:(kb + 1) * 128], identb.t[:], [vb, identb], [pz])
                        k.copy(vtok.t[:, kb, :], pzb[:, 0:128], [pz], [vtok], e="act")

                pairs = [(s, p) for s in range(NSEQ) for p in range(4)]
                steps = []
                for pi, (s, p) in enumerate(pairs):
                    for hh in range(2):
                        for QT in range(T // 512):
                            kb_hi = QT * 4 + 3
                            for kb in range(kb_hi, -1, -1):
                                steps.append((pi, s, p, hh, QT, kb, kb_hi))
                qtidx = {}
                for st in steps:
                    key = (st[0], st[3], st[4])
                    if key not in qtidx:
                        qtidx[key] = len(qtidx)

                def stepA(n):
                    pi, s, p, hh, QT, kb, kb_hi = steps[n]
                    R = slice(hh * 64, hh * 64 + 64)
                    qs = slice(QT * 512, (QT + 1) * 512)
                    qn, kn = qn2[pi % 2], kn2[pi % 2]
                    pz, sp_, zs_ = PZ[n % 2], spb[n % ND], zsb[n % ND]
                    rel = kb - QT * 4
                    k.mm(pz.t[:], kn.t[R, kb * 128:(kb + 1) * 128], qn.t[R, qs], True, True, [kn, qn], [pz])
                    k.act(zs_.t[:], pz.t[:], AF.Exp, [pz], [zs_], scale=0.125)
                    k.act(sp_.t[:], zs_.t[:], AF.Ln, [zs_, epsc], [sp_], bias=epsc.t[:, 2:3])
                    if rel >= 0:
                        k.tt(sp_.t[:], sp_.t[:], maskb.t[:, rel, :], ALU.mult, [sp_, maskb], [sp_])

                def stepB(n):
                    pi, s, p, hh, QT, kb, kb_hi = steps[n]
                    R = slice(hh * 64, hh * 64 + 64)
                    qs = slice(QT * 512, (QT + 1) * 512)
                    kn, qnn = kn2[pi % 2], qnn2[pi % 2]
                    pt, po = PT[n % 2], PR[n % 2]
                    sp_, t1_, at_ = spb[n % ND], t1[n % ND], att[n % ND]
                    first = kb == kb_hi
                    k.mm(pt.t[:], kn.t[R, kb * 128:(kb + 1) * 128], qnn.t[R, qs], True, False, [kn, qnn], [pt])
                    k.mm(pt.t[:], triu.t[:], sp_.t[:], False, True, [triu, sp_], [pt])
                    if kb > 0:
                        k.mm(po.t[:], onesb.t[:], sp_.t[:], True, True, [onesb, sp_], [po])
                    if first:
                        k.act(at_.t[:], pt.t[:], AF.Exp, [pt], [at_], scale=-1.0)
                    else:
                        k.tt(t1_.t[:], pt.t[:], Rb.t[:], ALU.add, [pt, Rb], [t1_])
                        k.act(at_.t[:], t1_.t[:], AF.Exp, [t1_], [at_], scale=-1.0)
                    if kb > 0:
                        if first:
                            k.copy(Rb.t[:], po.t[:], [po], [Rb], e="dve")
                        else:
                            k.tt(Rb.t[:], Rb.t[:], po.t[:], ALU.add, [Rb, po], [Rb])

                def stepC(n):
                    pi, s, p, hh, QT, kb, kb_hi = steps[n]
                    R = slice(hh * 64, hh * 64 + 64)
                    h = 2 * p + hh
                    q = qtidx[(pi, hh, QT)] % 2
                    vtok = vtok2[pi % 2]
                    at_ = att[n % ND]
                    pv, obb = PV[q], ob[q]
                    rel = kb - QT * 4
                    if rel >= 0:
                        k.tt(at_.t[:], at_.t[:], maskb.t[:, rel, :], ALU.mult, [at_, maskb], [at_])
                    k.mm(pv.t[0:64, :], vtok.t[:, kb, R], at_.t[:], kb == kb_hi, kb == 0, [vtok, at_], [pv])
                    if kb == 0:
                        ts0 = s * T
                        k.copy(obb.t[:], pv.t[0:64, :], [pv], [obb], e="act")
                        k.dma("sp", catT[512 + h * 64:512 + (h + 1) * 64, ts0 + QT * 512:ts0 + (QT + 1) * 512], obb.t[:], [obb], [dbuf("catT", s)])

                NS_ = len(steps)
                per_pair = NS_ // len(pairs)
                prep(pairs[0][0], pairs[0][1], 0)
                DA, DB = 4, 2
                for n in range(-DA, NS_):
                    if n >= 0 and n % per_pair == 8:
                        pi = n // per_pair
                        if pi + 1 < len(pairs):
                            prep(pairs[pi + 1][0], pairs[pi + 1][1], (pi + 1) % 2)
                    if n + DA < NS_:
                        stepA(n + DA)
                    if 0 <= n + DB < NS_:
                        stepB(n + DB)
                    if n >= 0:
                        stepC(n)
            k.barrier()

        if "attn" in phases:
            phase_attn()

        def phase_out_moe(layer, cat_src, cat_name, nkc, w_out_d, x_src, x_name, gidx, x_mid, x_mid_name, x_dst, x_dst_name, tagp):
            TT = min(1024, NTOK)
            NSUB = TT // 128
            NH = TT // 512
            with ExitStack() as pes:
                wo = sb(pes, tagp + "wo", [128, nkc, D], BF16)
                for kc in range(nkc):
                    k.dma("pool", wo.t[:, kc, :], w_out_d[kc * 128:(kc + 1) * 128, :], (), [wo])
                gb = sb(pes, tagp + "gb", [128, D])
                k.dma("sp", gb.t[:], gbc_d[gidx], (), [gb])
                wr = sb(pes, tagp + "wr", [128, 8, 20])
                k.dma("sp", wr.t[:], w_router[layer].rearrange("(c p) n -> p c n", p=128), (), [wr])
                br = sb(pes, tagp + "br", [128, 20])
                k.dma("sp", br.t[:], b_router[layer], (), [br])
                sel = sb(pes, tagp + "sel", [16, 16, 128], BF16)
                k.dma("pool", sel.t[:], c_sel[:, :, :], (), [sel])
                if layer == 0 and "l1out" in phases and "l0in" not in phases:
                    precast(1)
                if layer == 1 and "l0out" not in phases:
                    precast(1)
                if layer == 0 and "l0in" not in phases:
                    precast(0)
                catb = [sb(pes, tagp + "cat%d" % i, [128, nkc, 128], BF16) for i in range(2)]
                xt = [sb(pes, tagp + "xt%d" % i, [128, D]) for i in range(2)]
                ssb = sb(pes, tagp + "ss", [128, 4])
                hb = sb(pes, tagp + "hb", [128, D], BF16)
                hf = sb(pes, tagp + "hf", [128, D])
                sq = hf
                hTf = sb(pes, tagp + "hTf", [128, 8, 128])
                hT2 = [sb(pes, tagp + "hT%d" % i, [128, 8, TT], BF16) for i in range(2)]
                yacc = sb(pes, tagp + "yacc", [128, NSUB, D])
                rt_ = sb(pes, tagp + "rt", [128, NSUB, 64])
                LG = sb(pes, tagp + "LG", [128, NSUB, 20])
                dg = sb(pes, tagp + "dg", [128, NSUB, 16], BF16)
                gT2 = [sb(pes, tagp + "gT%d" % i, [16, TT], BF16) for i in range(2)]
                gbc_e = [sb(pes, tagp + "gbe%d" % i, [128, TT], BF16) for i in range(2)]
                wgu = [sb(pes, tagp + "wgu%d" % i, [128, 2, 8, DEXP], BF16) for i in range(2)]
                wdn = [sb(pes, tagp + "wdn%d" % i, [128, 4, D], BF16) for i in range(2)]
                silu = [sb(pes, tagp + "silu%d" % i, [128, 512]) for i in range(2)]
                hid = [sb(pes, tagp + "hid%d" % i, [128, 4, TT], BF16) for i in range(2)]
                ptr = ps(pes, tagp + "ptr", [128, 1024], BF16)
                pg = [ps(pes, tagp + "pg%d" % i, [128, 512]) for i in range(2)]
                pu = [ps(pes, tagp + "pu%d" % i, [128, 512]) for i in range(2)]
                py = [ps(pes, tagp + "py%d" % i, [128, 512]) for i in range(3)]
                cnt_ = {"nf": 0, "ny": 0, "nx": 0}
                nx = 0
                ne = 0
                nf = 0
                ny = 0
                def make_front(tile):
                    t0 = tile * TT
                    hT = hT2[tile % 2]
                    gT = gT2[tile % 2]
                    cl = []
                    for j in range(NSUB):
                        def sub1(j=j):
                            r0 = t0 + j * 128
                            cb = catb[cnt_["nx"] % 2]
                            xb = xt[cnt_["nx"] % 2]
                            cnt_["nx"] += 1
                            k.dma("sp", cb.t[:], cat_src[:, r0:r0 + 128].rearrange("(c p) t -> p c t", p=128), [dbuf(cat_name, r0 // T)], [cb])
                            k.dma("pool", xb.t[:], x_src[r0:r0 + 128, :], [dbuf(x_name, r0 // 128)], [xb])
                            for half in range(2):
                                pp = py[half]
                                for kc in range(nkc):
                                    k.mm(pp.t[:], cb.t[:, kc, :], wo.t[:, kc, half * 512:(half + 1) * 512], kc == 0, kc == nkc - 1, [cb, wo], [pp])
                                k.tt(xb.t[:, half * 512:(half + 1) * 512], xb.t[:, half * 512:(half + 1) * 512], pp.t[:], ALU.add, [xb, pp], [xb])
                            k.dma("pool", x_mid[r0:r0 + 128, :], xb.t[:], [xb], [dbuf(x_mid_name, r0 // 128)])
                            k.tt(sq.t[:], xb.t[:], xb.t[:], ALU.mult, [xb], [sq])
                            k.op("dve", lambda g: g.reduce_sum(out=ssb.t[:, 0:1], in_=sq.t[:], axis=AX.X), [sq], [ssb])
                            k.rsqrt(ssb.t[:, 2:3], ssb.t[:, 0:1], 1.0 / D, epsc.t[:, 0:1], [ssb, epsc], [ssb])
                            k.stt(hf.t[:], xb.t[:], ssb.t[:, 2:3], gb.t[:], ALU.mult, ALU.mult, [xb, ssb, gb], [hf])
                            k.copy(hb.t[:], hf.t[:], [hf], [hb], e="act")
                        def sub2(j=j):
                            for kc in range(8):
                                k.tr(ptr.t[:, kc * 128:(kc + 1) * 128], hb.t[:, kc * 128:(kc + 1) * 128], identb.t[:], [hb, identb], [ptr])
                                pf_ = py[kc // 4]
                                k.tr(pf_.t[:, (kc % 4) * 128:(kc % 4 + 1) * 128], hf.t[:, kc * 128:(kc + 1) * 128], ident.t[:], [hf, ident], [pf_])
                            k.copy(hT.t[:, :, j * 128:(j + 1) * 128], ptr.t[:].rearrange("p (c t) -> p c t", t=128), [ptr], [hT], e="act")
                            for q_ in range(2):
                                k.copy(hTf.t[:, q_ * 4:(q_ + 1) * 4, :], py[q_].t[:].rearrange("p (c t) -> p c t", t=128), [py[q_]], [hTf], e="dve")
                        def sub3(j=j):
                            pr = pg[0]
                            for kc in range(8):
                                k.mm(pr.t[:, 0:20], hTf.t[:, kc, :], wr.t[:, kc, :], kc == 0, kc == 7, [hTf, wr], [pr])
                            k.tt(LG.t[:, j, :], pr.t[:, 0:20], br.t[:], ALU.add, [pr, br], [LG])
                        cl += [sub1, None, None, sub2, None, None, sub3, None, None]

                    def routing():
                        Rr = rt_.t
                        NS = NSUB

                        def col(a_, b_=None):
                            return Rr[:, :, a_:(a_ + 1 if b_ is None else b_)]

                        def bc(ap1, n_):
                            return ap1.to_broadcast([128, NS, n_])

                        gl = LG.t[:, :, 0:4]
                        k.op("dve", lambda g: g.reduce_max(out=col(44), in_=gl, axis=AX.X), [LG], [rt_])
                        k.tt(col(0, 4), gl, bc(col(44), 4), ALU.is_equal, [LG, rt_], [rt_])
                        k.tt(col(4, 8), gl, bc(col(44), 4), ALU.subtract, [LG, rt_], [rt_])
                        k.act(col(4, 8), col(4, 8), AF.Exp, [rt_], [rt_])
                        k.op("dve", lambda g: g.reduce_sum(out=col(45), in_=col(4, 8), axis=AX.X), [rt_], [rt_])
                        k.op("dve", lambda g: g.reciprocal(col(45), col(45)), [rt_], [rt_])
                        el4 = LG.t[:, :, 4:20].rearrange("p s (g e) -> p s g e", e=4)
                        t16 = Rr[:, :, 28:44].rearrange("p s (g e) -> p s g e", e=4)
                        k.tt(t16, el4, Rr[:, :, 0:4].unsqueeze(3).to_broadcast([128, NS, 4, 4]), ALU.mult, [LG, rt_], [rt_])
                        k.op("dve", lambda g: g.reduce_sum(out=col(8, 12), in_=Rr[:, :, 28:44].rearrange("p s (g e) -> p s e g", e=4), axis=AX.X), [rt_], [rt_])
                        k.op("dve", lambda g: g.reduce_max(out=col(46), in_=col(8, 12), axis=AX.X), [rt_], [rt_])
                        k.tt(col(12, 16), col(8, 12), bc(col(46), 4), ALU.is_equal, [rt_], [rt_])
                        k.stt(col(16, 20), col(12, 16), -1e30, col(8, 12), ALU.mult, ALU.add, [rt_], [rt_])
                        k.op("dve", lambda g: g.reduce_max(out=col(47), in_=col(16, 20), axis=AX.X), [rt_], [rt_])
                        k.tt(col(20, 24), col(16, 20), bc(col(47), 4), ALU.is_equal, [rt_], [rt_])
                        k.tt(col(48), col(47), col(46), ALU.subtract, [rt_], [rt_])
                        k.act(col(49), col(48), AF.Exp, [rt_], [rt_])
                        k.ts(col(50), col(49), 1.0, None, ALU.add, None, [rt_], [rt_])
                        k.op("dve", lambda g: g.reciprocal(col(50), col(50)), [rt_], [rt_])
                        k.tt(col(51), col(50), col(45), ALU.mult, [rt_], [rt_])
                        k.tt(col(52), col(51), col(49), ALU.mult, [rt_], [rt_])
                        k.tt(col(24, 28), col(12, 16), bc(col(51), 4), ALU.mult, [rt_], [rt_])
                        k.tt(col(4, 8), col(20, 24), bc(col(52), 4), ALU.mult, [rt_], [rt_])
                        k.tt(col(24, 28), col(24, 28), col(4, 8), ALU.add, [rt_], [rt_])
                        k.tt(dg.t[:].rearrange("p s (g e) -> p s g e", e=4), Rr[:, :, 0:4].unsqueeze(3).to_broadcast([128, NS, 4, 4]),
                             Rr[:, :, 24:28].unsqueeze(2).to_broadcast([128, NS, 4, 4]), ALU.mult, [rt_], [dg])
                        for j in range(NSUB):
                            ptb = ptr.t[:]
                            k.tr(ptb[0:16, j * 128:(j + 1) * 128], dg.t[:, j, :], identb.t[:], [dg, identb], [ptr])
                        k.copy(gT.t[:, :], ptr.t[0:16, 0:TT], [ptr], [gT], e="act")

                    cl.append(routing)
                    return cl

                ntiles = NTOK // TT
                AQ = "pool"

                def make_expert(tile, e, wg_, wd_, ge_, hid_, hT, gT):
                    def pro():
                        for hlf in range(NH):
                            pp = py[hlf % 2]
                            k.mm(pp.t[:], sel.t[:, e, :], gT.t[:, hlf * 512:(hlf + 1) * 512], True, True, [sel, gT], [pp])
                            k.copy(ge_.t[:, hlf * 512:(hlf + 1) * 512], pp.t[:], [pp], [ge_], e="act")
                    G = []
                    for fc in range(4):
                        for hlf in range(NH):
                            def g(fc=fc, hlf=hlf):
                                hs_ = slice(hlf * 512, (hlf + 1) * 512)
                                i_ = cnt_["nf"] % 2
                                cnt_["nf"] += 1
                                pgg, puu, sl_ = pg[i_], pu[i_], silu[i_]
                                for kc in range(8):
                                    k.mm(pgg.t[:], wg_.t[:, 0, kc, fc * 128:(fc + 1) * 128], hT.t[:, kc, hs_], kc == 0, kc == 7, [wg_, hT], [pgg])
                                for kc in range(8):
                                    k.mm(puu.t[:], wg_.t[:, 1, kc, fc * 128:(fc + 1) * 128], hT.t[:, kc, hs_], kc == 0, kc == 7, [wg_, hT], [puu])
                                k.act(sl_.t[:], pgg.t[:], AF.Silu, [pgg], [sl_])
                                k.tt(sl_.t[:], sl_.t[:], puu.t[:], ALU.mult, [sl_, puu], [sl_])
                                k.tt(hid_.t[:, fc, hs_], sl_.t[:], ge_.t[:, hs_], ALU.mult, [sl_, ge_], [hid_])
                            G.append(g)
                    Dn = []
                    for j in range(NSUB):
                        for half in range(2):
                            def d(j=j, half=half):
                                pp = py[cnt_["ny"] % 3]
                                cnt_["ny"] += 1
                                for fc in range(4):
                                    k.mm(pp.t[:], hid_.t[:, fc, j * 128:(j + 1) * 128], wd_.t[:, fc, half * 512:(half + 1) * 512], fc == 0, fc == 3, [hid_, wd_], [pp])
                                ya = yacc.t[:, j, half * 512:(half + 1) * 512]
                                if e == 0:
                                    k.copy(ya, pp.t[:], [pp], [yacc], e="dve")
                                else:
                                    k.tt(ya, ya, pp.t[:], ALU.add, [yacc, pp], [yacc])
                            Dn.append(d)
                    return pro, G, Dn

                def tail(tile):
                    for j in range(NSUB):
                        r0 = tile * TT + j * 128
                        xb = xt[cnt_["nx"] % 2]
                        cnt_["nx"] += 1
                        k.dma(AQ, xb.t[:], x_mid[r0:r0 + 128, :], [dbuf(x_mid_name, r0 // 128)], [xb])
                        k.tt(xb.t[:], xb.t[:], yacc.t[:, j, :], ALU.add, [xb, yacc], [xb], e="pool")
                        k.dma(AQ, x_dst[r0:r0 + 128, :], xb.t[:], [xb], [dbuf(x_dst_name, r0 // 128)])

                inst = [(tile, e) for tile in range(ntiles) for e in range(NEXP)]

                def load_wg(n):
                    e_ = inst[n][1]
                    k.dma("sp", wgu[n % 2].t[:], wgu_bf[layer, e_], [dbuf("wbf", (layer, e_))], [wgu[n % 2]])

                def load_wd(n):
                    e_ = inst[n][1]
                    k.dma("sp", wdn[n % 2].t[:], wdn_bf[layer, e_], [dbuf("wbf", (layer, e_))], [wdn[n % 2]])

                load_wg(0)
                load_wd(0)
                for f_ in make_front(0):
                    if f_ is not None:
                        f_()
                prevD = None
                pending = []
                for n, (tile, e) in enumerate(inst):
                    if e == 0:
                        pending = make_front(tile + 1) if tile + 1 < ntiles else []
                    if n + 1 < len(inst):
                        load_wg(n + 1)
                    pro, G, Dn = make_expert(tile, e, wgu[n % 2], wdn[n % 2], gbc_e[n % 2], hid[n % 2], hT2[tile % 2], gT2[tile % 2])
                    pro()

                    def inject():
                        if pending:
                            it = pending.pop(0)
                            if it is not None:
                                it()
                    if prevD is None:
                        for g in G:
                            g()
                            inject()
                    else:
                        per = (len(prevD) + len(G) - 1) // len(G)
                        di = 0
                        for g in G:
                            g()
                            for _ in range(per):
                                if di < len(prevD):
                                    prevD[di]()
                                    di += 1
                            inject()
                        while di < len(prevD):
                            prevD[di]()
                            di += 1
                    if n + 1 < len(inst):
                        load_wd(n + 1)
                    if e == 0 and tile > 0:
                        tail(tile - 1)
                    if e == NEXP - 1:
                        while pending:
                            it = pending.pop(0)
                            if it is not None:
                                it()
                    prevD = Dn
                for d in prevD:
                    d()
                tail(ntiles - 1)
            k.barrier()

        if "l0out" in phases:
            phase_out_moe(0, catT, "catT", 8, w_out_even, x_in, "x", 1, x1, "x1", x2, "x2", "m0_")

        L1_CHUNKS = [(i * 128, 128) for i in range(20)]

        def l1_dst(ci, c0, m, tt_, pp, st):
            if ci < 10:
                k.act(st.t[:], pp.t[:], AF.Gelu_apprx_tanh, [pp], [st])
                k.dma("sp", gateT[c0:c0 + 128, tt_ * 512:(tt_ + 1) * 512], st.t[:], [st], [dbuf("gateT", tt_ * 512 // T)])
            else:
                k.copy(st.t[:], pp.t[:], [pp], [st], e="dve")
                k.dma("sp", recT[c0 - LRU_W:c0 - LRU_W + 128, tt_ * 512:(tt_ + 1) * 512], st.t[:], [st], [dbuf("recT", tt_ * 512 // T)])

        if "l1in" in phases:
            phase_in(x2, "x2", 2, w_in_odd, 2 * LRU_W, L1_CHUNKS, l1_dst, "b_")

        def phase_lru():
            with ExitStack() as pes:
                wrg = sb(pes, "u_wr", [128, 10, 128], BF16)
                wig = sb(pes, "u_wi", [128, 10, 128], BF16)
                k.dma("pool", wrg.t[:], w_rgate.rearrange("g i j -> i g j"), (), [wrg])
                k.dma("pool", wig.t[:], w_igate.rearrange("g i j -> i g j"), (), [wig])
                lam = sb(pes, "u_lam", [128, 10])
                for g_ in range(10):
                    k.act(lam.t[:, g_:g_ + 1], pcol("lam_%d" % g_), AF.Softplus, [pc], [lam], scale=-1.0)
                k.ts(lam.t[:], lam.t[:], -8.0, None, ALU.mult, None, [lam], [lam])
                u = [sb(pes, "u_u%d" % i, [128, T + 3]) for i in range(2)]
                gt = [sb(pes, "u_gt%d" % i, [128, T]) for i in range(2)]
                cv2 = [sb(pes, "u_cv%d" % i, [128, T]) for i in range(2)]
                cvb2 = [sb(pes, "u_cvb%d" % i, [128, T], BF16) for i in range(2)]
                rg2 = [sb(pes, "u_rg%d" % i, [128, T]) for i in range(2)]
                ig2 = [sb(pes, "u_ig%d" % i, [128, T]) for i in range(2)]
                aa = sb(pes, "u_aa", [128, T])
                bb = sb(pes, "u_bb", [128, T])
                hh_ = sb(pes, "u_hh", [128, T])
                ob = [sb(pes, "u_ob%d" % i, [128, T], BF16) for i in range(2)]
                pr = [ps(pes, "u_pr%d" % i, [128, 512]) for i in range(2)]
                pi = [ps(pes, "u_pi%d" % i, [128, 512]) for i in range(2)]
                items = [(s, g_) for s in range(NSEQ) for g_ in range(10)]

                def stageA(n):
                    s, g_ = items[n]
                    ts0 = s * T
                    ub, gtb = u[n % 2], gt[n % 2]
                    cv, cvb, rg, ig = cv2[n % 2], cvb2[n % 2], rg2[n % 2], ig2[n % 2]
                    k.memset(ub.t[:, 0:3], 0.0, [ub])
                    k.dma("sp", ub.t[:, 3:T + 3], recT[g_ * 128:(g_ + 1) * 128, ts0:ts0 + T], [dbuf("recT", s)], [ub])
                    k.dma("sp", gtb.t[:], gateT[g_ * 128:(g_ + 1) * 128, ts0:ts0 + T], [dbuf("gateT", s)], [gtb])
                    k.ts(cv.t[:], ub.t[:, 0:T], pcol("cw0_%d" % g_), pcol("cb_%d" % g_), ALU.mult, ALU.add, [ub, pc], [cv])
                    for tp in range(1, 4):
                        k.stt(cv.t[:], ub.t[:, tp:T + tp], pcol("cw%d_%d" % (tp, g_)), cv.t[:], ALU.mult, ALU.add, [ub, cv, pc], [cv])
                    k.copy(cvb.t[:], cv.t[:], [cv], [cvb], e="act")
                    for f in range(T // 512):
                        fs = slice(f * 512, (f + 1) * 512)
                        k.mm(pr[f % 2].t[:], wrg.t[:, g_, :], cvb.t[:, fs], True, True, [wrg, cvb], [pr[f % 2]])
                        k.act(rg.t[:, fs], pr[f % 2].t[:], AF.Sigmoid, [pr[f % 2], pc], [rg], bias=pcol("br_%d" % g_))
                        k.mm(pi[f % 2].t[:], wig.t[:, g_, :], cvb.t[:, fs], True, True, [wig, cvb], [pi[f % 2]])
                        k.act(ig.t[:, fs], pi[f % 2].t[:], AF.Sigmoid, [pi[f % 2], pc], [ig], bias=pcol("bi_%d" % g_))

                def stageB(n):
                    s, g_ = items[n]
                    ts0 = s * T
                    gtb, obb = gt[n % 2], ob[n % 2]
                    cv, rg, ig = cv2[n % 2], rg2[n % 2], ig2[n % 2]
                    k.act(aa.t[:], rg.t[:], AF.Exp, [rg, lam], [aa], scale=lam.t[:, g_:g_ + 1])
                    k.tt(bb.t[:], aa.t[:], aa.t[:], ALU.mult, [aa], [bb])
                    k.act(bb.t[:], bb.t[:], AF.Sqrt, [bb, epsc], [bb], scale=-1.0, bias=epsc.t[:, 2:3])
                    k.tt(ig.t[:], ig.t[:], cv.t[:], ALU.mult, [ig, cv], [ig])
                    k.tt(bb.t[:], bb.t[:], ig.t[:], ALU.mult, [bb, ig], [bb])
                    k.op("dve", lambda g, : g.tensor_tensor_scan(hh_.t[:], aa.t[:], bb.t[:], 0.0, ALU.mult, ALU.add), [aa, bb], [hh_])
                    k.tt(obb.t[:], hh_.t[:], gtb.t[:], ALU.mult, [hh_, gtb], [obb])
                    k.dma("sp", cat2T[g_ * 128:(g_ + 1) * 128, ts0:ts0 + T], obb.t[:], [obb], [dbuf("cat2T", s)])

                stageA(0)
                for n in range(len(items)):
                    if n + 1 < len(items):
                        stageA(n + 1)
                    stageB(n)
            k.barrier()

        if "lru" in phases:
            phase_lru()

        if "l1out" in phases:
            phase_out_moe(1, cat2T, "cat2T", 10, w_out_odd, x2, "x2", 3, x3, "x3", out_d, "out", "m1_")

        if debug:
            with ExitStack() as pes:
                a = sb(pes, "dbg_a", [128, 512], BF16)
                b = sb(pes, "dbg_b", [128, 512])
                for (src, dst, rows) in ((catT, catT_dbg, D), (cat2T, cat2T_dbg, LRU_W)):
                    for r in range(rows // 128):
                        for c in range(NTOK // 512):
                            k.dma("sp", a.t[:], src[r * 128:(r + 1) * 128, c * 512:(c + 1) * 512], (), [a])
                            k.copy(b.t[:], a.t[:], [a], [b], e="dve")
                            k.dma("sp", dst[r * 128:(r + 1) * 128, c * 512:(c + 1) * 512], b.t[:], [b], [a])
        k.barrier(skip_pre=False)
        print("kernel build: %d instructions" % k.n_inst)
    return nc


def host_inputs(inp):
    cols, pcarr = _pcols(inp)
    c = _consts()
    sel = np.zeros((16, 16, 128), np.float32)
    for e in range(16):
        sel[e, e, :] = 1.0
    f = lambda a: np.ascontiguousarray(np.asarray(a, np.float32))
    shared = {
        "gbc": f(np.stack([np.broadcast_to(inp["norm_mix"][0], (128, D)), np.broadcast_to(inp["norm_ffn"][0], (128, D)),
                           np.broadcast_to(inp["norm_mix"][1], (128, D)), np.broadcast_to(inp["norm_ffn"][1], (128, D))])),
        "pc": f(pcarr),
        "w_in_even": f(inp["w_in_even"][0]),
        "lora_w": f(np.concatenate([inp["decay_up"][0], inp["iclr_up"][0]], axis=0)),
        "gate_up": f(inp["gate_up"][0]),
        "w_out_even": f(inp["w_out_even"][0]),
        "w_in_odd": f(inp["w_in_odd"][0]),
        "w_rgate": f(inp["w_rgate"][0]),
        "w_igate": f(inp["w_igate"][0]),
        "w_out_odd": f(inp["w_out_odd"][0]),
        "w_router": f(np.concatenate([inp["w_group"], inp["w_erouter"]], axis=2)),
        "b_router": f(np.broadcast_to(np.concatenate([inp["b_group"], inp["b_erouter"]], axis=1)[:, None, :], (2, 128, 20))),
        "exp_w_gate": f(inp["exp_w_gate"]),
        "exp_w_up": f(inp["exp_w_up"]),
        "exp_w_down": f(inp["exp_w_down"]),
        "ident": c["ident"], "blockones": c["blockones"], "ones": c["ones"], "triu": c["triu"],
        "maskrel": f(c["maskrel"]), "m64": f(c["m64"]), "sel": sel,
    }
    return cols, pcarr.shape[1], shared


def kernel(**inputs):
    inp = {k_: np.asarray(v) for k_, v in inputs.items()}
    x = inp["x"]
    B, T, _ = x.shape
    nseq = B // NCORES
    cols, npc, shared = host_inputs(inp)
    nc = build(nseq, T, cols, npc)
    in_maps = []
    for c in range(NCORES):
        m = dict(shared)
        m["x"] = np.ascontiguousarray(x[c * nseq:(c + 1) * nseq].reshape(nseq * T, D), dtype=np.float32)
        in_maps.append(m)
    res = run_bass_kernel_spmd(nc, in_maps, core_ids=list(range(NCORES)))
    out = np.concatenate([np.asarray(r["out"]).reshape(nseq, T, D) for r in res.results], axis=0)
    return out.astype(np.float32)
```

```python
import os
import numpy as np
from contextlib import ExitStack
import concourse.bass as bass
import concourse.mybir as mybir
from concourse.bass_utils import run_bass_kernel_spmd

F32 = mybir.dt.float32
BF16 = mybir.dt.bfloat16
AF = mybir.ActivationFunctionType
ALU = mybir.AluOpType
AX = mybir.AxisListType

D = 1024
NCORES = 8
A_COLS = 1824
EVEN_COLS = 3360
LRU_W = 1280
NEXP = 16
DEXP = 512
DECAY_C = 0.6065306597126334
RMS_EPS = 1e-6
GN_EPS = 64e-5
SEM_LIMIT = 30000


class Buf:
    __slots__ = ("t", "w", "r")

    def __init__(self, t):
        self.t = t
        self.w = None
        self.r = {}


class MBuf:
    __slots__ = ("t", "ch")

    def __init__(self, t, n=2):
        self.t = t
        self.ch = [Buf(t) for _ in range(n)]


def _flat(bs):
    out = []
    for b in bs:
        if isinstance(b, MBuf):
            out.extend(b.ch)
        else:
            out.append(b)
    return out


class K:
    def __init__(self, nc, es):
        self.nc = nc
        self.es = es
        self.eng = {"pe": nc.tensor, "act": nc.scalar, "dve": nc.vector, "pool": nc.gpsimd, "sp": nc.sync}
        self.sems = {}
        self.epoch = {e: 0 for e in self.eng}
        self.cnt = {}
        self.waited = {e: {} for e in self.eng}
        for e in self.eng:
            self._new_epoch(e, first=True)
        self.ndslots = {"sp": 6, "pool": 6, "act": 2, "pre": 4}
        self.qeng = {"sp": "sp", "pool": "pool", "act": "act", "pre": "pool"}
        self.dslot = {q: 0 for q in self.ndslots}
        for q, n in self.ndslots.items():
            for i in range(n):
                key = ("d", q, i)
                self.sems[key] = es.enter_context(nc.semaphore("d_%s_%d" % (q, i)))
                self.cnt[key] = 0
        self.n_inst = 0

    def _new_epoch(self, e, first=False):
        if not first:
            self.epoch[e] += 1
        key = (e, self.epoch[e])
        self.sems[key] = self.es.enter_context(self.nc.semaphore("c_%s_%d" % (e, self.epoch[e])))
        self.cnt[key] = 0

    def _wait(self, e, deps):
        for key, val in deps.items():
            if self.waited[e].get(key, 0) < val:
                self.eng[e].wait_ge(self.sems[key], val)
                self.waited[e][key] = val

    def _deps(self, e, reads, writes):
        deps = {}

        def add(kv):
            if kv is None:
                return
            key, val = kv
            if deps.get(key, 0) < val:
                deps[key] = val

        for b in reads:
            add(b.w)
        for b in writes:
            if not (e == "pe" and b.w is not None and b.w[0][0] == "pe" and not b.r):
                add(b.w)
            for key, val in b.r.items():
                add((key, val))
        return deps

    def op(self, e, fn, reads=(), writes=()):
        reads, writes = _flat(reads), _flat(writes)
        self._wait(e, self._deps(e, reads, writes))
        inst = fn(self.eng[e])
        key = (e, self.epoch[e])
        self.cnt[key] += 1
        val = self.cnt[key]
        inst.then_inc(self.sems[key], 1)
        self.n_inst += 1
        for b in reads:
            if b.r.get(key, 0) < val:
                b.r[key] = val
        for b in writes:
            b.w = (key, val)
            b.r = {}
        if val >= SEM_LIMIT:
            self._new_epoch(e)

    def dma(self, qc, out, in_, reads=(), writes=()):
        reads, writes = _flat(reads), _flat(writes)
        q = self.qeng[qc]
        self._wait(q, self._deps(q, reads, writes))
        i = self.dslot[qc]
        self.dslot[qc] = (i + 1) % self.ndslots[qc]
        key = ("d", qc, i)
        if self.cnt[key] > 0 and self.waited[q].get(key, 0) < self.cnt[key]:
            self.eng[q].wait_ge(self.sems[key], self.cnt[key])
            self.waited[q][key] = self.cnt[key]
        self.eng[q].dma_start(out=out, in_=in_).then_inc(self.sems[key], 16)
        self.cnt[key] += 16
        val = self.cnt[key]
        self.n_inst += 1
        for b in reads:
            if b.r.get(key, 0) < val:
                b.r[key] = val
        for b in writes:
            b.w = (key, val)
            b.r = {}

    def barrier(self, engines=None, skip_pre=True):
        engines = engines or list(self.eng)
        deps = {key: val for key, val in self.cnt.items() if val > 0 and not (skip_pre and key[0] == "d" and key[1] == "pre")}
        for e in engines:
            self._wait(e, deps)

    def mm(self, out, lhsT, rhs, start, stop, reads, writes):
        self.op("pe", lambda g: g.matmul(out, lhsT, rhs, start=start, stop=stop), reads, writes)

    def tr(self, out, in_, ident, reads, writes):
        self.op("pe", lambda g: g.transpose(out, in_, ident), reads, writes)

    def act(self, out, in_, func, reads, writes, bias=None, scale=None, e="act"):
        kw = {}
        if bias is not None:
            kw["bias"] = bias
        if scale is not None:
            kw["scale"] = scale
        self.op(e, lambda g: g.activation(out=out, in_=in_, func=func, **kw), reads, writes)

    def tt(self, out, in0, in1, op, reads, writes, e="dve"):
        self.op(e, lambda g: g.tensor_tensor(out=out, in0=in0, in1=in1, op=op), reads, writes)

    def ts(self, out, in0, s1, s2, op0, op1, reads, writes, e="dve"):
        if op1 is None:
            self.op(e, lambda g: g.tensor_scalar(out, in0, s1, None, op0), reads, writes)
        else:
            self.op(e, lambda g: g.tensor_scalar(out, in0, s1, s2, op0, op1), reads, writes)

    def stt(self, out, in0, scalar, in1, op0, op1, reads, writes, e="dve"):
        self.op(e, lambda g: g.scalar_tensor_tensor(out=out, in0=in0, scalar=scalar, in1=in1, op0=op0, op1=op1), reads, writes)

    def rsqrt(self, out, in_, scale, bias, reads, writes, floor=None):
        kw = {"scale": scale}
        if bias is not None:
            kw["bias"] = bias
        self.op("act", lambda g: g.activation(out=out, in_=in_, func=AF.Sqrt, **kw), reads, writes)
        if floor is not None:
            self.op("dve", lambda g: g.tensor_scalar(out, out, floor, None, ALU.max), writes, writes)
        self.op("dve", lambda g: g.reciprocal(out, out), writes, writes)

    def copy(self, out, in_, reads, writes, e="dve"):
        if e == "act":
            self.op(e, lambda g: g.activation(out=out, in_=in_, func=AF.Copy), reads, writes)
        else:
            self.op(e, lambda g: g.tensor_copy(out, in_), reads, writes)

    def memset(self, ap, val, writes, e="dve"):
        self.op(e, lambda g: g.memset(ap, val), (), writes)


IN_CHUNKS = [(i * 128, 128) for i in range(12)] + [(1536, 128), (1664, 128), (1792, 32)] + \
            [(A_COLS + i * 128, 128) for i in range(12)]


def _pcols(inp):
    cols = {}
    arrs = []

    def add(name, v):
        v = np.asarray(v, np.float32).reshape(-1)
        col = np.zeros(128, np.float32)
        col[: v.shape[0]] = v
        cols[name] = len(arrs)
        arrs.append(col)

    mu = inp["mu_a"][0]
    for ci, (c0, m) in enumerate(IN_CHUNKS[:15]):
        add("mu%d" % ci, mu[c0:c0 + m])
    for p in range(4):
        sl = slice(p * 128, (p + 1) * 128)
        add("w0_%d" % p, inp["w0"][0][sl])
        add("a0_%d" % p, inp["a0"][0][sl])
        add("kk_%d" % p, inp["k_k"][0][sl])
        add("ka_%d" % p, inp["k_a"][0][sl])
        add("rk_%d" % p, inp["r_k"][0].reshape(-1)[sl])
        add("gnw_%d" % p, inp["gn_w"][0][sl])
        add("gnb_%d" % p, inp["gn_b"][0][sl])
    add("qg", np.tile(inp["q_norm_g"][0], 2))
    add("kg", np.tile(inp["k_norm_g"][0], 2))
    for g in range(10):
        sl = slice(g * 128, (g + 1) * 128)
        for t in range(4):
            add("cw%d_%d" % (t, g), inp["conv_w"][0][t][sl])
        add("cb_%d" % g, inp["conv_b"][0][sl])
        add("br_%d" % g, inp["b_rgate"][0][sl])
        add("bi_%d" % g, inp["b_igate"][0][sl])
        add("lam_%d" % g, inp["lru_lambda"][0][sl])
    return cols, np.stack(arrs, axis=1).copy()


def _consts():
    j = np.arange(128)
    c = {}
    c["ident"] = np.eye(128, dtype=np.float32)
    bo = np.zeros((128, 128), np.float32)
    bo[:64, :64] = 1.0
    bo[64:, 64:] = 1.0
    c["blockones"] = bo
    c["ones"] = np.ones((128, 128), np.float32)
    c["triu"] = (j[:, None] >= j[None, :]).astype(np.float32)
    t = np.arange(512)
    c["maskrel"] = np.stack([((r * 128 + j[:, None]) < t[None, :]).astype(np.float32) for r in range(4)], axis=1)
    jj = np.arange(64)
    m = np.zeros((64, 4, 64), np.float32)
    m[:, 0] = (jj[:, None] < jj[None, :])
    m[:, 1] = (jj[:, None] <= jj[None, :])
    m[:, 2] = (jj[None, :] < jj[:, None])
    m[:, 3] = np.eye(64)
    c["m64"] = m
    return c


def build(NSEQ, T, pcol_idx, npc, phases=("l0in", "rwkv", "attn", "l0out", "l1in", "lru", "l1out"), debug=False):
    NTOK = NSEQ * T
    NT512 = NTOK // 512
    nc = bass.Bass("TRN2", target_bir_lowering=False)
    dbgkind = "ExternalOutput" if debug else "Internal"

    def din(name, shape, dt=F32):
        return nc.dram_tensor(name, list(shape), dt, kind="ExternalInput").ap()

    x_in = din("x", [NTOK, D])
    out_d = nc.dram_tensor("out", [NTOK, D], F32, kind="ExternalOutput").ap()
    gbc_d = din("gbc", [4, 128, D])
    pc_d = din("pc", [128, npc])
    w_in_even = din("w_in_even", [D, EVEN_COLS])
    lora_w_d = din("lora_w", [128, 512])
    gate_up_d = din("gate_up", [160, 512])
    w_out_even = din("w_out_even", [D, D])
    w_in_odd = din("w_in_odd", [D, 2 * LRU_W])
    w_rgate = din("w_rgate", [10, 128, 128])
    w_igate = din("w_igate", [10, 128, 128])
    w_out_odd = din("w_out_odd", [LRU_W, D])
    w_router = din("w_router", [2, D, 20])
    b_router = din("b_router", [2, 128, 20])
    exp_w_gate = din("exp_w_gate", [2, NEXP, D, DEXP])
    exp_w_up = din("exp_w_up", [2, NEXP, D, DEXP])
    exp_w_down = din("exp_w_down", [2, NEXP, DEXP, D])
    c_ident = din("ident", [128, 128])
    c_blockones = din("blockones", [128, 128])
    c_ones = din("ones", [128, 128])
    c_triu = din("triu", [128, 128])
    c_maskrel = din("maskrel", [128, 4, 512])
    c_m64 = din("m64", [64, 4, 64])
    c_sel = din("sel", [16, 16, 128])

    projT = nc.dram_tensor("projT", [EVEN_COLS, NTOK], F32, kind=dbgkind).ap()
    catT = nc.dram_tensor("catT", [D, NTOK], BF16, kind="Internal").ap()
    x1 = nc.dram_tensor("x1", [NTOK, D], F32, kind=dbgkind).ap()
    x2 = nc.dram_tensor("x2", [NTOK, D], F32, kind=dbgkind).ap()
    x3 = nc.dram_tensor("x3", [NTOK, D], F32, kind=dbgkind).ap()
    gateT = nc.dram_tensor("gateT", [LRU_W, NTOK], F32, kind="Internal").ap()
    recT = nc.dram_tensor("recT", [LRU_W, NTOK], F32, kind="Internal").ap()
    cat2T = nc.dram_tensor("cat2T", [LRU_W, NTOK], BF16, kind="Internal").ap()
    if debug:
        catT_dbg = nc.dram_tensor("catT_dbg", [D, NTOK], F32, kind="ExternalOutput").ap()
        cat2T_dbg = nc.dram_tensor("cat2T_dbg", [LRU_W, NTOK], F32, kind="ExternalOutput").ap()

    wgu_bf = nc.dram_tensor("wgu_bf", [2, NEXP, 128, 2, 8, DEXP], BF16, kind="Internal").ap()
    wdn_bf = nc.dram_tensor("wdn_bf", [2, NEXP, 128, 4, D], BF16, kind="Internal").ap()
    PC = pcol_idx
    dbufs = {}
    dump_list = []

    def dump(k, name, buf, ap, shape):
        if not debug:
            return
        d = nc.dram_tensor("dump_" + name, list(shape), F32, kind="ExternalOutput").ap()
        k.dma("sp", d, ap, [buf], [])

    def dbuf(name, idx):
        b = dbufs.get((name, idx))
        if b is None:
            b = Buf(None)
            dbufs[(name, idx)] = b
        return b

    with ExitStack() as es:
        k = K(nc, es)

        def sb(es_, name, shape, dt=F32):
            return Buf(es_.enter_context(nc.sbuf_tensor("sb_" + name, list(shape), dt)))

        def ps(es_, name, shape, dt=F32):
            return Buf(es_.enter_context(nc.psum_tensor("ps_" + name, list(shape), dt)))

        pc = sb(es, "pc", [128, npc])
        ident = sb(es, "ident", [128, 128])
        identb = sb(es, "identb", [128, 128], BF16)
        blockones = sb(es, "blockones", [128, 128])
        k.dma("sp", pc.t[:], pc_d[:, :], (), [pc])
        k.dma("sp", ident.t[:], c_ident[:, :], (), [ident])
        k.dma("pool", identb.t[:], c_ident[:, :], (), [identb])
        k.dma("sp", blockones.t[:], c_blockones[:, :], (), [blockones])

        epsc = sb(es, "epsc", [128, 4])
        k.memset(epsc.t[:, 0:1], RMS_EPS, [epsc])
        k.memset(epsc.t[:, 1:2], GN_EPS, [epsc])
        k.memset(epsc.t[:, 2:3], 1.0, [epsc])

        def pcol(name, rows=128):
            i = PC[name]
            return pc.t[0:rows, i:i + 1]

        def rmsnorm_T(xt, gb, hT, j, sq, ssb, hb, ptr):
            k.tt(sq.t[:], xt.t[:], xt.t[:], ALU.mult, [xt], [sq])
            k.op("dve", lambda g: g.reduce_sum(out=ssb.t[:, 0:1], in_=sq.t[:], axis=AX.X), [sq], [ssb])
            k.rsqrt(ssb.t[:, 2:3], ssb.t[:, 0:1], 1.0 / D, epsc.t[:, 0:1], [ssb, epsc], [ssb])
            k.stt(hb.t[:], xt.t[:], ssb.t[:, 2:3], gb.t[:], ALU.mult, ALU.mult, [xt, ssb, gb], [hb])
            for kc in range(8):
                k.tr(ptr.t[:, kc * 128:(kc + 1) * 128], hb.t[:, kc * 128:(kc + 1) * 128], identb.t[:], [hb, identb], [ptr])
            k.copy(hT.t[:, :, j * 128:(j + 1) * 128], ptr.t[:].rearrange("p (c t) -> p c t", t=128), [ptr], [hT], e="act")

        def precast(layer):
            for e in range(NEXP):
                wb = dbuf("wbf", (layer, e))
                k.dma("pre", wgu_bf[layer, e, :, 0, :, :], exp_w_gate[layer, e].rearrange("(c p) f -> p c f", p=128), (), [wb])
                k.dma("pre", wgu_bf[layer, e, :, 1, :, :], exp_w_up[layer, e].rearrange("(c p) f -> p c f", p=128), (), [wb])
                k.dma("pre", wdn_bf[layer, e, :, :, :], exp_w_down[layer, e].rearrange("(c p) d -> p c d", p=128), (), [wb])

        def phase_in(x_src, x_name, gidx, w_d, ncols, chunks, dst_fn, tagp, after_loads=None):
            with ExitStack() as pes:
                wsb = sb(pes, tagp + "w", [128, 8, ncols], BF16)
                gb = sb(pes, tagp + "gb", [128, D])
                k.dma("sp", gb.t[:], gbc_d[gidx], (), [gb])
                for kc in range(8):
                    k.dma("pool", wsb.t[:, kc, :], w_d[kc * 128:(kc + 1) * 128, :], (), [wsb])
                if after_loads is not None:
                    after_loads()
                xts = [sb(pes, tagp + "xt%d" % i, [128, D]) for i in range(4)]
                sq = sb(pes, tagp + "sq", [128, D])
                ssbs = [sb(pes, tagp + "ss%d" % i, [128, 4]) for i in range(4)]
                hbs = [sb(pes, tagp + "hb%d" % i, [128, D], BF16) for i in range(4)]
                hTs = [sb(pes, tagp + "hT%d" % i, [128, 8, 512], BF16) for i in range(2)]
                ptr = ps(pes, tagp + "ptr", [128, 1024], BF16)
                pps = [ps(pes, tagp + "pp%d" % i, [128, 512]) for i in range(3)]
                stg = [sb(pes, tagp + "stg%d" % i, [128, 512]) for i in range(3)]

                def A1(t, j):
                    xt, ssb, hb = xts[j], ssbs[j], hbs[j]
                    r0 = t * 512 + j * 128
                    k.dma("sp", xt.t[:], x_src[r0:r0 + 128, :], [dbuf(x_name, r0 // 128)], [xt])
                    k.tt(sq.t[:], xt.t[:], xt.t[:], ALU.mult, [xt], [sq])
                    k.op("dve", lambda g: g.reduce_sum(out=ssb.t[:, 0:1], in_=sq.t[:], axis=AX.X), [sq], [ssb])
                    k.rsqrt(ssb.t[:, 2:3], ssb.t[:, 0:1], 1.0 / D, epsc.t[:, 0:1], [ssb, epsc], [ssb])
                    k.stt(hb.t[:], xt.t[:], ssb.t[:, 2:3], gb.t[:], ALU.mult, ALU.mult, [xt, ssb, gb], [hb])

                def A2(t, j):
                    hb, hT = hbs[j], hTs[t % 2]
                    for kc in range(8):
                        k.tr(ptr.t[:, kc * 128:(kc + 1) * 128], hb.t[:, kc * 128:(kc + 1) * 128], identb.t[:], [hb, identb], [ptr])
                    k.copy(hT.t[:, :, j * 128:(j + 1) * 128], ptr.t[:].rearrange("p (c t) -> p c t", t=128), [ptr], [hT], e="act")

                for j in range(4):
                    A1(0, j)
                    A2(0, j)
                n = 0
                nch = len(chunks)
                inj = {(jj + 1) * nch // 5: jj for jj in range(4)}
                for tt_ in range(NT512):
                    hT = hTs[tt_ % 2]
                    nxt = tt_ + 1 < NT512
                    if nxt:
                        for j in range(4):
                            A1(tt_ + 1, j)
                    for ci, (c0, m) in enumerate(chunks):
                        pp = pps[n % 3]
                        st = stg[n % 3]
                        n += 1
                        for kc in range(8):
                            k.mm(pp.t[0:m, :], wsb.t[:, kc, c0:c0 + m], hT.t[:, kc, :], kc == 0, kc == 7, [wsb, hT], [pp])
                        dst_fn(ci, c0, m, tt_, pp, st)
                        if nxt and ci in inj:
                            A2(tt_ + 1, inj[ci])
            k.barrier()

        def l0_dst(ci, c0, m, tt_, pp, st):
            k.copy(st.t[0:m, :], pp.t[0:m, :], [pp], [st], e="act")
            k.dma("sp", projT[c0:c0 + m, tt_ * 512:(tt_ + 1) * 512], st.t[0:m, :], [st], [dbuf("projT", tt_ * 512 // T)])

        if "l0in" in phases:
            phase_in(x_in, "x", 0, w_in_even, EVEN_COLS, IN_CHUNKS, l0_dst, "a_", after_loads=(lambda: (precast(0), precast(1) if "l1out" in phases else None)) if "l0out" in phases else None)

        def phase_rwkv():
            with ExitStack() as pes:
                SEG = 256
                lw_t = sb(pes, "r_lw", [128, 512])
                gu1 = sb(pes, "r_gu1", [128, 512])
                gu2 = sb(pes, "r_gu2", [32, 512])
                m64 = sb(pes, "r_m64", [64, 4, 64])
                onesf = sb(pes, "r_ones", [128, 64])
                k.dma("sp", lw_t.t[:], lora_w_d[:, :], (), [lw_t])
                k.dma("sp", gu1.t[:], gate_up_d[0:128, :], (), [gu1])
                k.dma("sp", gu2.t[:], gate_up_d[128:160, :], (), [gu2])
                k.dma("sp", m64.t[:], c_m64[:, :, :], (), [m64])
                k.memset(onesf.t[:], 1.0, [onesf])
                raw = {nm: sb(pes, "r_raw_" + nm, [128, 4, SEG + 1]) for nm in ("r", "k", "v")}
                rawl = sb(pes, "r_rawl", [128, 3, SEG + 1])
                sh = {nm: sb(pes, "r_sh_" + nm, [128, 4, SEG]) for nm in ("r", "k", "v")}
                shl = sb(pes, "r_shl", [128, 3, SEG])
                tmpA = sb(pes, "r_tmpA", [128, 4, SEG])
                tmpB = sb(pes, "r_tmpB", [128, 4, SEG])
                sg = sb(pes, "r_sg", [128, 4, SEG])
                aic = sb(pes, "r_aic", [128, 4, SEG])
                gg = sb(pes, "r_g", [128, 4, SEG])
                kk = sb(pes, "r_kk", [128, 4, SEG])
                kmod = sb(pes, "r_kmod", [128, 4, SEG])
                bonus = sb(pes, "r_bonus", [128, 4, SEG])
                cs = sb(pes, "r_cs", [128, 4, SEG])
                Ep = sb(pes, "r_Ep", [128, 4, SEG])
                En = sb(pes, "r_En", [128, 4, SEG])
                Em = sb(pes, "r_Em", [128, 4, SEG])
                MD = BF16
                rt = sb(pes, "r_rt", [128, 4, SEG], MD)
                kt = sb(pes, "r_kt", [128, 4, SEG], MD)
                bt = sb(pes, "r_bt", [128, 4, SEG], MD)
                at = sb(pes, "r_at", [128, 4, SEG], MD)
                vb16 = sb(pes, "r_vb16", [128, 4, SEG], MD)
                PCp = sb(pes, "r_PCp", [128, 4, SEG // 64])
                PCH = sb(pes, "r_PCH", [64, 8, SEG // 64])
                yseg = sb(pes, "r_y", [128, 4, SEG])
                yab = sb(pes, "r_yab", [128, 4, SEG], BF16)
                def sbm(name, shape, dt=F32):
                    return MBuf(pes.enter_context(nc.sbuf_tensor("sb_" + name, list(shape), dt)))

                def psm(name, shape, dt=F32):
                    return MBuf(pes.enter_context(nc.psum_tensor("ps_" + name, list(shape), dt)))

                S = sbm("r_S", [64, 8, 64])
                Stmp = sbm("r_Stmp", [64, 8, 64])
                Sb = sbm("r_Sb", [64, 8, 64], MD)
                X = [sbm("r_X%d" % i, [64, 8, 64], MD) for i in range(2)]
                Y = [sbm("r_Y%d" % i, [64, 8, 64], MD) for i in range(2)]
                Z = [sbm("r_Z%d" % i, [64, 8, 64], MD) for i in range(2)]
                LakT = sbm("r_LakT", [64, 8, 64], MD)
                MrbT = sbm("r_MrbT", [64, 8, 64], MD)
                MrkT = sbm("r_MrkT", [64, 8, 64], MD)
                Vt = sbm("r_Vt", [64, 8, 64], MD)
                Btk = sbm("r_Btk", [64, 8, 64], MD)
                Ktk = sbm("r_Ktk", [64, 8, 64], MD)
                Wsb = sbm("r_Wsb", [64, 8, 64], MD)
                Usb = sbm("r_Usb", [64, 8, 64], MD)
                atH = sb(pes, "r_atH", [64, 8, SEG], MD)
                btH = sb(pes, "r_btH", [64, 8, SEG], MD)
                ktH = sb(pes, "r_ktH", [64, 8, SEG], MD)
                rtH = sb(pes, "r_rtH", [64, 8, SEG], MD)
                yH = sbm("r_yH", [64, 8, SEG])
                QG = [[ps(pes, "r_Q%d%d" % (g_, i), [128, 512]) for i in range(4)] for g_ in range(2)]
                P1, P2, P3, P4 = QG[0]

                def v864(b):
                    return b.t[0:64, :].rearrange("p (h j) -> p h j", j=64)

                def v8128(b):
                    return b.t[0:64, :].rearrange("p (h j) -> p h j", j=128)

                def v4128(b, rows=128):
                    return b.t[0:rows, 0:512].rearrange("p (h j) -> p h j", j=128)

                mU = m64.t[:, 0:1, :].to_broadcast([64, 4, 64])
                mUI = m64.t[:, 1:2, :].to_broadcast([64, 4, 64])
                mL = m64.t[:, 2:3, :].to_broadcast([64, 4, 64])
                mI = m64.t[:, 3:4, :].to_broadcast([64, 4, 64])

                def loads_shift(s, seg):
                    tok0 = s * T + seg * SEG
                    dep = [dbuf("projT", s)]
                    def load(dst_ap, dstbuf, r0, m):
                        if seg == 0:
                            k.memset(dst_ap[0:m, 0:1], 0.0, [dstbuf])
                            k.dma("sp", dst_ap[0:m, 1:SEG + 1], projT[r0:r0 + m, tok0:tok0 + SEG], dep, [dstbuf])
                        else:
                            k.dma("sp", dst_ap[0:m, 0:SEG + 1], projT[r0:r0 + m, tok0 - 1:tok0 + SEG], dep, [dstbuf])
                    for i, nm in enumerate(("r", "k", "v")):
                        for p in range(4):
                            load(raw[nm].t[:, p, :], raw[nm], i * 512 + p * 128, 128)
                    load(rawl.t[:, 0, :], rawl, 1536, 128)
                    load(rawl.t[:, 1, :], rawl, 1664, 128)
                    load(rawl.t[:, 2, :], rawl, 1792, 32)
                    def shift(dst, dbuf_, src, sbuf_, m, mucol):
                        k.tt(tmpA.t[0:m, 0, :], src[0:m, 0:SEG], src[0:m, 1:SEG + 1], ALU.subtract, [sbuf_], [tmpA])
                        k.stt(dst[0:m, :], tmpA.t[0:m, 0, :], pcol(mucol, m), src[0:m, 1:SEG + 1], ALU.mult, ALU.add, [tmpA, sbuf_, pc], [dbuf_])
                    for i, nm in enumerate(("r", "k", "v")):
                        for p in range(4):
                            shift(sh[nm].t[:, p, :], sh[nm], raw[nm].t[:, p, :], raw[nm], 128, "mu%d" % (i * 4 + p))
                    shift(shl.t[:, 0, :], shl, rawl.t[:, 0, :], rawl, 128, "mu12")
                    shift(shl.t[:, 1, :], shl, rawl.t[:, 1, :], rawl, 128, "mu13")
                    shift(shl.t[:, 2, :], shl, rawl.t[:, 2, :], rawl, 32, "mu14")

                seglist = [(s_, g_) for s_ in range(NSEQ) for g_ in range(T // SEG)]
                loads_shift(*seglist[0])
                for s in range(NSEQ):
                    k.memset(S.t[:], 0.0, [S])
                    k.memset(Sb.t[:], 0.0, [Sb])
                    for seg in range(T // SEG):
                        tok0 = s * T + seg * SEG
                        seg_idx = s * (T // SEG) + seg
                        STOP = int(os.environ.get("RWKV_STOP", "99"))
                        if STOP <= 0:
                            continue
                        k.act(shl.t[0:64, 0, :], shl.t[0:64, 0, :], AF.Tanh, [shl], [shl])
                        k.act(shl.t[:, 1, :], shl.t[:, 1, :], AF.Sigmoid, [shl], [shl])
                        k.act(shl.t[0:32, 2, :], shl.t[0:32, 2, :], AF.Sigmoid, [shl], [shl])
                        for p in range(4):
                            pch = slice(p * 128, (p + 1) * 128)
                            k.mm(P1.t[:, 0:SEG], lw_t.t[0:64, pch], shl.t[0:64, 0, :], True, True, [lw_t, shl], [P1])
                            k.act(sg.t[:, p, :], P1.t[:, 0:SEG], AF.Sigmoid, [P1, pc], [sg], bias=pcol("w0_%d" % p))
                            k.mm(P2.t[:, 0:SEG], lw_t.t[64:128, pch], shl.t[64:128, 0, :], True, True, [lw_t, shl], [P2])
                            k.act(aic.t[:, p, :], P2.t[:, 0:SEG], AF.Sigmoid, [P2, pc], [aic], bias=pcol("a0_%d" % p))
                            k.mm(P3.t[:, 0:SEG], gu1.t[:, pch], shl.t[:, 1, :], True, False, [gu1, shl], [P3])
                            k.mm(P3.t[:, 0:SEG], gu2.t[0:32, pch], shl.t[0:32, 2, :], False, True, [gu2, shl], [P3])
                            k.copy(gg.t[:, p, :], P3.t[:, 0:SEG], [P3], [gg], e="act")
                            k.ts(tmpA.t[:, p, :], sh["k"].t[:, p, :], pcol("kk_%d" % p), None, ALU.mult, None, [sh["k"], pc], [tmpA])
                            k.tt(tmpB.t[:, p, :], tmpA.t[:, p, :], tmpA.t[:, p, :], ALU.mult, [tmpA], [tmpB])
                            k.mm(P4.t[:, 0:SEG], blockones.t[:], tmpB.t[:, p, :], True, True, [blockones, tmpB], [P4])
                            k.rsqrt(tmpB.t[:, p, :], P4.t[:, 0:SEG], 1.0, None, [P4], [tmpB], floor=1e-12)
                            k.tt(kk.t[:, p, :], tmpA.t[:, p, :], tmpB.t[:, p, :], ALU.mult, [tmpA, tmpB], [kk])
                            k.ts(tmpA.t[:, p, :], aic.t[:, p, :], -1.0, pcol("ka_%d" % p), ALU.add, ALU.mult, [aic, pc], [tmpA])
                            k.stt(kmod.t[:, p, :], tmpA.t[:, p, :], 1.0, sh["k"].t[:, p, :], ALU.add, ALU.mult, [tmpA, sh["k"]], [kmod])
                            k.stt(tmpB.t[:, p, :], sh["r"].t[:, p, :], pcol("rk_%d" % p), kmod.t[:, p, :], ALU.mult, ALU.mult, [sh["r"], kmod, pc], [tmpB])
                            k.mm(P1.t[:, 0:SEG], blockones.t[:], tmpB.t[:, p, :], True, True, [blockones, tmpB], [P1])
                            k.tt(bonus.t[:, p, :], P1.t[:, 0:SEG], sh["v"].t[:, p, :], ALU.mult, [P1, sh["v"]], [bonus])
                            for c in range(SEG // 64):
                                ch = slice(c * 64, (c + 1) * 64)
                                k.op("dve", lambda g, p=p, ch=ch: g.tensor_tensor_scan(cs.t[:, p, ch], onesf.t[:, :], sg.t[:, p, ch], 0.0, ALU.mult, ALU.add), [sg, onesf], [cs])
                        if STOP <= 1:
                            continue
                        k.tt(tmpA.t[:], cs.t[:], sg.t[:], ALU.subtract, [cs, sg], [tmpA])
                        k.act(Ep.t[:], cs.t[:], AF.Exp, [cs], [Ep], scale=-DECAY_C)
                        k.act(En.t[:], cs.t[:], AF.Exp, [cs], [En], scale=DECAY_C)
                        k.act(Em.t[:], tmpA.t[:], AF.Exp, [tmpA], [Em], scale=-DECAY_C)
                        k.tt(rt.t[:], sh["r"].t[:], Ep.t[:], ALU.mult, [sh["r"], Ep], [rt])
                        k.tt(kt.t[:], kmod.t[:], En.t[:], ALU.mult, [kmod, En], [kt])
                        k.tt(tmpB.t[:], kk.t[:], aic.t[:], ALU.mult, [kk, aic], [tmpB])
                        k.tt(bt.t[:], tmpB.t[:], En.t[:], ALU.mult, [tmpB, En], [bt])
                        k.stt(at.t[:], kk.t[:], -1.0, Em.t[:], ALU.mult, ALU.mult, [kk, Em], [at])
                        k.copy(PCp.t[:], Ep.t[:].rearrange("p a (c j) -> p a c j", j=64)[:, :, :, 63], [Ep], [PCp], e="dve")
                        k.copy(vb16.t[:], sh["v"].t[:], [sh["v"]], [vb16], e="act")
                        if s == 0 and seg == 0:
                            for nm_, b_ in (("shr", sh["r"]), ("shk", sh["k"]), ("shv", sh["v"]), ("sg", sg), ("aic", aic), ("gg", gg), ("kk", kk),
                                            ("kmod", kmod), ("bonus", bonus), ("cs", cs), ("Ep", Ep), ("En", En), ("Em", Em),
                                            ):
                                dump(k, nm_, b_, b_.t[:], [128, 4, SEG])
                        if STOP <= 2:
                            continue
                        if seg_idx + 1 < len(seglist):
                            loads_shift(*seglist[seg_idx + 1])
                        def to_head(dstb, srcb):
                            dv = dstb.t[:].rearrange("q (p b) t -> q p b t", b=2)
                            k.dma("sp", dv[:, :, 0, :], srcb.t[0:64, :, :], [srcb], [dstb])
                            k.dma("sp", dv[:, :, 1, :], srcb.t[64:128, :, :], [srcb], [dstb])
                        to_head(atH, at)
                        to_head(btH, bt)
                        to_head(ktH, kt)
                        to_head(rtH, rt)
                        to_head(PCH, PCp)
                        identm = identb if MD == BF16 else ident

                        def chunk_gen(c, g):
                            ch = slice(c * 64, (c + 1) * 64)
                            H = slice(4 * g, 4 * g + 4)
                            hl = list(range(4 * g, 4 * g + 4))
                            bmap = {1: 0, 2: 1, 3: 2, 5: 0, 6: 1, 7: 2, 8: 3}
                            pb = {i: QG[g][bi] for i, bi in bmap.items()}
                            pv = {i: pb[i].t[0:64, 0:256].rearrange("q (h j) -> q h j", j=64) for i in bmap}

                            def T_(mb):
                                return mb.t[:, H, :]
                            Xg = [X[0].ch[g], X[1].ch[g]]
                            Yg = [Y[0].ch[g], Y[1].ch[g]]
                            Zg = [Z[0].ch[g], Z[1].ch[g]]
                            for j_, h in enumerate(hl):
                                k.mm(pv[1][:, j_, :], btH.t[:, h, ch], atH.t[:, h, ch], True, True, [btH, atH], [pb[1]])
                                k.mm(pv[2][:, j_, :], atH.t[:, h, ch], btH.t[:, h, ch], True, True, [btH, atH], [pb[2]])
                                k.mm(pv[3][:, j_, :], ktH.t[:, h, ch], atH.t[:, h, ch], True, True, [ktH, atH], [pb[3]])
                            k.tt(T_(X[0]), pv[1], mU, ALU.mult, [pb[1], m64], [Xg[0]])
                            k.tt(T_(Y[0]), pv[2], mL, ALU.mult, [pb[2], m64], [Yg[0]])
                            k.tt(T_(LakT), pv[3], mU, ALU.mult, [pb[3], m64], [LakT.ch[g]])
                            k.tt(T_(Z[0]), T_(X[0]), mI, ALU.add, [Xg[0], m64], [Zg[0]])
                            yield
                            for j_, h in enumerate(hl):
                                k.mm(pv[1][:, j_, :], Y[0].t[:, h, :], X[0].t[:, h, :], True, True, [Xg[0], Yg[0]], [pb[1]])
                                k.mm(pv[2][:, j_, :], X[0].t[:, h, :], Y[0].t[:, h, :], True, True, [Xg[0], Yg[0]], [pb[2]])
                            k.copy(T_(X[1]), pv[1], [pb[1]], [Xg[1]], e="act")
                            k.copy(T_(Y[1]), pv[2], [pb[2]], [Yg[1]], e="dve")
                            yield
                            xi, zi = 1, 0
                            for r in range(1, 6):
                                for j_, h in enumerate(hl):
                                    k.mm(pv[3][:, j_, :], Y[xi].t[:, h, :], Z[zi].t[:, h, :], True, True, [Yg[xi], Zg[zi]], [pb[3]])
                                    if r < 5:
                                        k.mm(pv[2][:, j_, :], X[xi].t[:, h, :], Y[xi].t[:, h, :], True, True, [Xg[xi], Yg[xi]], [pb[2]])
                                    if r < 4:
                                        k.mm(pv[1][:, j_, :], Y[xi].t[:, h, :], X[xi].t[:, h, :], True, True, [Xg[xi], Yg[xi]], [pb[1]])
                                k.tt(T_(Z[1 - zi]), pv[3], T_(Z[zi]), ALU.add, [pb[3], Zg[zi]], [Zg[1 - zi]])
                                if r < 5:
                                    k.copy(T_(Y[1 - xi]), pv[2], [pb[2]], [Yg[1 - xi]], e="act")
                                if r < 4:
                                    k.copy(T_(X[1 - xi]), pv[1], [pb[1]], [Xg[1 - xi]], e="dve")
                                xi, zi = 1 - xi, 1 - zi
                                yield
                            Zf, Zfb = Z[zi], Zg[zi]
                            for j_, h in enumerate(hl):
                                k.mm(pv[1][:, j_, :], btH.t[:, h, ch], rtH.t[:, h, ch], True, True, [btH, rtH], [pb[1]])
                                k.mm(pv[2][:, j_, :], ktH.t[:, h, ch], rtH.t[:, h, ch], True, True, [ktH, rtH], [pb[2]])
                            k.tt(T_(MrbT), pv[1], mUI, ALU.mult, [pb[1], m64], [MrbT.ch[g]])
                            k.tt(T_(MrkT), pv[2], mUI, ALU.mult, [pb[2], m64], [MrkT.ch[g]])
                            yield
                            tvs = [P.t[:].bitcast(BF16) if MD == BF16 else P.t[:] for P in (pb[1], pb[2], pb[3])]
                            for pp in range(2):
                                p = 2 * g + pp
                                cs_ = slice(pp * 128, (pp + 1) * 128)
                                k.tr(tvs[0][0:64, cs_], vb16.t[:, p, ch], identm.t[:], [vb16, identm], [pb[1]])
                                k.tr(tvs[1][0:64, cs_], bt.t[:, p, ch], identm.t[:], [bt, identm], [pb[2]])
                                k.tr(tvs[2][0:64, cs_], kt.t[:, p, ch], identm.t[:], [kt, identm], [pb[3]])
                            gv = [tv[0:64, 0:256].rearrange("q (h j) -> q h j", j=64) for tv in tvs]
                            k.copy(T_(Vt), gv[0], [pb[1]], [Vt.ch[g]], e="act")
                            k.copy(T_(Btk), gv[1], [pb[2]], [Btk.ch[g]], e="dve")
                            k.copy(T_(Ktk), gv[2], [pb[3]], [Ktk.ch[g]], e="act")
                            yield
                            for j_, h in enumerate(hl):
                                k.mm(pv[5][:, j_, :], atH.t[:, h, ch], Sb.t[:, h, :], True, False, [atH, Sb.ch[g]], [pb[5]])
                                k.mm(pv[5][:, j_, :], LakT.t[:, h, :], Vt.t[:, h, :], False, True, [LakT.ch[g], Vt.ch[g]], [pb[5]])
                            k.copy(T_(Wsb), pv[5], [pb[5]], [Wsb.ch[g]], e="act")
                            yield
                            for j_, h in enumerate(hl):
                                k.mm(pv[6][:, j_, :], Zf.t[:, h, :], Wsb.t[:, h, :], True, True, [Zfb, Wsb.ch[g]], [pb[6]])
                            k.copy(T_(Usb), pv[6], [pb[6]], [Usb.ch[g]], e="dve")
                            yield
                            for j_, h in enumerate(hl):
                                k.mm(pv[7][:, j_, :], Sb.t[:, h, :], rtH.t[:, h, ch], True, False, [Sb.ch[g], rtH], [pb[7]])
                                k.mm(pv[7][:, j_, :], Usb.t[:, h, :], MrbT.t[:, h, :], False, False, [Usb.ch[g], MrbT.ch[g]], [pb[7]])
                                k.mm(pv[7][:, j_, :], Vt.t[:, h, :], MrkT.t[:, h, :], False, True, [Vt.ch[g], MrkT.ch[g]], [pb[7]])
                            for j_, h in enumerate(hl):
                                k.mm(pv[8][:, j_, :], Btk.t[:, h, :], Usb.t[:, h, :], True, False, [Btk.ch[g], Usb.ch[g]], [pb[8]])
                                k.mm(pv[8][:, j_, :], Ktk.t[:, h, :], Vt.t[:, h, :], False, True, [Ktk.ch[g], Vt.ch[g]], [pb[8]])
                            k.copy(yH.t[:, H, ch], pv[7], [pb[7]], [yH.ch[g]], e="act")
                            pcc = PCH.t[:, H, c:c + 1].to_broadcast([64, 4, 64])
                            k.tt(T_(Stmp), T_(S), pv[8], ALU.add, [S.ch[g], pb[8]], [Stmp.ch[g]])
                            k.tt(T_(S), T_(Stmp), pcc, ALU.mult, [Stmp.ch[g], PCH], [S.ch[g]])
                            k.copy(T_(Sb), T_(S), [S.ch[g]], [Sb.ch[g]], e="act")
                            yield

                        for c in range(SEG // 64):
                            gens = [chunk_gen(c, 0), chunk_gen(c, 1)]
                            alive = [True, True]
                            while any(alive):
                                for gi in range(2):
                                    if alive[gi]:
                                        try:
                                            next(gens[gi])
                                        except StopIteration:
                                            alive[gi] = False
                        yv = yH.t[:].rearrange("q (p b) t -> q p b t", b=2)
                        k.dma("sp", yseg.t[0:64, :, :], yv[:, :, 0, :], [yH], [yseg])
                        k.dma("sp", yseg.t[64:128, :, :], yv[:, :, 1, :], [yH], [yseg])
                        if s == 0 and seg == 0:
                            dump(k, "yseg", yseg, yseg.t[:], [128, 4, SEG])
                        for p in range(4):
                            k.mm(P1.t[:, 0:SEG], blockones.t[:], yseg.t[:, p, :], True, True, [blockones, yseg], [P1])
                            k.stt(tmpA.t[:, p, :], P1.t[:, 0:SEG], -1.0 / 64, yseg.t[:, p, :], ALU.mult, ALU.add, [P1, yseg], [tmpA])
                            k.tt(tmpB.t[:, p, :], tmpA.t[:, p, :], tmpA.t[:, p, :], ALU.mult, [tmpA], [tmpB])
                            k.mm(P2.t[:, 0:SEG], blockones.t[:], tmpB.t[:, p, :], True, True, [blockones, tmpB], [P2])
                            k.rsqrt(tmpB.t[:, p, :], P2.t[:, 0:SEG], 1.0 / 64, epsc.t[:, 1:2], [P2, epsc], [tmpB])
                            k.tt(tmpA.t[:, p, :], tmpA.t[:, p, :], tmpB.t[:, p, :], ALU.mult, [tmpA, tmpB], [tmpA])
                            k.ts(tmpA.t[:, p, :], tmpA.t[:, p, :], pcol("gnw_%d" % p), pcol("gnb_%d" % p), ALU.mult, ALU.add, [tmpA, pc], [tmpA])
                            k.tt(tmpA.t[:, p, :], tmpA.t[:, p, :], bonus.t[:, p, :], ALU.add, [tmpA, bonus], [tmpA])
                            k.tt(yab.t[:, p, :], tmpA.t[:, p, :], gg.t[:, p, :], ALU.mult, [tmpA, gg], [yab])
                            k.dma("sp", catT[p * 128:(p + 1) * 128, tok0:tok0 + SEG], yab.t[:, p, :], [yab], [dbuf("catT", s)])
            k.barrier()

        if "rwkv" in phases:
            phase_rwkv()

        def phase_attn():
            with ExitStack() as pes:
                triu = sb(pes, "s_triu", [128, 128], BF16)
                onesb = sb(pes, "s_ones", [128, 128], BF16)
                maskb = sb(pes, "s_maskb", [128, 4, 512], BF16)
                k.dma("pool", triu.t[:], c_triu[:, :], (), [triu])
                k.dma("pool", onesb.t[:], c_ones[:, :], (), [onesb])
                k.dma("pool", maskb.t[:], c_maskrel[:, :, :], (), [maskb])
                qraw = sb(pes, "s_qraw", [128, T])
                kraw = sb(pes, "s_kraw", [128, T])
                vb = sb(pes, "s_vb", [128, T], BF16)
                sq = sb(pes, "s_sq", [128, T])
                rs = sb(pes, "s_rs", [128, T])
                qn2 = [sb(pes, "s_qn%d" % i, [128, T], BF16) for i in range(2)]
                kn2 = [sb(pes, "s_kn%d" % i, [128, T], BF16) for i in range(2)]
                vtok2 = [sb(pes, "s_vtok%d" % i, [128, T // 128, 128], BF16) for i in range(2)]
                ND = 6
                spb = [sb(pes, "s_sp%d" % i, [128, 512], BF16) for i in range(ND)]
                zsb = [sb(pes, "s_zs%d" % i, [128, 512]) for i in range(ND)]
                t1 = [sb(pes, "s_t1%d" % i, [128, 512]) for i in range(ND)]
                att = [sb(pes, "s_att%d" % i, [128, 512], BF16) for i in range(ND)]
                ob = [sb(pes, "s_ob%d" % i, [64, 512], BF16) for i in range(2)]
                PZ = [ps(pes, "s_PZ%d" % i, [128, 512]) for i in range(2)]
                PT = [ps(pes, "s_PT%d" % i, [128, 512]) for i in range(2)]
                PR = [ps(pes, "s_PR%d" % i, [128, 512]) for i in range(2)]
                PV = [ps(pes, "s_PV%d" % i, [128, 512]) for i in range(2)]
                Rb = sb(pes, "s_R", [128, 512])
                qnn2 = [sb(pes, "s_qnn%d" % i, [128, T], BF16) for i in range(2)]

                def prep(s, p, par):
                    dep = [dbuf("projT", s)]
                    ts0 = s * T
                    qn, kn, vtok = qn2[par], kn2[par], vtok2[par]
                    k.dma("sp", qraw.t[:], projT[A_COLS + p * 128:A_COLS + (p + 1) * 128, ts0:ts0 + T], dep, [qraw])
                    k.dma("sp", kraw.t[:], projT[A_COLS + 512 + p * 128:A_COLS + 512 + (p + 1) * 128, ts0:ts0 + T], dep, [kraw])
                    k.dma("pool", vb.t[:], projT[A_COLS + 1024 + p * 128:A_COLS + 1024 + (p + 1) * 128, ts0:ts0 + T], dep, [vb])
                    for (rawb, outb, gname) in ((qraw, qn, "qg"), (kraw, kn, "kg")):
                        k.tt(sq.t[:], rawb.t[:], rawb.t[:], ALU.mult, [rawb], [sq])
                        for f in range(T // 512):
                            fs = slice(f * 512, (f + 1) * 512)
                            pz = PZ[f % 2]
                            k.mm(pz.t[:], blockones.t[:], sq.t[:, fs], True, True, [blockones, sq], [pz])
                            k.rsqrt(rs.t[:, fs], pz.t[:], 1.0 / 64, epsc.t[:, 0:1], [pz, epsc], [rs])
                        k.stt(outb.t[:], rawb.t[:], pcol(gname), rs.t[:], ALU.mult, ALU.mult, [rawb, rs, pc], [outb])
                    k.ts(qnn2[par].t[:], qn.t[:], -0.125, None, ALU.mult, None, [qn], [qnn2[par]])
                    for kb in range(T // 128):
                        pz = PT[kb % 2]
                        pzb = pz.t[:].bitcast(BF16)
                        k.tr(pzb[:, 0:128], vb.t[:, kb * 128:(kb + 1) * 128], identb.t[:], [vb, identb], [pz])
                        k.copy(vtok.t[:, kb, :], pzb[:, 0:128], [pz], [vtok], e="act")

                pairs = [(s, p) for s in range(NSEQ) for p in range(4)]
                steps = []
                for pi, (s, p) in enumerate(pairs):
                    for hh in range(2):
                        for QT in range(T // 512):
                            kb_hi = QT * 4 + 3
                            for kb in range(kb_hi, -1, -1):
                                steps.append((pi, s, p, hh, QT, kb, kb_hi))
                qtidx = {}
                for st in steps:
                    key = (st[0], st[3], st[4])
                    if key not in qtidx:
                        qtidx[key] = len(qtidx)

                def stepA(n):
                    pi, s, p, hh, QT, kb, kb_hi = steps[n]
                    R = slice(hh * 64, hh * 64 + 64)
                    qs = slice(QT * 512, (QT + 1) * 512)
                    qn, kn = qn2[pi % 2], kn2[pi % 2]
                    pz, sp_, zs_ = PZ[n % 2], spb[n % ND], zsb[n % ND]
                    rel = kb - QT * 4
                    k.mm(pz.t[:], kn.t[R, kb * 128:(kb + 1) * 128], qn.t[R, qs], True, True, [kn, qn], [pz])
                    k.act(zs_.t[:], pz.t[:], AF.Exp, [pz], [zs_], scale=0.125)
                    k.act(sp_.t[:], zs_.t[:], AF.Ln, [zs_, epsc], [sp_], bias=epsc.t[:, 2:3])
                    if rel >= 0:
                        k.tt(sp_.t[:], sp_.t[:], maskb.t[:, rel, :], ALU.mult, [sp_, maskb], [sp_])

                def stepB(n):
                    pi, s, p, hh, QT, kb, kb_hi = steps[n]
                    R = slice(hh * 64, hh * 64 + 64)
                    qs = slice(QT * 512, (QT + 1) * 512)
                    kn, qnn = kn2[pi % 2], qnn2[pi % 2]
                    pt, po = PT[n % 2], PR[n % 2]
                    sp_, t1_, at_ = spb[n % ND], t1[n % ND], att[n % ND]
                    first = kb == kb_hi
                    k.mm(pt.t[:], kn.t[R, kb * 128:(kb + 1) * 128], qnn.t[R, qs], True, False, [kn, qnn], [pt])
                    k.mm(pt.t[:], triu.t[:], sp_.t[:], False, True, [triu, sp_], [pt])
                    if kb > 0:
                        k.mm(po.t[:], onesb.t[:], sp_.t[:], True, True, [onesb, sp_], [po])
                    if first:
                        k.act(at_.t[:], pt.t[:], AF.Exp, [pt], [at_], scale=-1.0)
                    else:
                        k.tt(t1_.t[:], pt.t[:], Rb.t[:], ALU.add, [pt, Rb], [t1_])
                        k.act(at_.t[:], t1_.t[:], AF.Exp, [t1_], [at_], scale=-1.0)
                    if kb > 0:
                        if first:
                            k.copy(Rb.t[:], po.t[:], [po], [Rb], e="dve")
                        else:
                            k.tt(Rb.t[:], Rb.t[:], po.t[:], ALU.add, [Rb, po], [Rb])

                def stepC(n):
                    pi, s, p, hh, QT, kb, kb_hi = steps[n]
                    R = slice(hh * 64, hh * 64 + 64)
                    h = 2 * p + hh
                    q = qtidx[(pi, hh, QT)] % 2
                    vtok = vtok2[pi % 2]
                    at_ = att[n % ND]
                    pv, obb = PV[q], ob[q]
                    rel = kb - QT * 4
                    if rel >= 0:
                        k.tt(at_.t[:], at_.t[:], maskb.t[:, rel, :], ALU.mult, [at_, maskb], [at_])
                    k.mm(pv.t[0:64, :], vtok.t[:, kb, R], at_.t[:], kb == kb_hi, kb == 0, [vtok, at_], [pv])
                    if kb == 0:
                        ts0 = s * T
                        k.copy(obb.t[:], pv.t[0:64, :], [pv], [obb], e="act")
                        k.dma("sp", catT[512 + h * 64:512 + (h + 1) * 64, ts0 + QT * 512:ts0 + (QT + 1) * 512], obb.t[:], [obb], [dbuf("catT", s)])

                NS_ = len(steps)
                per_pair = NS_ // len(pairs)
                prep(pairs[0][0], pairs[0][1], 0)
                DA, DB = 4, 2
                for n in range(-DA, NS_):
                    if n >= 0 and n % per_pair == 8:
                        pi = n // per_pair
                        if pi + 1 < len(pairs):
                            prep(pairs[pi + 1][0], pairs[pi + 1][1], (pi + 1) % 2)
                    if n + DA < NS_:
                        stepA(n + DA)
                    if 0 <= n + DB < NS_:
                        stepB(n + DB)
                    if n >= 0:
                        stepC(n)
            k.barrier()

        if "attn" in phases:
            phase_attn()

        def phase_out_moe(layer, cat_src, cat_name, nkc, w_out_d, x_src, x_name, gidx, x_mid, x_mid_name, x_dst, x_dst_name, tagp):
            TT = min(1024, NTOK)
            NSUB = TT // 128
            NH = TT // 512
            with ExitStack() as pes:
                wo = sb(pes, tagp + "wo", [128, nkc, D], BF16)
                for kc in range(nkc):
                    k.dma("pool", wo.t[:, kc, :], w_out_d[kc * 128:(kc + 1) * 128, :], (), [wo])
                gb = sb(pes, tagp + "gb", [128, D])
                k.dma("sp", gb.t[:], gbc_d[gidx], (), [gb])
                wr = sb(pes, tagp + "wr", [128, 8, 20])
                k.dma("sp", wr.t[:], w_router[layer].rearrange("(c p) n -> p c n", p=128), (), [wr])
                br = sb(pes, tagp + "br", [128, 20])
                k.dma("sp", br.t[:], b_router[layer], (), [br])
                sel = sb(pes, tagp + "sel", [16, 16, 128], BF16)
                k.dma("pool", sel.t[:], c_sel[:, :, :], (), [sel])
                if layer == 0 and "l1out" in phases and "l0in" not in phases:
                    precast(1)
                if layer == 1 and "l0out" not in phases:
                    precast(1)
                if layer == 0 and "l0in" not in phases:
                    precast(0)
                catb = [sb(pes, tagp + "cat%d" % i, [128, nkc, 128], BF16) for i in range(2)]
                xt = [sb(pes, tagp + "xt%d" % i, [128, D]) for i in range(2)]
                ssb = sb(pes, tagp + "ss", [128, 4])
                hb = sb(pes, tagp + "hb", [128, D], BF16)
                hf = sb(pes, tagp + "hf", [128, D])
                sq = hf
                hTf = sb(pes, tagp + "hTf", [128, 8, 128])
                hT2 = [sb(pes, tagp + "hT%d" % i, [128, 8, TT], BF16) for i in range(2)]
                yacc = sb(pes, tagp + "yacc", [128, NSUB, D])
                rt_ = sb(pes, tagp + "rt", [128, NSUB, 64])
                LG = sb(pes, tagp + "LG", [128, NSUB, 20])
                dg = sb(pes, tagp + "dg", [128, NSUB, 16], BF16)
                gT2 = [sb(pes, tagp + "gT%d" % i, [16, TT], BF16) for i in range(2)]
                gbc_e = [sb(pes, tagp + "gbe%d" % i, [128, TT], BF16) for i in range(2)]
                wgu = [sb(pes, tagp + "wgu%d" % i, [128, 2, 8, DEXP], BF16) for i in range(2)]
                wdn = [sb(pes, tagp + "wdn%d" % i, [128, 4, D], BF16) for i in range(2)]
                silu = [sb(pes, tagp + "silu%d" % i, [128, 512]) for i in range(2)]
                hid = [sb(pes, tagp + "hid%d" % i, [128, 4, TT], BF16) for i in range(2)]
                ptr = ps(pes, tagp + "ptr", [128, 1024], BF16)
                pg = [ps(pes, tagp + "pg%d" % i, [128, 512]) for i in range(2)]
                pu = [ps(pes, tagp + "pu%d" % i, [128, 512]) for i in range(2)]
                py = [ps(pes, tagp + "py%d" % i, [128, 512]) for i in range(3)]
                cnt_ = {"nf": 0, "ny": 0, "nx": 0}
                nx = 0
                ne = 0
                nf = 0
                ny = 0
                def make_front(tile):
                    t0 = tile * TT
                    hT = hT2[tile % 2]
                    gT = gT2[tile % 2]
                    cl = []
                    for j in range(NSUB):
                        def sub1(j=j):
                            r0 = t0 + j * 128
                            cb = catb[cnt_["nx"] % 2]
                            xb = xt[cnt_["nx"] % 2]
                            cnt_["nx"] += 1
                            k.dma("sp", cb.t[:], cat_src[:, r0:r0 + 128].rearrange("(c p) t -> p c t", p=128), [dbuf(cat_name, r0 // T)], [cb])
                            k.dma("pool", xb.t[:], x_src[r0:r0 + 128, :], [dbuf(x_name, r0 // 128)], [xb])
                            for half in range(2):
                                pp = py[half]
                                for kc in range(nkc):
                                    k.mm(pp.t[:], cb.t[:, kc, :], wo.t[:, kc, half * 512:(half + 1) * 512], kc == 0, kc == nkc - 1, [cb, wo], [pp])
                                k.tt(xb.t[:, half * 512:(half + 1) * 512], xb.t[:, half * 512:(half + 1) * 512], pp.t[:], ALU.add, [xb, pp], [xb])
                            k.dma("pool", x_mid[r0:r0 + 128, :], xb.t[:], [xb], [dbuf(x_mid_name, r0 // 128)])
                            k.tt(sq.t[:], xb.t[:], xb.t[:], ALU.mult, [xb], [sq])
                            k.op("dve", lambda g: g.reduce_sum(out=ssb.t[:, 0:1], in_=sq.t[:], axis=AX.X), [sq], [ssb])
                            k.rsqrt(ssb.t[:, 2:3], ssb.t[:, 0:1], 1.0 / D, epsc.t[:, 0:1], [ssb, epsc], [ssb])
                            k.stt(hf.t[:], xb.t[:], ssb.t[:, 2:3], gb.t[:], ALU.mult, ALU.mult, [xb, ssb, gb], [hf])
                            k.copy(hb.t[:], hf.t[:], [hf], [hb], e="act")
                        def sub2(j=j):
                            for kc in range(8):
                                k.tr(ptr.t[:, kc * 128:(kc + 1) * 128], hb.t[:, kc * 128:(kc + 1) * 128], identb.t[:], [hb, identb], [ptr])
                                pf_ = py[kc // 4]
                                k.tr(pf_.t[:, (kc % 4) * 128:(kc % 4 + 1) * 128], hf.t[:, kc * 128:(kc + 1) * 128], ident.t[:], [hf, ident], [pf_])
                            k.copy(hT.t[:, :, j * 128:(j + 1) * 128], ptr.t[:].rearrange("p (c t) -> p c t", t=128), [ptr], [hT], e="act")
                            for q_ in range(2):
                                k.copy(hTf.t[:, q_ * 4:(q_ + 1) * 4, :], py[q_].t[:].rearrange("p (c t) -> p c t", t=128), [py[q_]], [hTf], e="dve")
                        def sub3(j=j):
                            pr = pg[0]
                            for kc in range(8):
                                k.mm(pr.t[:, 0:20], hTf.t[:, kc, :], wr.t[:, kc, :], kc == 0, kc == 7, [hTf, wr], [pr])
                            k.tt(LG.t[:, j, :], pr.t[:, 0:20], br.t[:], ALU.add, [pr, br], [LG])
                        cl += [sub1, None, None, sub2, None, None, sub3, None, None]

                    def routing():
                        Rr = rt_.t
                        NS = NSUB

                        def col(a_, b_=None):
                            return Rr[:, :, a_:(a_ + 1 if b_ is None else b_)]

                        def bc(ap1, n_):
                            return ap1.to_broadcast([128, NS, n_])

                        gl = LG.t[:, :, 0:4]
                        k.op("dve", lambda g: g.reduce_max(out=col(44), in_=gl, axis=AX.X), [LG], [rt_])
                        k.tt(col(0, 4), gl, bc(col(44), 4), ALU.is_equal, [LG, rt_], [rt_])
                        k.tt(col(4, 8), gl, bc(col(44), 4), ALU.subtract, [LG, rt_], [rt_])
                        k.act(col(4, 8), col(4, 8), AF.Exp, [rt_], [rt_])
                        k.op("dve", lambda g: g.reduce_sum(out=col(45), in_=col(4, 8), axis=AX.X), [rt_], [rt_])
                        k.op("dve", lambda g: g.reciprocal(col(45), col(45)), [rt_], [rt_])
                        el4 = LG.t[:, :, 4:20].rearrange("p s (g e) -> p s g e", e=4)
                        t16 = Rr[:, :, 28:44].rearrange("p s (g e) -> p s g e", e=4)
                        k.tt(t16, el4, Rr[:, :, 0:4].unsqueeze(3).to_broadcast([128, NS, 4, 4]), ALU.mult, [LG, rt_], [rt_])
                        k.op("dve", lambda g: g.reduce_sum(out=col(8, 12), in_=Rr[:, :, 28:44].rearrange("p s (g e) -> p s e g", e=4), axis=AX.X), [rt_], [rt_])
                        k.op("dve", lambda g: g.reduce_max(out=col(46), in_=col(8, 12), axis=AX.X), [rt_], [rt_])
                        k.tt(col(12, 16), col(8, 12), bc(col(46), 4), ALU.is_equal, [rt_], [rt_])
                        k.stt(col(16, 20), col(12, 16), -1e30, col(8, 12), ALU.mult, ALU.add, [rt_], [rt_])
                        k.op("dve", lambda g: g.reduce_max(out=col(47), in_=col(16, 20), axis=AX.X), [rt_], [rt_])
                        k.tt(col(20, 24), col(16, 20), bc(col(47), 4), ALU.is_equal, [rt_], [rt_])
                        k.tt(col(48), col(47), col(46), ALU.subtract, [rt_], [rt_])
                        k.act(col(49), col(48), AF.Exp, [rt_], [rt_])
                        k.ts(col(50), col(49), 1.0, None, ALU.add, None, [rt_], [rt_])
                        k.op("dve", lambda g: g.reciprocal(col(50), col(50)), [rt_], [rt_])
                        k.tt(col(51), col(50), col(45), ALU.mult, [rt_], [rt_])
                        k.tt(col(52), col(51), col(49), ALU.mult, [rt_], [rt_])
                        k.tt(col(24, 28), col(12, 16), bc(col(51), 4), ALU.mult, [rt_], [rt_])
                        k.tt(col(4, 8), col(20, 24), bc(col(52), 4), ALU.mult, [rt_], [rt_])
                        k.tt(col(24, 28), col(24, 28), col(4, 8), ALU.add, [rt_], [rt_])
                        k.tt(dg.t[:].rearrange("p s (g e) -> p s g e", e=4), Rr[:, :, 0:4].unsqueeze(3).to_broadcast([128, NS, 4, 4]),
                             Rr[:, :, 24:28].unsqueeze(2).to_broadcast([128, NS, 4, 4]), ALU.mult, [rt_], [dg])
                        for j in range(NSUB):
                            ptb = ptr.t[:]
                            k.tr(ptb[0:16, j * 128:(j + 1) * 128], dg.t[:, j, :], identb.t[:], [dg, identb], [ptr])
                        k.copy(gT.t[:, :], ptr.t[0:16, 0:TT], [ptr], [gT], e="act")

                    cl.append(routing)
                    return cl

                ntiles = NTOK // TT
                AQ = "pool"

                def make_expert(tile, e, wg_, wd_, ge_, hid_, hT, gT):
                    def pro():
                        for hlf in range(NH):
                            pp = py[hlf % 2]
                            k.mm(pp.t[:], sel.t[:, e, :], gT.t[:, hlf * 512:(hlf + 1) * 512], True, True, [sel, gT], [pp])
                            k.copy(ge_.t[:, hlf * 512:(hlf + 1) * 512], pp.t[:], [pp], [ge_], e="act")
                    G = []
                    for fc in range(4):
                        for hlf in range(NH):
                            def g(fc=fc, hlf=hlf):
                                hs_ = slice(hlf * 512, (hlf + 1) * 512)
                                i_ = cnt_["nf"] % 2
                                cnt_["nf"] += 1
                                pgg, puu, sl_ = pg[i_], pu[i_], silu[i_]
                                for kc in range(8):
                                    k.mm(pgg.t[:], wg_.t[:, 0, kc, fc * 128:(fc + 1) * 128], hT.t[:, kc, hs_], kc == 0, kc == 7, [wg_, hT], [pgg])
                                for kc in range(8):
                                    k.mm(puu.t[:], wg_.t[:, 1, kc, fc * 128:(fc + 1) * 128], hT.t[:, kc, hs_], kc == 0, kc == 7, [wg_, hT], [puu])
                                k.act(sl_.t[:], pgg.t[:], AF.Silu, [pgg], [sl_])
                                k.tt(sl_.t[:], sl_.t[:], puu.t[:], ALU.mult, [sl_, puu], [sl_])
                                k.tt(hid_.t[:, fc, hs_], sl_.t[:], ge_.t[:, hs_], ALU.mult, [sl_, ge_], [hid_])
                            G.append(g)
                    Dn = []
                    for j in range(NSUB):
                        for half in range(2):
                            def d(j=j, half=half):
                                pp = py[cnt_["ny"] % 3]
                                cnt_["ny"] += 1
                                for fc in range(4):
                                    k.mm(pp.t[:], hid_.t[:, fc, j * 128:(j + 1) * 128], wd_.t[:, fc, half * 512:(half + 1) * 512], fc == 0, fc == 3, [hid_, wd_], [pp])
                                ya = yacc.t[:, j, half * 512:(half + 1) * 512]
                                if e == 0:
                                    k.copy(ya, pp.t[:], [pp], [yacc], e="dve")
                                else:
                                    k.tt(ya, ya, pp.t[:], ALU.add, [yacc, pp], [yacc])
                            Dn.append(d)
                    return pro, G, Dn

                def tail(tile):
                    for j in range(NSUB):
                        r0 = tile * TT + j * 128
                        xb = xt[cnt_["nx"] % 2]
                        cnt_["nx"] += 1
                        k.dma(AQ, xb.t[:], x_mid[r0:r0 + 128, :], [dbuf(x_mid_name, r0 // 128)], [xb])
                        k.tt(xb.t[:], xb.t[:], yacc.t[:, j, :], ALU.add, [xb, yacc], [xb], e="pool")
                        k.dma(AQ, x_dst[r0:r0 + 128, :], xb.t[:], [xb], [dbuf(x_dst_name, r0 // 128)])

                inst = [(tile, e) for tile in range(ntiles) for e in range(NEXP)]

                def load_wg(n):
                    e_ = inst[n][1]
                    k.dma("sp", wgu[n % 2].t[:], wgu_bf[layer, e_], [dbuf("wbf", (layer, e_))], [wgu[n % 2]])

                def load_wd(n):
                    e_ = inst[n][1]
                    k.dma("sp", wdn[n % 2].t[:], wdn_bf[layer, e_], [dbuf("wbf", (layer, e_))], [wdn[n % 2]])

                load_wg(0)
                load_wd(0)
                for f_ in make_front(0):
                    if f_ is not None:
                        f_()
                prevD = None
                pending = []
                for n, (tile, e) in enumerate(inst):
                    if e == 0:
                        pending = make_front(tile + 1) if tile + 1 < ntiles else []
                    if n + 1 < len(inst):
                        load_wg(n + 1)
                    pro, G, Dn = make_expert(tile, e, wgu[n % 2], wdn[n % 2], gbc_e[n % 2], hid[n % 2], hT2[tile % 2], gT2[tile % 2])
                    pro()

                    def inject():
                        if pending:
                            it = pending.pop(0)
                            if it is not None:
                                it()
                    if prevD is None:
                        for g in G:
                            g()
                            inject()
                    else:
                        per = (len(prevD) + len(G) - 1) // len(G)
                        di = 0
                        for g in G:
                            g()
                            for _ in range(per):
                                if di < len(prevD):
                                    prevD[di]()
                                    di += 1
                            inject()
                        while di < len(prevD):
                            prevD[di]()
                            di += 1
                    if n + 1 < len(inst):
                        load_wd(n + 1)
                    if e == 0 and tile > 0:
                        tail(tile - 1)
                    if e == NEXP - 1:
                        while pending:
                            it = pending.pop(0)
                            if it is not None:
                                it()
                    prevD = Dn
                for d in prevD:
                    d()
                tail(ntiles - 1)
            k.barrier()

        if "l0out" in phases:
            phase_out_moe(0, catT, "catT", 8, w_out_even, x_in, "x", 1, x1, "x1", x2, "x2", "m0_")

        L1_CHUNKS = [(i * 128, 128) for i in range(20)]

        def l1_dst(ci, c0, m, tt_, pp, st):
            if ci < 10:
                k.act(st.t[:], pp.t[:], AF.Gelu_apprx_tanh, [pp], [st])
                k.dma("sp", gateT[c0:c0 + 128, tt_ * 512:(tt_ + 1) * 512], st.t[:], [st], [dbuf("gateT", tt_ * 512 // T)])
            else:
                k.copy(st.t[:], pp.t[:], [pp], [st], e="act")
                k.dma("sp", recT[c0 - LRU_W:c0 - LRU_W + 128, tt_ * 512:(tt_ + 1) * 512], st.t[:], [st], [dbuf("recT", tt_ * 512 // T)])

        if "l1in" in phases:
            phase_in(x2, "x2", 2, w_in_odd, 2 * LRU_W, L1_CHUNKS, l1_dst, "b_")

        def phase_lru():
            with ExitStack() as pes:
                wrg = sb(pes, "u_wr", [128, 10, 128], BF16)
                wig = sb(pes, "u_wi", [128, 10, 128], BF16)
                k.dma("pool", wrg.t[:], w_rgate.rearrange("g i j -> i g j"), (), [wrg])
                k.dma("pool", wig.t[:], w_igate.rearrange("g i j -> i g j"), (), [wig])
                lam = sb(pes, "u_lam", [128, 10])
                for g_ in range(10):
                    k.act(lam.t[:, g_:g_ + 1], pcol("lam_%d" % g_), AF.Softplus, [pc], [lam], scale=-1.0)
                k.ts(lam.t[:], lam.t[:], -8.0, None, ALU.mult, None, [lam], [lam])
                u = [sb(pes, "u_u%d" % i, [128, T + 3]) for i in range(2)]
                gt = [sb(pes, "u_gt%d" % i, [128, T]) for i in range(2)]
                cv2 = [sb(pes, "u_cv%d" % i, [128, T]) for i in range(2)]
                cvb2 = [sb(pes, "u_cvb%d" % i, [128, T], BF16) for i in range(2)]
                rg2 = [sb(pes, "u_rg%d" % i, [128, T]) for i in range(2)]
                ig2 = [sb(pes, "u_ig%d" % i, [128, T]) for i in range(2)]
                aa = sb(pes, "u_aa", [128, T])
                bb = sb(pes, "u_bb", [128, T])
                hh_ = sb(pes, "u_hh", [128, T])
                ob = [sb(pes, "u_ob%d" % i, [128, T], BF16) for i in range(2)]
                pr = [ps(pes, "u_pr%d" % i, [128, 512]) for i in range(2)]
                pi = [ps(pes, "u_pi%d" % i, [128, 512]) for i in range(2)]
                items = [(s, g_) for s in range(NSEQ) for g_ in range(10)]

                def stageA(n):
                    s, g_ = items[n]
                    ts0 = s * T
                    ub, gtb = u[n % 2], gt[n % 2]
                    cv, cvb, rg, ig = cv2[n % 2], cvb2[n % 2], rg2[n % 2], ig2[n % 2]
                    k.memset(ub.t[:, 0:3], 0.0, [ub])
                    k.dma("sp", ub.t[:, 3:T + 3], recT[g_ * 128:(g_ + 1) * 128, ts0:ts0 + T], [dbuf("recT", s)], [ub])
                    k.dma("sp", gtb.t[:], gateT[g_ * 128:(g_ + 1) * 128, ts0:ts0 + T], [dbuf("gateT", s)], [gtb])
                    k.ts(cv.t[:], ub.t[:, 0:T], pcol("cw0_%d" % g_), pcol("cb_%d" % g_), ALU.mult, ALU.add, [ub, pc], [cv])
                    for tp in range(1, 4):
                        k.stt(cv.t[:], ub.t[:, tp:T + tp], pcol("cw%d_%d" % (tp, g_)), cv.t[:], ALU.mult, ALU.add, [ub, cv, pc], [cv])
                    k.copy(cvb.t[:], cv.t[:], [cv], [cvb], e="act")
                    for f in range(T // 512):
                        fs = slice(f * 512, (f + 1) * 512)
                        k.mm(pr[f % 2].t[:], wrg.t[:, g_, :], cvb.t[:, fs], True, True, [wrg, cvb], [pr[f % 2]])
                        k.act(rg.t[:, fs], pr[f % 2].t[:], AF.Sigmoid, [pr[f % 2], pc], [rg], bias=pcol("br_%d" % g_))
                        k.mm(pi[f % 2].t[:], wig.t[:, g_, :], cvb.t[:, fs], True, True, [wig, cvb], [pi[f % 2]])
                        k.act(ig.t[:, fs], pi[f % 2].t[:], AF.Sigmoid, [pi[f % 2], pc], [ig], bias=pcol("bi_%d" % g_))

                def stageB(n):
                    s, g_ = items[n]
                    ts0 = s * T
                    gtb, obb = gt[n % 2], ob[n % 2]
                    cv, rg, ig = cv2[n % 2], rg2[n % 2], ig2[n % 2]
                    k.act(aa.t[:], rg.t[:], AF.Exp, [rg, lam], [aa], scale=lam.t[:, g_:g_ + 1])
                    k.tt(bb.t[:], aa.t[:], aa.t[:], ALU.mult, [aa], [bb])
                    k.act(bb.t[:], bb.t[:], AF.Sqrt, [bb, epsc], [bb], scale=-1.0, bias=epsc.t[:, 2:3])
                    k.tt(ig.t[:], ig.t[:], cv.t[:], ALU.mult, [ig, cv], [ig])
                    k.tt(bb.t[:], bb.t[:], ig.t[:], ALU.mult, [bb, ig], [bb])
                    k.op("dve", lambda g, : g.tensor_tensor_scan(hh_.t[:], aa.t[:], bb.t[:], 0.0, ALU.mult, ALU.add), [aa, bb], [hh_])
                    k.tt(obb.t[:], hh_.t[:], gtb.t[:], ALU.mult, [hh_, gtb], [obb])
                    k.dma("sp", cat2T[g_ * 128:(g_ + 1) * 128, ts0:ts0 + T], obb.t[:], [obb], [dbuf("cat2T", s)])

                stageA(0)
                for n in range(len(items)):
                    if n + 1 < len(items):
                        stageA(n + 1)
                    stageB(n)
            k.barrier()

        if "lru" in phases:
            phase_lru()

        if "l1out" in phases:
            phase_out_moe(1, cat2T, "cat2T", 10, w_out_odd, x2, "x2", 3, x3, "x3", out_d, "out", "m1_")

        if debug:
            with ExitStack() as pes:
                a = sb(pes, "dbg_a", [128, 512], BF16)
                b = sb(pes, "dbg_b", [128, 512])
                for (src, dst, rows) in ((catT, catT_dbg, D), (cat2T, cat2T_dbg, LRU_W)):
                    for r in range(rows // 128):
                        for c in range(NTOK // 512):
                            k.dma("sp", a.t[:], src[r * 128:(r + 1) * 128, c * 512:(c + 1) * 512], (), [a])
                            k.copy(b.t[:], a.t[:], [a], [b], e="dve")
                            k.dma("sp", dst[r * 128:(r + 1) * 128, c * 512:(c + 1) * 512], b.t[:], [b], [a])
        k.barrier(skip_pre=False)
        print("kernel build: %d instructions" % k.n_inst)
    return nc


def host_inputs(inp):
    cols, pcarr = _pcols(inp)
    c = _consts()
    sel = np.zeros((16, 16, 128), np.float32)
    for e in range(16):
        sel[e, e, :] = 1.0
    f = lambda a: np.ascontiguousarray(np.asarray(a, np.float32))
    shared = {
        "gbc": f(np.stack([np.broadcast_to(inp["norm_mix"][0], (128, D)), np.broadcast_to(inp["norm_ffn"][0], (128, D)),
                           np.broadcast_to(inp["norm_mix"][1], (128, D)), np.broadcast_to(inp["norm_ffn"][1], (128, D))])),
        "pc": f(pcarr),
        "w_in_even": f(inp["w_in_even"][0]),
        "lora_w": f(np.concatenate([inp["decay_up"][0], inp["iclr_up"][0]], axis=0)),
        "gate_up": f(inp["gate_up"][0]),
        "w_out_even": f(inp["w_out_even"][0]),
        "w_in_odd": f(inp["w_in_odd"][0]),
        "w_rgate": f(inp["w_rgate"][0]),
        "w_igate": f(inp["w_igate"][0]),
        "w_out_odd": f(inp["w_out_odd"][0]),
        "w_router": f(np.concatenate([inp["w_group"], inp["w_erouter"]], axis=2)),
        "b_router": f(np.broadcast_to(np.concatenate([inp["b_group"], inp["b_erouter"]], axis=1)[:, None, :], (2, 128, 20))),
        "exp_w_gate": f(inp["exp_w_gate"]),
        "exp_w_up": f(inp["exp_w_up"]),
        "exp_w_down": f(inp["exp_w_down"]),
        "ident": c["ident"], "blockones": c["blockones"], "ones": c["ones"], "triu": c["triu"],
        "maskrel": f(c["maskrel"]), "m64": f(c["m64"]), "sel": sel,
    }
    return cols, pcarr.shape[1], shared


def kernel(**inputs):
    inp = {k_: np.asarray(v) for k_, v in inputs.items()}
    x = inp["x"]
    B, T, _ = x.shape
    nseq = B // NCORES
    cols, npc, shared = host_inputs(inp)
    nc = build(nseq, T, cols, npc)
    in_maps = []
    for c in range(NCORES):
        m = dict(shared)
        m["x"] = np.ascontiguousarray(x[c * nseq:(c + 1) * nseq].reshape(nseq * T, D), dtype=np.float32)
        in_maps.append(m)
    res = run_bass_kernel_spmd(nc, in_maps, core_ids=list(range(NCORES)))
    out = np.concatenate([np.asarray(r["out"]).reshape(nseq, T, D) for r in res.results], axis=0)
    return out.astype(np.float32)
```

```python
import os
import numpy as np
from contextlib import ExitStack
import concourse.bass as bass
import concourse.mybir as mybir
from concourse.bass_utils import run_bass_kernel_spmd

F32 = mybir.dt.float32
BF16 = mybir.dt.bfloat16
AF = mybir.ActivationFunctionType
ALU = mybir.AluOpType
AX = mybir.AxisListType

D = 1024
NCORES = 8
A_COLS = 1824
EVEN_COLS = 3360
LRU_W = 1280
NEXP = 16
DEXP = 512
DECAY_C = 0.6065306597126334
RMS_EPS = 1e-6
GN_EPS = 64e-5
SEM_LIMIT = 30000


class Buf:
    __slots__ = ("t", "w", "r")

    def __init__(self, t):
        self.t = t
        self.w = None
        self.r = {}


class MBuf:
    __slots__ = ("t", "ch")

    def __init__(self, t, n=2):
        self.t = t
        self.ch = [Buf(t) for _ in range(n)]


def _flat(bs):
    out = []
    for b in bs:
        if isinstance(b, MBuf):
            out.extend(b.ch)
        else:
            out.append(b)
    return out


class K:
    def __init__(self, nc, es):
        self.nc = nc
        self.es = es
        self.eng = {"pe": nc.tensor, "act": nc.scalar, "dve": nc.vector, "pool": nc.gpsimd, "sp": nc.sync}
        self.sems = {}
        self.epoch = {e: 0 for e in self.eng}
        self.cnt = {}
        self.waited = {e: {} for e in self.eng}
        for e in self.eng:
            self._new_epoch(e, first=True)
        self.ndslots = {"sp": 6, "pool": 6, "act": 2, "pre": 4}
        self.qeng = {"sp": "sp", "pool": "pool", "act": "act", "pre": "pool"}
        self.dslot = {q: 0 for q in self.ndslots}
        for q, n in self.ndslots.items():
            for i in range(n):
                key = ("d", q, i)
                self.sems[key] = es.enter_context(nc.semaphore("d_%s_%d" % (q, i)))
                self.cnt[key] = 0
        self.n_inst = 0

    def _new_epoch(self, e, first=False):
        if not first:
            self.epoch[e] += 1
        key = (e, self.epoch[e])
        self.sems[key] = self.es.enter_context(self.nc.semaphore("c_%s_%d" % (e, self.epoch[e])))
        self.cnt[key] = 0

    def _wait(self, e, deps):
        for key, val in deps.items():
            if self.waited[e].get(key, 0) < val:
                self.eng[e].wait_ge(self.sems[key], val)
                self.waited[e][key] = val

    def _deps(self, e, reads, writes):
        deps = {}

        def add(kv):
            if kv is None:
                return
            key, val = kv
            if deps.get(key, 0) < val:
                deps[key] = val

        for b in reads:
            add(b.w)
        for b in writes:
            if not (e == "pe" and b.w is not None and b.w[0][0] == "pe" and not b.r):
                add(b.w)
            for key, val in b.r.items():
                add((key, val))
        return deps

    def op(self, e, fn, reads=(), writes=()):
        reads, writes = _flat(reads), _flat(writes)
        self._wait(e, self._deps(e, reads, writes))
        inst = fn(self.eng[e])
        key = (e, self.epoch[e])
        self.cnt[key] += 1
        val = self.cnt[key]
        inst.then_inc(self.sems[key], 1)
        self.n_inst += 1
        for b in reads:
            if b.r.get(key, 0) < val:
                b.r[key] = val
        for b in writes:
            b.w = (key, val)
            b.r = {}
        if val >= SEM_LIMIT:
            self._new_epoch(e)

    def dma(self, qc, out, in_, reads=(), writes=()):
        reads, writes = _flat(reads), _flat(writes)
        q = self.qeng[qc]
        self._wait(q, self._deps(q, reads, writes))
        i = self.dslot[qc]
        self.dslot[qc] = (i + 1) % self.ndslots[qc]
        key = ("d", qc, i)
        if self.cnt[key] > 0 and self.waited[q].get(key, 0) < self.cnt[key]:
            self.eng[q].wait_ge(self.sems[key], self.cnt[key])
            self.waited[q][key] = self.cnt[key]
        self.eng[q].dma_start(out=out, in_=in_).then_inc(self.sems[key], 16)
        self.cnt[key] += 16
        val = self.cnt[key]
        self.n_inst += 1
        for b in reads:
            if b.r.get(key, 0) < val:
                b.r[key] = val
        for b in writes:
            b.w = (key, val)
            b.r = {}

    def barrier(self, engines=None, skip_pre=True):
        engines = engines or list(self.eng)
        deps = {key: val for key, val in self.cnt.items() if val > 0 and not (skip_pre and key[0] == "d" and key[1] == "pre")}
        for e in engines:
            self._wait(e, deps)

    def mm(self, out, lhsT, rhs, start, stop, reads, writes):
        self.op("pe", lambda g: g.matmul(out, lhsT, rhs, start=start, stop=stop), reads, writes)

    def tr(self, out, in_, ident, reads, writes):
        self.op("pe", lambda g: g.transpose(out, in_, ident), reads, writes)

    def act(self, out, in_, func, reads, writes, bias=None, scale=None, e="act"):
        kw = {}
        if bias is not None:
            kw["bias"] = bias
        if scale is not None:
            kw["scale"] = scale
        self.op(e, lambda g: g.activation(out=out, in_=in_, func=func, **kw), reads, writes)

    def tt(self, out, in0, in1, op, reads, writes, e="dve"):
        self.op(e, lambda g: g.tensor_tensor(out=out, in0=in0, in1=in1, op=op), reads, writes)

    def ts(self, out, in0, s1, s2, op0, op1, reads, writes, e="dve"):
        if op1 is None:
            self.op(e, lambda g: g.tensor_scalar(out, in0, s1, None, op0), reads, writes)
        else:
            self.op(e, lambda g: g.tensor_scalar(out, in0, s1, s2, op0, op1), reads, writes)

    def stt(self, out, in0, scalar, in1, op0, op1, reads, writes, e="dve"):
        self.op(e, lambda g: g.scalar_tensor_tensor(out=out, in0=in0, scalar=scalar, in1=in1, op0=op0, op1=op1), reads, writes)

    def rsqrt(self, out, in_, scale, bias, reads, writes, floor=None):
        kw = {"scale": scale}
        if bias is not None:
            kw["bias"] = bias
        self.op("act", lambda g: g.activation(out=out, in_=in_, func=AF.Sqrt, **kw), reads, writes)
        if floor is not None:
            self.op("dve", lambda g: g.tensor_scalar(out, out, floor, None, ALU.max), writes, writes)
        self.op("dve", lambda g: g.reciprocal(out, out), writes, writes)

    def copy(self, out, in_, reads, writes, e="dve"):
        if e == "act":
            self.op(e, lambda g: g.activation(out=out, in_=in_, func=AF.Copy), reads, writes)
        else:
            self.op(e, lambda g: g.tensor_copy(out, in_), reads, writes)

    def memset(self, ap, val, writes, e="dve"):
        self.op(e, lambda g: g.memset(ap, val), (), writes)


IN_CHUNKS = [(i * 128, 128) for i in range(12)] + [(1536, 128), (1664, 128), (1792, 32)] + \
            [(A_COLS + i * 128, 128) for i in range(12)]


def _pcols(inp):
    cols = {}
    arrs = []

    def add(name, v):
        v = np.asarray(v, np.float32).reshape(-1)
        col = np.zeros(128, np.float32)
        col[: v.shape[0]] = v
        cols[name] = len(arrs)
        arrs.append(col)

    mu = inp["mu_a"][0]
    for ci, (c0, m) in enumerate(IN_CHUNKS[:15]):
        add("mu%d" % ci, mu[c0:c0 + m])
    for p in range(4):
        sl = slice(p * 128, (p + 1) * 128)
        add("w0_%d" % p, inp["w0"][0][sl])
        add("a0_%d" % p, inp["a0"][0][sl])
        add("kk_%d" % p, inp["k_k"][0][sl])
        add("ka_%d" % p, inp["k_a"][0][sl])
        add("rk_%d" % p, inp["r_k"][0].reshape(-1)[sl])
        add("gnw_%d" % p, inp["gn_w"][0][sl])
        add("gnb_%d" % p, inp["gn_b"][0][sl])
    add("qg", np.tile(inp["q_norm_g"][0], 2))
    add("kg", np.tile(inp["k_norm_g"][0], 2))
    for g in range(10):
        sl = slice(g * 128, (g + 1) * 128)
        for t in range(4):
            add("cw%d_%d" % (t, g), inp["conv_w"][0][t][sl])
        add("cb_%d" % g, inp["conv_b"][0][sl])
        add("br_%d" % g, inp["b_rgate"][0][sl])
        add("bi_%d" % g, inp["b_igate"][0][sl])
        add("lam_%d" % g, inp["lru_lambda"][0][sl])
    return cols, np.stack(arrs, axis=1).copy()


def _consts():
    j = np.arange(128)
    c = {}
    c["ident"] = np.eye(128, dtype=np.float32)
    bo = np.zeros((128, 128), np.float32)
    bo[:64, :64] = 1.0
    bo[64:, 64:] = 1.0
    c["blockones"] = bo
    c["ones"] = np.ones((128, 128), np.float32)
    c["triu"] = (j[:, None] >= j[None, :]).astype(np.float32)
    t = np.arange(512)
    c["maskrel"] = np.stack([((r * 128 + j[:, None]) < t[None, :]).astype(np.float32) for r in range(4)], axis=1)
    jj = np.arange(64)
    m = np.zeros((64, 4, 64), np.float32)
    m[:, 0] = (jj[:, None] < jj[None, :])
    m[:, 1] = (jj[:, None] <= jj[None, :])
    m[:, 2] = (jj[None, :] < jj[:, None])
    m[:, 3] = np.eye(64)
    c["m64"] = m
    return c


def build(NSEQ, T, pcol_idx, npc, phases=("l0in", "rwkv", "attn", "l0out", "l1in", "lru", "l1out"), debug=False):
    NTOK = NSEQ * T
    NT512 = NTOK // 512
    nc = bass.Bass("TRN2", target_bir_lowering=False)
    dbgkind = "ExternalOutput" if debug else "Internal"

    def din(name, shape, dt=F32):
        return nc.dram_tensor(name, list(shape), dt, kind="ExternalInput").ap()

    x_in = din("x", [NTOK, D])
    out_d = nc.dram_tensor("out", [NTOK, D], F32, kind="ExternalOutput").ap()
    gbc_d = din("gbc", [4, 128, D])
    pc_d = din("pc", [128, npc])
    w_in_even = din("w_in_even", [D, EVEN_COLS])
    lora_w_d = din("lora_w", [128, 512])
    gate_up_d = din("gate_up", [160, 512])
    w_out_even = din("w_out_even", [D, D])
    w_in_odd = din("w_in_odd", [D, 2 * LRU_W])
    w_rgate = din("w_rgate", [10, 128, 128])
    w_igate = din("w_igate", [10, 128, 128])
    w_out_odd = din("w_out_odd", [LRU_W, D])
    w_router = din("w_router", [2, D, 20])
    b_router = din("b_router", [2, 128, 20])
    exp_w_gate = din("exp_w_gate", [2, NEXP, D, DEXP])
    exp_w_up = din("exp_w_up", [2, NEXP, D, DEXP])
    exp_w_down = din("exp_w_down", [2, NEXP, DEXP, D])
    c_ident = din("ident", [128, 128])
    c_blockones = din("blockones", [128, 128])
    c_ones = din("ones", [128, 128])
    c_triu = din("triu", [128, 128])
    c_maskrel = din("maskrel", [128, 4, 512])
    c_m64 = din("m64", [64, 4, 64])
    c_sel = din("sel", [16, 16, 128])

    projT = nc.dram_tensor("projT", [EVEN_COLS, NTOK], F32, kind=dbgkind).ap()
    catT = nc.dram_tensor("catT", [D, NTOK], BF16, kind="Internal").ap()
    x1 = nc.dram_tensor("x1", [NTOK, D], F32, kind=dbgkind).ap()
    x2 = nc.dram_tensor("x2", [NTOK, D], F32, kind=dbgkind).ap()
    x3 = nc.dram_tensor("x3", [NTOK, D], F32, kind=dbgkind).ap()
    gateT = nc.dram_tensor("gateT", [LRU_W, NTOK], F32, kind="Internal").ap()
    recT = nc.dram_tensor("recT", [LRU_W, NTOK], F32, kind="Internal").ap()
    cat2T = nc.dram_tensor("cat2T", [LRU_W, NTOK], BF16, kind="Internal").ap()
    if debug:
        catT_dbg = nc.dram_tensor("catT_dbg", [D, NTOK], F32, kind="ExternalOutput").ap()
        cat2T_dbg = nc.dram_tensor("cat2T_dbg", [LRU_W, NTOK], F32, kind="ExternalOutput").ap()

    wgu_bf = nc.dram_tensor("wgu_bf", [2, NEXP, 128, 2, 8, DEXP], BF16, kind="Internal").ap()
    wdn_bf = nc.dram_tensor("wdn_bf", [2, NEXP, 128, 4, D], BF16, kind="Internal").ap()
    PC = pcol_idx
    dbufs = {}
    dump_list = []

    def dump(k, name, buf, ap, shape):
        if not debug:
            return
        d = nc.dram_tensor("dump_" + name, list(shape), F32, kind="ExternalOutput").ap()
        k.dma("sp", d, ap, [buf], [])

    def dbuf(name, idx):
        b = dbufs.get((name, idx))
        if b is None:
            b = Buf(None)
            dbufs[(name, idx)] = b
        return b

    with ExitStack() as es:
        k = K(nc, es)

        def sb(es_, name, shape, dt=F32):
            return Buf(es_.enter_context(nc.sbuf_tensor("sb_" + name, list(shape), dt)))

        def ps(es_, name, shape, dt=F32):
            return Buf(es_.enter_context(nc.psum_tensor("ps_" + name, list(shape), dt)))

        pc = sb(es, "pc", [128, npc])
        ident = sb(es, "ident", [128, 128])
        identb = sb(es, "identb", [128, 128], BF16)
        blockones = sb(es, "blockones", [128, 128])
        k.dma("sp", pc.t[:], pc_d[:, :], (), [pc])
        k.dma("sp", ident.t[:], c_ident[:, :], (), [ident])
        k.dma("pool", identb.t[:], c_ident[:, :], (), [identb])
        k.dma("sp", blockones.t[:], c_blockones[:, :], (), [blockones])

        epsc = sb(es, "epsc", [128, 4])
        k.memset(epsc.t[:, 0:1], RMS_EPS, [epsc])
        k.memset(epsc.t[:, 1:2], GN_EPS, [epsc])
        k.memset(epsc.t[:, 2:3], 1.0, [epsc])

        def pcol(name, rows=128):
            i = PC[name]
            return pc.t[0:rows, i:i + 1]

        def rmsnorm_T(xt, gb, hT, j, sq, ssb, hb, ptr):
            k.tt(sq.t[:], xt.t[:], xt.t[:], ALU.mult, [xt], [sq])
            k.op("dve", lambda g: g.reduce_sum(out=ssb.t[:, 0:1], in_=sq.t[:], axis=AX.X), [sq], [ssb])
            k.rsqrt(ssb.t[:, 2:3], ssb.t[:, 0:1], 1.0 / D, epsc.t[:, 0:1], [ssb, epsc], [ssb])
            k.stt(hb.t[:], xt.t[:], ssb.t[:, 2:3], gb.t[:], ALU.mult, ALU.mult, [xt, ssb, gb], [hb])
            for kc in range(8):
                k.tr(ptr.t[:, kc * 128:(kc + 1) * 128], hb.t[:, kc * 128:(kc + 1) * 128], identb.t[:], [hb, identb], [ptr])
            k.copy(hT.t[:, :, j * 128:(j + 1) * 128], ptr.t[:].rearrange("p (c t) -> p c t", t=128), [ptr], [hT], e="act")

        def precast(layer):
            for e in range(NEXP):
                wb = dbuf("wbf", (layer, e))
                k.dma("pre", wgu_bf[layer, e, :, 0, :, :], exp_w_gate[layer, e].rearrange("(c p) f -> p c f", p=128), (), [wb])
                k.dma("pre", wgu_bf[layer, e, :, 1, :, :], exp_w_up[layer, e].rearrange("(c p) f -> p c f", p=128), (), [wb])
                k.dma("pre", wdn_bf[layer, e, :, :, :], exp_w_down[layer, e].rearrange("(c p) d -> p c d", p=128), (), [wb])

        def phase_in(x_src, x_name, gidx, w_d, ncols, chunks, dst_fn, tagp, after_loads=None):
            with ExitStack() as pes:
                wsb = sb(pes, tagp + "w", [128, 8, ncols], BF16)
                gb = sb(pes, tagp + "gb", [128, D])
                k.dma("sp", gb.t[:], gbc_d[gidx], (), [gb])
                for kc in range(8):
                    k.dma("pool", wsb.t[:, kc, :], w_d[kc * 128:(kc + 1) * 128, :], (), [wsb])
                if after_loads is not None:
                    after_loads()
                xts = [sb(pes, tagp + "xt%d" % i, [128, D]) for i in range(4)]
                sq = sb(pes, tagp + "sq", [128, D])
                ssbs = [sb(pes, tagp + "ss%d" % i, [128, 4]) for i in range(4)]
                hbs = [sb(pes, tagp + "hb%d" % i, [128, D], BF16) for i in range(4)]
                hTs = [sb(pes, tagp + "hT%d" % i, [128, 8, 512], BF16) for i in range(2)]
                ptr = ps(pes, tagp + "ptr", [128, 1024], BF16)
                pps = [ps(pes, tagp + "pp%d" % i, [128, 512]) for i in range(3)]
                stg = [sb(pes, tagp + "stg%d" % i, [128, 512]) for i in range(3)]

                def A1(t, j):
                    xt, ssb, hb = xts[j], ssbs[j], hbs[j]
                    r0 = t * 512 + j * 128
                    k.dma("sp", xt.t[:], x_src[r0:r0 + 128, :], [dbuf(x_name, r0 // 128)], [xt])
                    k.tt(sq.t[:], xt.t[:], xt.t[:], ALU.mult, [xt], [sq])
                    k.op("dve", lambda g: g.reduce_sum(out=ssb.t[:, 0:1], in_=sq.t[:], axis=AX.X), [sq], [ssb])
                    k.rsqrt(ssb.t[:, 2:3], ssb.t[:, 0:1], 1.0 / D, epsc.t[:, 0:1], [ssb, epsc], [ssb])
                    k.stt(hb.t[:], xt.t[:], ssb.t[:, 2:3], gb.t[:], ALU.mult, ALU.mult, [xt, ssb, gb], [hb])

                def A2(t, j):
                    hb, hT = hbs[j], hTs[t % 2]
                    for kc in range(8):
                        k.tr(ptr.t[:, kc * 128:(kc + 1) * 128], hb.t[:, kc * 128:(kc + 1) * 128], identb.t[:], [hb, identb], [ptr])
                    k.copy(hT.t[:, :, j * 128:(j + 1) * 128], ptr.t[:].rearrange("p (c t) -> p c t", t=128), [ptr], [hT], e="act")

                for j in range(4):
                    A1(0, j)
                    A2(0, j)
                n = 0
                nch = len(chunks)
                inj = {(jj + 1) * nch // 5: jj for jj in range(4)}
                for tt_ in range(NT512):
                    hT = hTs[tt_ % 2]
                    nxt = tt_ + 1 < NT512
                    if nxt:
                        for j in range(4):
                            A1(tt_ + 1, j)
                    for ci, (c0, m) in enumerate(chunks):
                        pp = pps[n % 3]
                        st = stg[n % 3]
                        n += 1
                        for kc in range(8):
                            k.mm(pp.t[0:m, :], wsb.t[:, kc, c0:c0 + m], hT.t[:, kc, :], kc == 0, kc == 7, [wsb, hT], [pp])
                        dst_fn(ci, c0, m, tt_, pp, st)
                        if nxt and ci in inj:
                            A2(tt_ + 1, inj[ci])
            k.barrier()

        def l0_dst(ci, c0, m, tt_, pp, st):
            k.copy(st.t[0:m, :], pp.t[0:m, :], [pp], [st], e="act")
            k.dma("sp", projT[c0:c0 + m, tt_ * 512:(tt_ + 1) * 512], st.t[0:m, :], [st], [dbuf("projT", tt_ * 512 // T)])

        if "l0in" in phases:
            phase_in(x_in, "x", 0, w_in_even, EVEN_COLS, IN_CHUNKS, l0_dst, "a_", after_loads=(lambda: (precast(0), precast(1) if "l1out" in phases else None)) if "l0out" in phases else None)

        def phase_rwkv():
            with ExitStack() as pes:
                SEG = 256
                lw_t = sb(pes, "r_lw", [128, 512])
                gu1 = sb(pes, "r_gu1", [128, 512])
                gu2 = sb(pes, "r_gu2", [32, 512])
                m64 = sb(pes, "r_m64", [64, 4, 64])
                onesf = sb(pes, "r_ones", [128, 64])
                k.dma("sp", lw_t.t[:], lora_w_d[:, :], (), [lw_t])
                k.dma("sp", gu1.t[:], gate_up_d[0:128, :], (), [gu1])
                k.dma("sp", gu2.t[:], gate_up_d[128:160, :], (), [gu2])
                k.dma("sp", m64.t[:], c_m64[:, :, :], (), [m64])
                k.memset(onesf.t[:], 1.0, [onesf])
                raw = {nm: sb(pes, "r_raw_" + nm, [128, 4, SEG + 1]) for nm in ("r", "k", "v")}
                rawl = sb(pes, "r_rawl", [128, 3, SEG + 1])
                sh = {nm: sb(pes, "r_sh_" + nm, [128, 4, SEG]) for nm in ("r", "k", "v")}
                shl = sb(pes, "r_shl", [128, 3, SEG])
                tmpA = sb(pes, "r_tmpA", [128, 4, SEG])
                tmpB = sb(pes, "r_tmpB", [128, 4, SEG])
                sg = sb(pes, "r_sg", [128, 4, SEG])
                aic = sb(pes, "r_aic", [128, 4, SEG])
                gg = sb(pes, "r_g", [128, 4, SEG])
                kk = sb(pes, "r_kk", [128, 4, SEG])
                kmod = sb(pes, "r_kmod", [128, 4, SEG])
                bonus = sb(pes, "r_bonus", [128, 4, SEG])
                cs = sb(pes, "r_cs", [128, 4, SEG])
                Ep = sb(pes, "r_Ep", [128, 4, SEG])
                En = sb(pes, "r_En", [128, 4, SEG])
                Em = sb(pes, "r_Em", [128, 4, SEG])
                MD = BF16
                rt = sb(pes, "r_rt", [128, 4, SEG], MD)
                kt = sb(pes, "r_kt", [128, 4, SEG], MD)
                bt = sb(pes, "r_bt", [128, 4, SEG], MD)
                at = sb(pes, "r_at", [128, 4, SEG], MD)
                vb16 = sb(pes, "r_vb16", [128, 4, SEG], MD)
                PCp = sb(pes, "r_PCp", [128, 4, SEG // 64])
                PCH = sb(pes, "r_PCH", [64, 8, SEG // 64])
                yseg = sb(pes, "r_y", [128, 4, SEG])
                yab = sb(pes, "r_yab", [128, 4, SEG], BF16)
                def sbm(name, shape, dt=F32):
                    return MBuf(pes.enter_context(nc.sbuf_tensor("sb_" + name, list(shape), dt)))

                def psm(name, shape, dt=F32):
                    return MBuf(pes.enter_context(nc.psum_tensor("ps_" + name, list(shape), dt)))

                S = sbm("r_S", [64, 8, 64])
                Stmp = sbm("r_Stmp", [64, 8, 64])
                Sb = sbm("r_Sb", [64, 9, 64], MD)
                X = [sbm("r_X%d" % i, [64, 9, 64], MD) for i in range(2)]
                Y = [sbm("r_Y%d" % i, [64, 9, 64], MD) for i in range(2)]
                Z = [sbm("r_Z%d" % i, [64, 9, 64], MD) for i in range(2)]
                LakT = sbm("r_LakT", [64, 8, 64], MD)
                MrbT = sbm("r_MrbT", [64, 8, 64], MD)
                MrkT = sbm("r_MrkT", [64, 8, 64], MD)
                Vt = sbm("r_Vt", [64, 9, 64], MD)
                Btk = sbm("r_Btk", [64, 9, 64], MD)
                Ktk = sbm("r_Ktk", [64, 9, 64], MD)
                Wsb = sbm("r_Wsb", [64, 8, 64], MD)
                Usb = sbm("r_Usb", [64, 9, 64], MD)
                atH = sb(pes, "r_atH", [64, 8, SEG], MD)
                btH = sb(pes, "r_btH", [64, 8, SEG], MD)
                ktH = sb(pes, "r_ktH", [64, 8, SEG], MD)
                rtH = sb(pes, "r_rtH", [64, 8, SEG], MD)
                yH = sbm("r_yH", [64, 8, SEG])
                QG = [[ps(pes, "r_Q%d%d" % (g_, i), [128, 512]) for i in range(4)] for g_ in range(2)]
                P1, P2, P3, P4 = QG[0]

                def v864(b):
                    return b.t[0:64, :].rearrange("p (h j) -> p h j", j=64)

                def v8128(b):
                    return b.t[0:64, :].rearrange("p (h j) -> p h j", j=128)

                def v4128(b, rows=128):
                    return b.t[0:rows, 0:512].rearrange("p (h j) -> p h j", j=128)

                mU = m64.t[:, 0:1, :].to_broadcast([64, 4, 64])
                mUI = m64.t[:, 1:2, :].to_broadcast([64, 4, 64])
                mL = m64.t[:, 2:3, :].to_broadcast([64, 4, 64])
                mI = m64.t[:, 3:4, :].to_broadcast([64, 4, 64])

                def loads_shift(s, seg):
                    tok0 = s * T + seg * SEG
                    dep = [dbuf("projT", s)]
                    def load(dst_ap, dstbuf, r0, m):
                        if seg == 0:
                            k.memset(dst_ap[0:m, 0:1], 0.0, [dstbuf])
                            k.dma("sp", dst_ap[0:m, 1:SEG + 1], projT[r0:r0 + m, tok0:tok0 + SEG], dep, [dstbuf])
                        else:
                            k.dma("sp", dst_ap[0:m, 0:SEG + 1], projT[r0:r0 + m, tok0 - 1:tok0 + SEG], dep, [dstbuf])
                    for i, nm in enumerate(("r", "k", "v")):
                        for p in range(4):
                            load(raw[nm].t[:, p, :], raw[nm], i * 512 + p * 128, 128)
                    load(rawl.t[:, 0, :], rawl, 1536, 128)
                    load(rawl.t[:, 1, :], rawl, 1664, 128)
                    load(rawl.t[:, 2, :], rawl, 1792, 32)
                    def shift(dst, dbuf_, src, sbuf_, m, mucol):
                        k.tt(tmpA.t[0:m, 0, :], src[0:m, 0:SEG], src[0:m, 1:SEG + 1], ALU.subtract, [sbuf_], [tmpA])
                        k.stt(dst[0:m, :], tmpA.t[0:m, 0, :], pcol(mucol, m), src[0:m, 1:SEG + 1], ALU.mult, ALU.add, [tmpA, sbuf_, pc], [dbuf_])
                    for i, nm in enumerate(("r", "k", "v")):
                        for p in range(4):
                            shift(sh[nm].t[:, p, :], sh[nm], raw[nm].t[:, p, :], raw[nm], 128, "mu%d" % (i * 4 + p))
                    shift(shl.t[:, 0, :], shl, rawl.t[:, 0, :], rawl, 128, "mu12")
                    shift(shl.t[:, 1, :], shl, rawl.t[:, 1, :], rawl, 128, "mu13")
                    shift(shl.t[:, 2, :], shl, rawl.t[:, 2, :], rawl, 32, "mu14")

                seglist = [(s_, g_) for s_ in range(NSEQ) for g_ in range(T // SEG)]
                loads_shift(*seglist[0])
                for s in range(NSEQ):
                    k.memset(S.t[:], 0.0, [S])
                    k.memset(Sb.t[:], 0.0, [Sb])
                    for seg in range(T // SEG):
                        tok0 = s * T + seg * SEG
                        seg_idx = s * (T // SEG) + seg
                        STOP = int(os.environ.get("RWKV_STOP", "99"))
                        if STOP <= 0:
                            continue
                        k.act(shl.t[0:64, 0, :], shl.t[0:64, 0, :], AF.Tanh, [shl], [shl])
                        k.act(shl.t[:, 1, :], shl.t[:, 1, :], AF.Sigmoid, [shl], [shl])
                        k.act(shl.t[0:32, 2, :], shl.t[0:32, 2, :], AF.Sigmoid, [shl], [shl])
                        for p in range(4):
                            pch = slice(p * 128, (p + 1) * 128)
                            k.mm(P1.t[:, 0:SEG], lw_t.t[0:64, pch], shl.t[0:64, 0, :], True, True, [lw_t, shl], [P1])
                            k.act(sg.t[:, p, :], P1.t[:, 0:SEG], AF.Sigmoid, [P1, pc], [sg], bias=pcol("w0_%d" % p))
                            k.mm(P2.t[:, 0:SEG], lw_t.t[64:128, pch], shl.t[64:128, 0, :], True, True, [lw_t, shl], [P2])
                            k.act(aic.t[:, p, :], P2.t[:, 0:SEG], AF.Sigmoid, [P2, pc], [aic], bias=pcol("a0_%d" % p))
                            k.mm(P3.t[:, 0:SEG], gu1.t[:, pch], shl.t[:, 1, :], True, False, [gu1, shl], [P3])
                            k.mm(P3.t[:, 0:SEG], gu2.t[0:32, pch], shl.t[0:32, 2, :], False, True, [gu2, shl], [P3])
                            k.copy(gg.t[:, p, :], P3.t[:, 0:SEG], [P3], [gg], e="act")
                            k.ts(tmpA.t[:, p, :], sh["k"].t[:, p, :], pcol("kk_%d" % p), None, ALU.mult, None, [sh["k"], pc], [tmpA])
                            k.tt(tmpB.t[:, p, :], tmpA.t[:, p, :], tmpA.t[:, p, :], ALU.mult, [tmpA], [tmpB])
                            k.mm(P4.t[:, 0:SEG], blockones.t[:], tmpB.t[:, p, :], True, True, [blockones, tmpB], [P4])
                            k.rsqrt(tmpB.t[:, p, :], P4.t[:, 0:SEG], 1.0, None, [P4], [tmpB], floor=1e-12)
                            k.tt(kk.t[:, p, :], tmpA.t[:, p, :], tmpB.t[:, p, :], ALU.mult, [tmpA, tmpB], [kk])
                            k.ts(tmpA.t[:, p, :], aic.t[:, p, :], -1.0, pcol("ka_%d" % p), ALU.add, ALU.mult, [aic, pc], [tmpA])
                            k.stt(kmod.t[:, p, :], tmpA.t[:, p, :], 1.0, sh["k"].t[:, p, :], ALU.add, ALU.mult, [tmpA, sh["k"]], [kmod])
                            k.stt(tmpB.t[:, p, :], sh["r"].t[:, p, :], pcol("rk_%d" % p), kmod.t[:, p, :], ALU.mult, ALU.mult, [sh["r"], kmod, pc], [tmpB])
                            k.mm(P1.t[:, 0:SEG], blockones.t[:], tmpB.t[:, p, :], True, True, [blockones, tmpB], [P1])
                            k.tt(bonus.t[:, p, :], P1.t[:, 0:SEG], sh["v"].t[:, p, :], ALU.mult, [P1, sh["v"]], [bonus])
                            for c in range(SEG // 64):
                                ch = slice(c * 64, (c + 1) * 64)
                                k.op("dve", lambda g, p=p, ch=ch: g.tensor_tensor_scan(cs.t[:, p, ch], onesf.t[:, :], sg.t[:, p, ch], 0.0, ALU.mult, ALU.add), [sg, onesf], [cs])
                        if STOP <= 1:
                            continue
                        k.tt(tmpA.t[:], cs.t[:], sg.t[:], ALU.subtract, [cs, sg], [tmpA])
                        k.act(Ep.t[:], cs.t[:], AF.Exp, [cs], [Ep], scale=-DECAY_C)
                        k.act(En.t[:], cs.t[:], AF.Exp, [cs], [En], scale=DECAY_C)
                        k.act(Em.t[:], tmpA.t[:], AF.Exp, [tmpA], [Em], scale=-DECAY_C)
                        k.tt(rt.t[:], sh["r"].t[:], Ep.t[:], ALU.mult, [sh["r"], Ep], [rt])
                        k.tt(kt.t[:], kmod.t[:], En.t[:], ALU.mult, [kmod, En], [kt])
                        k.tt(tmpB.t[:], kk.t[:], aic.t[:], ALU.mult, [kk, aic], [tmpB])
                        k.tt(bt.t[:], tmpB.t[:], En.t[:], ALU.mult, [tmpB, En], [bt])
                        k.stt(at.t[:], kk.t[:], -1.0, Em.t[:], ALU.mult, ALU.mult, [kk, Em], [at])
                        k.copy(PCp.t[:], Ep.t[:].rearrange("p a (c j) -> p a c j", j=64)[:, :, :, 63], [Ep], [PCp], e="dve")
                        k.copy(vb16.t[:], sh["v"].t[:], [sh["v"]], [vb16], e="act")
                        if s == 0 and seg == 0:
                            for nm_, b_ in (("shr", sh["r"]), ("shk", sh["k"]), ("shv", sh["v"]), ("sg", sg), ("aic", aic), ("gg", gg), ("kk", kk),
                                            ("kmod", kmod), ("bonus", bonus), ("cs", cs), ("Ep", Ep), ("En", En), ("Em", Em),
                                            ):
                                dump(k, nm_, b_, b_.t[:], [128, 4, SEG])
                        if STOP <= 2:
                            continue
                        if seg_idx + 1 < len(seglist):
                            loads_shift(*seglist[seg_idx + 1])
                        def to_head(dstb, srcb):
                            dv = dstb.t[:].rearrange("q (p b) t -> q p b t", b=2)
                            k.dma("sp", dv[:, :, 0, :], srcb.t[0:64, :, :], [srcb], [dstb])
                            k.dma("sp", dv[:, :, 1, :], srcb.t[64:128, :, :], [srcb], [dstb])
                        to_head(atH, at)
                        to_head(btH, bt)
                        to_head(ktH, kt)
                        to_head(rtH, rt)
                        to_head(PCH, PCp)
                        identm = identb if MD == BF16 else ident

                        def chunk_gen(c, g):
                            ch = slice(c * 64, (c + 1) * 64)
                            H = slice(4 * g, 4 * g + 4)
                            hl = list(range(4 * g, 4 * g + 4))
                            bmap = {1: 0, 2: 1, 3: 2, 5: 0, 6: 1, 7: 2, 8: 3}
                            pb = {i: QG[g][bi] for i, bi in bmap.items()}
                            pv = {i: pb[i].t[0:64, 0:256].rearrange("q (h j) -> q h j", j=64) for i in bmap}

                            def T_(mb):
                                return mb.t[:, H, :]

                            def L2(mb, h):
                                return mb.t[:, h:h + 2, :].rearrange("q a b -> q (a b)")

                            def O2(i, j_):
                                return pb[i].t[0:128, j_ * 64:(j_ + 1) * 64]
                            Xg = [X[0].ch[g], X[1].ch[g]]
                            Yg = [Y[0].ch[g], Y[1].ch[g]]
                            Zg = [Z[0].ch[g], Z[1].ch[g]]
                            for j_, h in enumerate(hl):
                                k.mm(pv[1][:, j_, :], btH.t[:, h, ch], atH.t[:, h, ch], True, True, [btH, atH], [pb[1]])
                                k.mm(pv[2][:, j_, :], atH.t[:, h, ch], btH.t[:, h, ch], True, True, [btH, atH], [pb[2]])
                                k.mm(pv[3][:, j_, :], ktH.t[:, h, ch], atH.t[:, h, ch], True, True, [ktH, atH], [pb[3]])
                            k.tt(T_(X[0]), pv[1], mU, ALU.mult, [pb[1], m64], [Xg[0]])
                            k.tt(T_(Y[0]), pv[2], mL, ALU.mult, [pb[2], m64], [Yg[0]])
                            k.tt(T_(LakT), pv[3], mU, ALU.mult, [pb[3], m64], [LakT.ch[g]])
                            k.tt(T_(Z[0]), T_(X[0]), mI, ALU.add, [Xg[0], m64], [Zg[0]])
                            yield
                            b3 = QG[g][3]
                            b3v = b3.t[0:64, 0:256].rearrange("q (h j) -> q h j", j=64)
                            b3t = b3.t[:].bitcast(BF16) if MD == BF16 else b3.t[:]
                            b3tv = b3t[0:64, 0:256].rearrange("q (h j) -> q h j", j=64)

                            def side(idx):
                                if idx == 0:
                                    for j_, h in enumerate(hl):
                                        k.mm(b3v[:, j_, :], btH.t[:, h, ch], rtH.t[:, h, ch], True, True, [btH, rtH], [b3])
                                    k.tt(T_(MrbT), b3v, mUI, ALU.mult, [b3, m64], [MrbT.ch[g]])
                                elif idx == 1:
                                    for j_, h in enumerate(hl):
                                        k.mm(b3v[:, j_, :], ktH.t[:, h, ch], rtH.t[:, h, ch], True, True, [ktH, rtH], [b3])
                                    k.tt(T_(MrkT), b3v, mUI, ALU.mult, [b3, m64], [MrkT.ch[g]])
                                else:
                                    srcb, dstb = ((vb16, Vt), (bt, Btk), (kt, Ktk))[idx - 2]
                                    for pp in range(2):
                                        k.tr(b3t[0:64, pp * 128:(pp + 1) * 128], srcb.t[:, 2 * g + pp, ch], identm.t[:], [srcb, identm], [b3])
                                    k.copy(T_(dstb), b3tv, [b3], [dstb.ch[g]], e=("act" if idx != 3 else "dve"))

                            for j_, h in enumerate(hl):
                                k.mm(O2(1, j_), L2(Y[0], h), X[0].t[:, h, :], True, True, [Xg[0], Yg[0]], [pb[1]])
                                k.mm(O2(2, j_), L2(X[0], h), Y[0].t[:, h, :], True, True, [Xg[0], Yg[0]], [pb[2]])
                            side(0)
                            k.copy(T_(X[1]), pv[1], [pb[1]], [Xg[1]], e="act")
                            k.copy(T_(Y[1]), pv[2], [pb[2]], [Yg[1]], e="dve")
                            yield
                            xi, zi = 1, 0
                            for r in range(1, 6):
                                for j_, h in enumerate(hl):
                                    k.mm(O2(3, j_), L2(Y[xi], h), Z[zi].t[:, h, :], True, True, [Yg[xi], Zg[zi]], [pb[3]])
                                    if r < 5:
                                        k.mm(O2(2, j_), L2(X[xi], h), Y[xi].t[:, h, :], True, True, [Xg[xi], Yg[xi]], [pb[2]])
                                    if r < 4:
                                        k.mm(O2(1, j_), L2(Y[xi], h), X[xi].t[:, h, :], True, True, [Xg[xi], Yg[xi]], [pb[1]])
                                if r <= 4:
                                    side(r)
                                k.tt(T_(Z[1 - zi]), pv[3], T_(Z[zi]), ALU.add, [pb[3], Zg[zi]], [Zg[1 - zi]])
                                if r < 5:
                                    k.copy(T_(Y[1 - xi]), pv[2], [pb[2]], [Yg[1 - xi]], e="act")
                                if r < 4:
                                    k.copy(T_(X[1 - xi]), pv[1], [pb[1]], [Xg[1 - xi]], e="dve")
                                xi, zi = 1 - xi, 1 - zi
                                yield
                            Zf, Zfb = Z[zi], Zg[zi]
                            for j_, h in enumerate(hl):
                                k.mm(pv[5][:, j_, :], atH.t[:, h, ch], Sb.t[:, h, :], True, False, [atH, Sb.ch[g]], [pb[5]])
                                k.mm(pv[5][:, j_, :], LakT.t[:, h, :], Vt.t[:, h, :], False, True, [LakT.ch[g], Vt.ch[g]], [pb[5]])
                            k.copy(T_(Wsb), pv[5], [pb[5]], [Wsb.ch[g]], e="act")
                            yield
                            for j_, h in enumerate(hl):
                                k.mm(O2(6, j_), L2(Zf, h), Wsb.t[:, h, :], True, True, [Zfb, Wsb.ch[g]], [pb[6]])
                            k.copy(T_(Usb), pv[6], [pb[6]], [Usb.ch[g]], e="dve")
                            yield
                            for j_, h in enumerate(hl):
                                k.mm(O2(7, j_), L2(Sb, h), rtH.t[:, h, ch], True, False, [Sb.ch[g], rtH], [pb[7]])
                                k.mm(O2(7, j_), L2(Usb, h), MrbT.t[:, h, :], False, False, [Usb.ch[g], MrbT.ch[g]], [pb[7]])
                                k.mm(O2(7, j_), L2(Vt, h), MrkT.t[:, h, :], False, True, [Vt.ch[g], MrkT.ch[g]], [pb[7]])
                            for j_, h in enumerate(hl):
                                k.mm(O2(8, j_), L2(Btk, h), Usb.t[:, h, :], True, False, [Btk.ch[g], Usb.ch[g]], [pb[8]])
                                k.mm(O2(8, j_), L2(Ktk, h), Vt.t[:, h, :], False, True, [Ktk.ch[g], Vt.ch[g]], [pb[8]])
                            k.copy(yH.t[:, H, ch], pv[7], [pb[7]], [yH.ch[g]], e="act")
                            pcc = PCH.t[:, H, c:c + 1].to_broadcast([64, 4, 64])
                            k.tt(T_(Stmp), T_(S), pv[8], ALU.add, [S.ch[g], pb[8]], [Stmp.ch[g]])
                            k.tt(T_(S), T_(Stmp), pcc, ALU.mult, [Stmp.ch[g], PCH], [S.ch[g]])
                            k.copy(T_(Sb), T_(S), [S.ch[g]], [Sb.ch[g]], e="act")
                            yield

                        for c in range(SEG // 64):
                            gens = [chunk_gen(c, 0), chunk_gen(c, 1)]
                            alive = [True, True]
                            while any(alive):
                                for gi in range(2):
                                    if alive[gi]:
                                        try:
                                            next(gens[gi])
                                        except StopIteration:
                                            alive[gi] = False
                        yv = yH.t[:].rearrange("q (p b) t -> q p b t", b=2)
                        k.dma("sp", yseg.t[0:64, :, :], yv[:, :, 0, :], [yH], [yseg])
                        k.dma("sp", yseg.t[64:128, :, :], yv[:, :, 1, :], [yH], [yseg])
                        if s == 0 and seg == 0:
                            dump(k, "yseg", yseg, yseg.t[:], [128, 4, SEG])
                        for p in range(4):
                            k.mm(P1.t[:, 0:SEG], blockones.t[:], yseg.t[:, p, :], True, True, [blockones, yseg], [P1])
                            k.stt(tmpA.t[:, p, :], P1.t[:, 0:SEG], -1.0 / 64, yseg.t[:, p, :], ALU.mult, ALU.add, [P1, yseg], [tmpA])
                            k.tt(tmpB.t[:, p, :], tmpA.t[:, p, :], tmpA.t[:, p, :], ALU.mult, [tmpA], [tmpB])
                            k.mm(P2.t[:, 0:SEG], blockones.t[:], tmpB.t[:, p, :], True, True, [blockones, tmpB], [P2])
                            k.rsqrt(tmpB.t[:, p, :], P2.t[:, 0:SEG], 1.0 / 64, epsc.t[:, 1:2], [P2, epsc], [tmpB])
                            k.tt(tmpA.t[:, p, :], tmpA.t[:, p, :], tmpB.t[:, p, :], ALU.mult, [tmpA, tmpB], [tmpA])
                            k.ts(tmpA.t[:, p, :], tmpA.t[:, p, :], pcol("gnw_%d" % p), pcol("gnb_%d" % p), ALU.mult, ALU.add, [tmpA, pc], [tmpA])
                            k.tt(tmpA.t[:, p, :], tmpA.t[:, p, :], bonus.t[:, p, :], ALU.add, [tmpA, bonus], [tmpA])
                            k.tt(yab.t[:, p, :], tmpA.t[:, p, :], gg.t[:, p, :], ALU.mult, [tmpA, gg], [yab])
                            k.dma("sp", catT[p * 128:(p + 1) * 128, tok0:tok0 + SEG], yab.t[:, p, :], [yab], [dbuf("catT", s)])
            k.barrier()

        if "rwkv" in phases:
            phase_rwkv()

        def phase_attn():
            with ExitStack() as pes:
                triu = sb(pes, "s_triu", [128, 128], BF16)
                onesb = sb(pes, "s_ones", [128, 128], BF16)
                maskb = sb(pes, "s_maskb", [128, 4, 512], BF16)
                k.dma("pool", triu.t[:], c_triu[:, :], (), [triu])
                k.dma("pool", onesb.t[:], c_ones[:, :], (), [onesb])
                k.dma("pool", maskb.t[:], c_maskrel[:, :, :], (), [maskb])
                qraw = sb(pes, "s_qraw", [128, T])
                kraw = sb(pes, "s_kraw", [128, T])
                vb = sb(pes, "s_vb", [128, T], BF16)
                sq = sb(pes, "s_sq", [128, T])
                rs = sb(pes, "s_rs", [128, T])
                qn2 = [sb(pes, "s_qn%d" % i, [128, T], BF16) for i in range(2)]
                kn2 = [sb(pes, "s_kn%d" % i, [128, T], BF16) for i in range(2)]
                vtok2 = [sb(pes, "s_vtok%d" % i, [128, T // 128, 128], BF16) for i in range(2)]
                ND = 6
                spb = [sb(pes, "s_sp%d" % i, [128, 512], BF16) for i in range(ND)]
                zsb = [sb(pes, "s_zs%d" % i, [128, 512]) for i in range(ND)]
                t1 = [sb(pes, "s_t1%d" % i, [128, 512]) for i in range(ND)]
                att = [sb(pes, "s_att%d" % i, [128, 512], BF16) for i in range(ND)]
                ob = [sb(pes, "s_ob%d" % i, [64, 512], BF16) for i in range(2)]
                PZ = [ps(pes, "s_PZ%d" % i, [128, 512]) for i in range(2)]
                PT = [ps(pes, "s_PT%d" % i, [128, 512]) for i in range(2)]
                PR = [ps(pes, "s_PR%d" % i, [128, 512]) for i in range(2)]
                PV = [ps(pes, "s_PV%d" % i, [128, 512]) for i in range(2)]
                Rb = sb(pes, "s_R", [128, 512])
                qnn2 = [sb(pes, "s_qnn%d" % i, [128, T], BF16) for i in range(2)]

                def prep(s, p, par):
                    dep = [dbuf("projT", s)]
                    ts0 = s * T
                    qn, kn, vtok = qn2[par], kn2[par], vtok2[par]
                    k.dma("sp", qraw.t[:], projT[A_COLS + p * 128:A_COLS + (p + 1) * 128, ts0:ts0 + T], dep, [qraw])
                    k.dma("sp", kraw.t[:], projT[A_COLS + 512 + p * 128:A_COLS + 512 + (p + 1) * 128, ts0:ts0 + T], dep, [kraw])
                    k.dma("pool", vb.t[:], projT[A_COLS + 1024 + p * 128:A_COLS + 1024 + (p + 1) * 128, ts0:ts0 + T], dep, [vb])
                    for (rawb, outb, gname) in ((qraw, qn, "qg"), (kraw, kn, "kg")):
                        k.tt(sq.t[:], rawb.t[:], rawb.t[:], ALU.mult, [rawb], [sq])
                        for f in range(T // 512):
                            fs = slice(f * 512, (f + 1) * 512)
                            pz = PZ[f % 2]
                            k.mm(pz.t[:], blockones.t[:], sq.t[:, fs], True, True, [blockones, sq], [pz])
                            k.rsqrt(rs.t[:, fs], pz.t[:], 1.0 / 64, epsc.t[:, 0:1], [pz, epsc], [rs])
                        k.stt(outb.t[:], rawb.t[:], pcol(gname), rs.t[:], ALU.mult, ALU.mult, [rawb, rs, pc], [outb])
                    k.ts(qnn2[par].t[:], qn.t[:], -0.125, None, ALU.mult, None, [qn], [qnn2[par]])
                    for kb in range(T // 128):
                        pz = PT[kb % 2]
                        pzb = pz.t[:].bitcast(BF16)
                        k.tr(pzb[:, 0:128], vb.t[:, kb * 128:(kb + 1) * 128], identb.t[:], [vb, identb], [pz])
                        k.copy(vtok.t[:, kb, :], pzb[:, 0:128], [pz], [vtok], e="act")

                pairs = [(s, p) for s in range(NSEQ) for p in range(4)]
                steps = []
                for pi, (s, p) in enumerate(pairs):
                    for hh in range(2):
                        for QT in range(T // 512):
                            kb_hi = QT * 4 + 3
                            for kb in range(kb_hi, -1, -1):
                                steps.append((pi, s, p, hh, QT, kb, kb_hi))
                qtidx = {}
                for st in steps:
                    key = (st[0], st[3], st[4])
                    if key not in qtidx:
                        qtidx[key] = len(qtidx)

                def stepA(n):
                    pi, s, p, hh, QT, kb, kb_hi = steps[n]
                    R = slice(hh * 64, hh * 64 + 64)
                    qs = slice(QT * 512, (QT + 1) * 512)
                    qn, kn = qn2[pi % 2], kn2[pi % 2]
                    pz, sp_, zs_ = PZ[n % 2], spb[n % ND], zsb[n % ND]
                    rel = kb - QT * 4
                    k.mm(pz.t[:], kn.t[R, kb * 128:(kb + 1) * 128], qn.t[R, qs], True, True, [kn, qn], [pz])
                    k.act(zs_.t[:], pz.t[:], AF.Exp, [pz], [zs_], scale=0.125)
                    k.act(sp_.t[:], zs_.t[:], AF.Ln, [zs_, epsc], [sp_], bias=epsc.t[:, 2:3])
                    if rel >= 0:
                        k.tt(sp_.t[:], sp_.t[:], maskb.t[:, rel, :], ALU.mult, [sp_, maskb], [sp_])

                def stepB(n):
                    pi, s, p, hh, QT, kb, kb_hi = steps[n]
                    R = slice(hh * 64, hh * 64 + 64)
                    qs = slice(QT * 512, (QT + 1) * 512)
                    kn, qnn = kn2[pi % 2], qnn2[pi % 2]
                    pt, po = PT[n % 2], PR[n % 2]
                    sp_, t1_, at_ = spb[n % ND], t1[n % ND], att[n % ND]
                    first = kb == kb_hi
                    k.mm(pt.t[:], kn.t[R, kb * 128:(kb + 1) * 128], qnn.t[R, qs], True, False, [kn, qnn], [pt])
                    k.mm(pt.t[:], triu.t[:], sp_.t[:], False, True, [triu, sp_], [pt])
                    if kb > 0:
                        k.mm(po.t[:], onesb.t[:], sp_.t[:], True, True, [onesb, sp_], [po])
                    if first:
                        k.act(at_.t[:], pt.t[:], AF.Exp, [pt], [at_], scale=-1.0)
                    else:
                        k.tt(t1_.t[:], pt.t[:], Rb.t[:], ALU.add, [pt, Rb], [t1_])
                        k.act(at_.t[:], t1_.t[:], AF.Exp, [t1_], [at_], scale=-1.0)
                    if kb > 0:
                        if first:
                            k.copy(Rb.t[:], po.t[:], [po], [Rb], e="dve")
                        else:
                            k.tt(Rb.t[:], Rb.t[:], po.t[:], ALU.add, [Rb, po], [Rb])

                def stepC(n):
                    pi, s, p, hh, QT, kb, kb_hi = steps[n]
                    R = slice(hh * 64, hh * 64 + 64)
                    h = 2 * p + hh
                    q = qtidx[(pi, hh, QT)] % 2
                    vtok = vtok2[pi % 2]
                    at_ = att[n % ND]
                    pv, obb = PV[q], ob[q]
                    rel = kb - QT * 4
                    if rel >= 0:
                        k.tt(at_.t[:], at_.t[:], maskb.t[:, rel, :], ALU.mult, [at_, maskb], [at_])
                    k.mm(pv.t[0:64, :], vtok.t[:, kb, R], at_.t[:], kb == kb_hi, kb == 0, [vtok, at_], [pv])
                    if kb == 0:
                        ts0 = s * T
                        k.copy(obb.t[:], pv.t[0:64, :], [pv], [obb], e="act")
                        k.dma("sp", catT[512 + h * 64:512 + (h + 1) * 64, ts0 + QT * 512:ts0 + (QT + 1) * 512], obb.t[:], [obb], [dbuf("catT", s)])

                NS_ = len(steps)
                per_pair = NS_ // len(pairs)
                prep(pairs[0][0], pairs[0][1], 0)
                DA, DB = 4, 2
                for n in range(-DA, NS_):
                    if n >= 0 and n % per_pair == 8:
                        pi = n // per_pair
                        if pi + 1 < len(pairs):
                            prep(pairs[pi + 1][0], pairs[pi + 1][1], (pi + 1) % 2)
                    if n + DA < NS_:
                        stepA(n + DA)
                    if 0 <= n + DB < NS_:
                        stepB(n + DB)
                    if n >= 0:
                        stepC(n)
            k.barrier()

        if "attn" in phases:
            phase_attn()

        def phase_out_moe(layer, cat_src, cat_name, nkc, w_out_d, x_src, x_name, gidx, x_mid, x_mid_name, x_dst, x_dst_name, tagp):
            TT = min(1024, NTOK)
            NSUB = TT // 128
            NH = TT // 512
            with ExitStack() as pes:
                wo = sb(pes, tagp + "wo", [128, nkc, D], BF16)
                for kc in range(nkc):
                    k.dma("pool", wo.t[:, kc, :], w_out_d[kc * 128:(kc + 1) * 128, :], (), [wo])
                gb = sb(pes, tagp + "gb", [128, D])
                k.dma("sp", gb.t[:], gbc_d[gidx], (), [gb])
                wr = sb(pes, tagp + "wr", [128, 8, 20])
                k.dma("sp", wr.t[:], w_router[layer].rearrange("(c p) n -> p c n", p=128), (), [wr])
                br = sb(pes, tagp + "br", [128, 20])
                k.dma("sp", br.t[:], b_router[layer], (), [br])
                sel = sb(pes, tagp + "sel", [16, 16, 128], BF16)
                k.dma("pool", sel.t[:], c_sel[:, :, :], (), [sel])
                if layer == 0 and "l1out" in phases and "l0in" not in phases:
                    precast(1)
                if layer == 1 and "l0out" not in phases:
                    precast(1)
                if layer == 0 and "l0in" not in phases:
                    precast(0)
                catb = [sb(pes, tagp + "cat%d" % i, [128, nkc, 128], BF16) for i in range(2)]
                xt = [sb(pes, tagp + "xt%d" % i, [128, D]) for i in range(2)]
                ssb = sb(pes, tagp + "ss", [128, 4])
                hb = sb(pes, tagp + "hb", [128, D], BF16)
                hf = sb(pes, tagp + "hf", [128, D])
                sq = hf
                hTf = sb(pes, tagp + "hTf", [128, 8, 128])
                hT2 = [sb(pes, tagp + "hT%d" % i, [128, 8, TT], BF16) for i in range(2)]
                yacc = sb(pes, tagp + "yacc", [128, NSUB, D])
                rt_ = sb(pes, tagp + "rt", [128, NSUB, 64])
                LG = sb(pes, tagp + "LG", [128, NSUB, 20])
                dg = sb(pes, tagp + "dg", [128, NSUB, 16], BF16)
                gT2 = [sb(pes, tagp + "gT%d" % i, [16, TT], BF16) for i in range(2)]
                gbc_e = [sb(pes, tagp + "gbe%d" % i, [128, TT], BF16) for i in range(2)]
                wgu = [sb(pes, tagp + "wgu%d" % i, [128, 2, 8, DEXP], BF16) for i in range(2)]
                wdn = [sb(pes, tagp + "wdn%d" % i, [128, 4, D], BF16) for i in range(2)]
                silu = [sb(pes, tagp + "silu%d" % i, [128, 512]) for i in range(2)]
                hid = [sb(pes, tagp + "hid%d" % i, [128, 4, TT], BF16) for i in range(2)]
                ptr = ps(pes, tagp + "ptr", [128, 1024], BF16)
                pg = [ps(pes, tagp + "pg%d" % i, [128, 512]) for i in range(2)]
                pu = [ps(pes, tagp + "pu%d" % i, [128, 512]) for i in range(2)]
                py = [ps(pes, tagp + "py%d" % i, [128, 512]) for i in range(3)]
                cnt_ = {"nf": 0, "ny": 0, "nx": 0}
                nx = 0
                ne = 0
                nf = 0
                ny = 0
                def make_front(tile):
                    t0 = tile * TT
                    hT = hT2[tile % 2]
                    gT = gT2[tile % 2]
                    cl = []
                    for j in range(NSUB):
                        def sub1(j=j):
                            r0 = t0 + j * 128
                            cb = catb[cnt_["nx"] % 2]
                            xb = xt[cnt_["nx"] % 2]
                            cnt_["nx"] += 1
                            k.dma("sp", cb.t[:], cat_src[:, r0:r0 + 128].rearrange("(c p) t -> p c t", p=128), [dbuf(cat_name, r0 // T)], [cb])
                            k.dma("pool", xb.t[:], x_src[r0:r0 + 128, :], [dbuf(x_name, r0 // 128)], [xb])
                            for half in range(2):
                                pp = py[half]
                                for kc in range(nkc):
                                    k.mm(pp.t[:], cb.t[:, kc, :], wo.t[:, kc, half * 512:(half + 1) * 512], kc == 0, kc == nkc - 1, [cb, wo], [pp])
                                k.tt(xb.t[:, half * 512:(half + 1) * 512], xb.t[:, half * 512:(half + 1) * 512], pp.t[:], ALU.add, [xb, pp], [xb])
                            k.dma("pool", x_mid[r0:r0 + 128, :], xb.t[:], [xb], [dbuf(x_mid_name, r0 // 128)])
                            k.tt(sq.t[:], xb.t[:], xb.t[:], ALU.mult, [xb], [sq])
                            k.op("dve", lambda g: g.reduce_sum(out=ssb.t[:, 0:1], in_=sq.t[:], axis=AX.X), [sq], [ssb])
                            k.rsqrt(ssb.t[:, 2:3], ssb.t[:, 0:1], 1.0 / D, epsc.t[:, 0:1], [ssb, epsc], [ssb])
                            k.stt(hf.t[:], xb.t[:], ssb.t[:, 2:3], gb.t[:], ALU.mult, ALU.mult, [xb, ssb, gb], [hf])
                            k.copy(hb.t[:], hf.t[:], [hf], [hb], e="act")
                        def sub2(j=j):
                            for kc in range(8):
                                k.tr(ptr.t[:, kc * 128:(kc + 1) * 128], hb.t[:, kc * 128:(kc + 1) * 128], identb.t[:], [hb, identb], [ptr])
                                pf_ = py[kc // 4]
                                k.tr(pf_.t[:, (kc % 4) * 128:(kc % 4 + 1) * 128], hf.t[:, kc * 128:(kc + 1) * 128], ident.t[:], [hf, ident], [pf_])
                            k.copy(hT.t[:, :, j * 128:(j + 1) * 128], ptr.t[:].rearrange("p (c t) -> p c t", t=128), [ptr], [hT], e="act")
                            for q_ in range(2):
                                k.copy(hTf.t[:, q_ * 4:(q_ + 1) * 4, :], py[q_].t[:].rearrange("p (c t) -> p c t", t=128), [py[q_]], [hTf], e="dve")
                        def sub3(j=j):
                            pr = pg[0]
                            for kc in range(8):
                                k.mm(pr.t[:, 0:20], hTf.t[:, kc, :], wr.t[:, kc, :], kc == 0, kc == 7, [hTf, wr], [pr])
                            k.tt(LG.t[:, j, :], pr.t[:, 0:20], br.t[:], ALU.add, [pr, br], [LG])
                        cl += [sub1, None, None, sub2, None, None, sub3, None, None]

                    def routing():
                        Rr = rt_.t
                        NS = NSUB

                        def col(a_, b_=None):
                            return Rr[:, :, a_:(a_ + 1 if b_ is None else b_)]

                        def bc(ap1, n_):
                            return ap1.to_broadcast([128, NS, n_])

                        gl = LG.t[:, :, 0:4]
                        k.op("dve", lambda g: g.reduce_max(out=col(44), in_=gl, axis=AX.X), [LG], [rt_])
                        k.tt(col(0, 4), gl, bc(col(44), 4), ALU.is_equal, [LG, rt_], [rt_])
                        k.tt(col(4, 8), gl, bc(col(44), 4), ALU.subtract, [LG, rt_], [rt_])
                        k.act(col(4, 8), col(4, 8), AF.Exp, [rt_], [rt_])
                        k.op("dve", lambda g: g.reduce_sum(out=col(45), in_=col(4, 8), axis=AX.X), [rt_], [rt_])
                        k.op("dve", lambda g: g.reciprocal(col(45), col(45)), [rt_], [rt_])
                        el4 = LG.t[:, :, 4:20].rearrange("p s (g e) -> p s g e", e=4)
                        t16 = Rr[:, :, 28:44].rearrange("p s (g e) -> p s g e", e=4)
                        k.tt(t16, el4, Rr[:, :, 0:4].unsqueeze(3).to_broadcast([128, NS, 4, 4]), ALU.mult, [LG, rt_], [rt_])
                        k.op("dve", lambda g: g.reduce_sum(out=col(8, 12), in_=Rr[:, :, 28:44].rearrange("p s (g e) -> p s e g", e=4), axis=AX.X), [rt_], [rt_])
                        k.op("dve", lambda g: g.reduce_max(out=col(46), in_=col(8, 12), axis=AX.X), [rt_], [rt_])
                        k.tt(col(12, 16), col(8, 12), bc(col(46), 4), ALU.is_equal, [rt_], [rt_])
                        k.stt(col(16, 20), col(12, 16), -1e30, col(8, 12), ALU.mult, ALU.add, [rt_], [rt_])
                        k.op("dve", lambda g: g.reduce_max(out=col(47), in_=col(16, 20), axis=AX.X), [rt_], [rt_])
                        k.tt(col(20, 24), col(16, 20), bc(col(47), 4), ALU.is_equal, [rt_], [rt_])
                        k.tt(col(48), col(47), col(46), ALU.subtract, [rt_], [rt_])
                        k.act(col(49), col(48), AF.Exp, [rt_], [rt_])
                        k.ts(col(50), col(49), 1.0, None, ALU.add, None, [rt_], [rt_])
                        k.op("dve", lambda g: g.reciprocal(col(50), col(50)), [rt_], [rt_])
                        k.tt(col(51), col(50), col(45), ALU.mult, [rt_], [rt_])
                        k.tt(col(52), col(51), col(49), ALU.mult, [rt_], [rt_])
                        k.tt(col(24, 28), col(12, 16), bc(col(51), 4), ALU.mult, [rt_], [rt_])
                        k.tt(col(4, 8), col(20, 24), bc(col(52), 4), ALU.mult, [rt_], [rt_])
                        k.tt(col(24, 28), col(24, 28), col(4, 8), ALU.add, [rt_], [rt_])
                        k.tt(dg.t[:].rearrange("p s (g e) -> p s g e", e=4), Rr[:, :, 0:4].unsqueeze(3).to_broadcast([128, NS, 4, 4]),
                             Rr[:, :, 24:28].unsqueeze(2).to_broadcast([128, NS, 4, 4]), ALU.mult, [rt_], [dg])
                        for j in range(NSUB):
                            ptb = ptr.t[:]
                            k.tr(ptb[0:16, j * 128:(j + 1) * 128], dg.t[:, j, :], identb.t[:], [dg, identb], [ptr])
                        k.copy(gT.t[:, :], ptr.t[0:16, 0:TT], [ptr], [gT], e="act")

                    cl.append(routing)
                    return cl

                ntiles = NTOK // TT
                AQ = "pool"

                def make_expert(tile, e, wg_, wd_, ge_, hid_, hT, gT):
                    def pro():
                        for hlf in range(NH):
                            pp = py[hlf % 2]
                            k.mm(pp.t[:], sel.t[:, e, :], gT.t[:, hlf * 512:(hlf + 1) * 512], True, True, [sel, gT], [pp])
                            k.copy(ge_.t[:, hlf * 512:(hlf + 1) * 512], pp.t[:], [pp], [ge_], e="act")
                    G = []
                    for fc in range(4):
                        for hlf in range(NH):
                            def g(fc=fc, hlf=hlf):
                                hs_ = slice(hlf * 512, (hlf + 1) * 512)
                                i_ = cnt_["nf"] % 2
                                cnt_["nf"] += 1
                                pgg, puu, sl_ = pg[i_], pu[i_], silu[i_]
                                for kc in range(8):
                                    k.mm(pgg.t[:], wg_.t[:, 0, kc, fc * 128:(fc + 1) * 128], hT.t[:, kc, hs_], kc == 0, kc == 7, [wg_, hT], [pgg])
                                for kc in range(8):
                                    k.mm(puu.t[:], wg_.t[:, 1, kc, fc * 128:(fc + 1) * 128], hT.t[:, kc, hs_], kc == 0, kc == 7, [wg_, hT], [puu])
                                k.act(sl_.t[:], pgg.t[:], AF.Silu, [pgg], [sl_])
                                k.tt(sl_.t[:], sl_.t[:], puu.t[:], ALU.mult, [sl_, puu], [sl_])
                                k.tt(hid_.t[:, fc, hs_], sl_.t[:], ge_.t[:, hs_], ALU.mult, [sl_, ge_], [hid_])
                            G.append(g)
                    Dn = []
                    for j in range(NSUB):
                        for half in range(2):
                            def d(j=j, half=half):
                                pp = py[cnt_["ny"] % 3]
                                cnt_["ny"] += 1
                                for fc in range(4):
                                    k.mm(pp.t[:], hid_.t[:, fc, j * 128:(j + 1) * 128], wd_.t[:, fc, half * 512:(half + 1) * 512], fc == 0, fc == 3, [hid_, wd_], [pp])
                                ya = yacc.t[:, j, half * 512:(half + 1) * 512]
                                if e == 0:
                                    k.copy(ya, pp.t[:], [pp], [yacc], e="dve")
                                else:
                                    k.tt(ya, ya, pp.t[:], ALU.add, [yacc, pp], [yacc])
                            Dn.append(d)
                    return pro, G, Dn

                def tail(tile):
                    for j in range(NSUB):
                        r0 = tile * TT + j * 128
                        xb = xt[cnt_["nx"] % 2]
                        cnt_["nx"] += 1
                        k.dma(AQ, xb.t[:], x_mid[r0:r0 + 128, :], [dbuf(x_mid_name, r0 // 128)], [xb])
                        k.tt(xb.t[:], xb.t[:], yacc.t[:, j, :], ALU.add, [xb, yacc], [xb], e="pool")
                        k.dma(AQ, x_dst[r0:r0 + 128, :], xb.t[:], [xb], [dbuf(x_dst_name, r0 // 128)])

                inst = [(tile, e) for tile in range(ntiles) for e in range(NEXP)]

                def load_wg(n):
                    e_ = inst[n][1]
                    k.dma("sp", wgu[n % 2].t[:], wgu_bf[layer, e_], [dbuf("wbf", (layer, e_))], [wgu[n % 2]])

                def load_wd(n):
                    e_ = inst[n][1]
                    k.dma("sp", wdn[n % 2].t[:], wdn_bf[layer, e_], [dbuf("wbf", (layer, e_))], [wdn[n % 2]])

                load_wg(0)
                load_wd(0)
                for f_ in make_front(0):
                    if f_ is not None:
                        f_()
                prevD = None
                pending = []
                for n, (tile, e) in enumerate(inst):
                    if e == 0:
                        pending = make_front(tile + 1) if tile + 1 < ntiles else []
                    if n + 1 < len(inst):
                        load_wg(n + 1)
                    pro, G, Dn = make_expert(tile, e, wgu[n % 2], wdn[n % 2], gbc_e[n % 2], hid[n % 2], hT2[tile % 2], gT2[tile % 2])
                    pro()

                    def inject():
                        if pending:
                            it = pending.pop(0)
                            if it is not None:
                                it()
                    if prevD is None:
                        for g in G:
                            g()
                            inject()
                    else:
                        per = (len(prevD) + len(G) - 1) // len(G)
                        di = 0
                        for g in G:
                            g()
                            for _ in range(per):
                                if di < len(prevD):
                                    prevD[di]()
                                    di += 1
                            inject()
                        while di < len(prevD):
                            prevD[di]()
                            di += 1
                    if n + 1 < len(inst):
                        load_wd(n + 1)
                    if e == 0 and tile > 0:
                        tail(tile - 1)
                    if e == NEXP - 1:
                        while pending:
                            it = pending.pop(0)
                            if it is not None:
                                it()
                    prevD = Dn
                for d in prevD:
                    d()
                tail(ntiles - 1)
            k.barrier()

        if "l0out" in phases:
            phase_out_moe(0, catT, "catT", 8, w_out_even, x_in, "x", 1, x1, "x1", x2, "x2", "m0_")

        L1_CHUNKS = [(i * 128, 128) for i in range(20)]

        def l1_dst(ci, c0, m, tt_, pp, st):
            if ci < 10:
                k.act(st.t[:], pp.t[:], AF.Gelu_apprx_tanh, [pp], [st])
                k.dma("sp", gateT[c0:c0 + 128, tt_ * 512:(tt_ + 1) * 512], st.t[:], [st], [dbuf("gateT", tt_ * 512 // T)])
            else:
                k.copy(st.t[:], pp.t[:], [pp], [st], e="act")
                k.dma("sp", recT[c0 - LRU_W:c0 - LRU_W + 128, tt_ * 512:(tt_ + 1) * 512], st.t[:], [st], [dbuf("recT", tt_ * 512 // T)])

        if "l1in" in phases:
            phase_in(x2, "x2", 2, w_in_odd, 2 * LRU_W, L1_CHUNKS, l1_dst, "b_")

        def phase_lru():
            with ExitStack() as pes:
                wrg = sb(pes, "u_wr", [128, 10, 128], BF16)
                wig = sb(pes, "u_wi", [128, 10, 128], BF16)
                k.dma("pool", wrg.t[:], w_rgate.rearrange("g i j -> i g j"), (), [wrg])
                k.dma("pool", wig.t[:], w_igate.rearrange("g i j -> i g j"), (), [wig])
                lam = sb(pes, "u_lam", [128, 10])
                for g_ in range(10):
                    k.act(lam.t[:, g_:g_ + 1], pcol("lam_%d" % g_), AF.Softplus, [pc], [lam], scale=-1.0)
                k.ts(lam.t[:], lam.t[:], -8.0, None, ALU.mult, None, [lam], [lam])
                u = [sb(pes, "u_u%d" % i, [128, T + 3]) for i in range(2)]
                gt = [sb(pes, "u_gt%d" % i, [128, T]) for i in range(2)]
                cv2 = [sb(pes, "u_cv%d" % i, [128, T]) for i in range(2)]
                cvb2 = [sb(pes, "u_cvb%d" % i, [128, T], BF16) for i in range(2)]
                rg2 = [sb(pes, "u_rg%d" % i, [128, T]) for i in range(2)]
                ig2 = [sb(pes, "u_ig%d" % i, [128, T]) for i in range(2)]
                aa = sb(pes, "u_aa", [128, T])
                bb = sb(pes, "u_bb", [128, T])
                hh_ = sb(pes, "u_hh", [128, T])
                ob = [sb(pes, "u_ob%d" % i, [128, T], BF16) for i in range(2)]
                pr = [ps(pes, "u_pr%d" % i, [128, 512]) for i in range(2)]
                pi = [ps(pes, "u_pi%d" % i, [128, 512]) for i in range(2)]
                items = [(s, g_) for s in range(NSEQ) for g_ in range(10)]

                def stageA(n):
                    s, g_ = items[n]
                    ts0 = s * T
                    ub, gtb = u[n % 2], gt[n % 2]
                    cv, cvb, rg, ig = cv2[n % 2], cvb2[n % 2], rg2[n % 2], ig2[n % 2]
                    k.memset(ub.t[:, 0:3], 0.0, [ub])
                    k.dma("sp", ub.t[:, 3:T + 3], recT[g_ * 128:(g_ + 1) * 128, ts0:ts0 + T], [dbuf("recT", s)], [ub])
                    k.dma("sp", gtb.t[:], gateT[g_ * 128:(g_ + 1) * 128, ts0:ts0 + T], [dbuf("gateT", s)], [gtb])
                    k.ts(cv.t[:], ub.t[:, 0:T], pcol("cw0_%d" % g_), pcol("cb_%d" % g_), ALU.mult, ALU.add, [ub, pc], [cv])
                    for tp in range(1, 4):
                        k.stt(cv.t[:], ub.t[:, tp:T + tp], pcol("cw%d_%d" % (tp, g_)), cv.t[:], ALU.mult, ALU.add, [ub, cv, pc], [cv])
                    k.copy(cvb.t[:], cv.t[:], [cv], [cvb], e="act")
                    for f in range(T // 512):
                        fs = slice(f * 512, (f + 1) * 512)
                        k.mm(pr[f % 2].t[:], wrg.t[:, g_, :], cvb.t[:, fs], True, True, [wrg, cvb], [pr[f % 2]])
                        k.act(rg.t[:, fs], pr[f % 2].t[:], AF.Sigmoid, [pr[f % 2], pc], [rg], bias=pcol("br_%d" % g_))
                        k.mm(pi[f % 2].t[:], wig.t[:, g_, :], cvb.t[:, fs], True, True, [wig, cvb], [pi[f % 2]])
                        k.act(ig.t[:, fs], pi[f % 2].t[:], AF.Sigmoid, [pi[f % 2], pc], [ig], bias=pcol("bi_%d" % g_))

                def stageB(n):
                    s, g_ = items[n]
                    ts0 = s * T
                    gtb, obb = gt[n % 2], ob[n % 2]
                    cv, rg, ig = cv2[n % 2], rg2[n % 2], ig2[n % 2]
                    k.act(aa.t[:], rg.t[:], AF.Exp, [rg, lam], [aa], scale=lam.t[:, g_:g_ + 1])
                    k.tt(bb.t[:], aa.t[:], aa.t[:], ALU.mult, [aa], [bb])
                    k.act(bb.t[:], bb.t[:], AF.Sqrt, [bb, epsc], [bb], scale=-1.0, bias=epsc.t[:, 2:3])
                    k.tt(ig.t[:], ig.t[:], cv.t[:], ALU.mult, [ig, cv], [ig])
                    k.tt(bb.t[:], bb.t[:], ig.t[:], ALU.mult, [bb, ig], [bb])
                    k.op("dve", lambda g, : g.tensor_tensor_scan(hh_.t[:], aa.t[:], bb.t[:], 0.0, ALU.mult, ALU.add), [aa, bb], [hh_])
                    k.tt(obb.t[:], hh_.t[:], gtb.t[:], ALU.mult, [hh_, gtb], [obb])
                    k.dma("sp", cat2T[g_ * 128:(g_ + 1) * 128, ts0:ts0 + T], obb.t[:], [obb], [dbuf("cat2T", s)])

                stageA(0)
                for n in range(len(items)):
                    if n + 1 < len(items):
                        stageA(n + 1)
                    stageB(n)
            k.barrier()

        if "lru" in phases:
            phase_lru()

        if "l1out" in phases:
            phase_out_moe(1, cat2T, "cat2T", 10, w_out_odd, x2, "x2", 3, x3, "x3", out_d, "out", "m1_")

        if debug:
            with ExitStack() as pes:
                a = sb(pes, "dbg_a", [128, 512], BF16)
                b = sb(pes, "dbg_b", [128, 512])
                for (src, dst, rows) in ((catT, catT_dbg, D), (cat2T, cat2T_dbg, LRU_W)):
                    for r in range(rows // 128):
                        for c in range(NTOK // 512):
                            k.dma("sp", a.t[:], src[r * 128:(r + 1) * 128, c * 512:(c + 1) * 512], (), [a])
                            k.copy(b.t[:], a.t[:], [a], [b], e="dve")
                            k.dma("sp", dst[r * 128:(r + 1) * 128, c * 512:(c + 1) * 512], b.t[:], [b], [a])
        k.barrier(skip_pre=False)
        print("kernel build: %d instructions" % k.n_inst)
    return nc


def host_inputs(inp):
    cols, pcarr = _pcols(inp)
    c = _consts()
    sel = np.zeros((16, 16, 128), np.float32)
    for e in range(16):
        sel[e, e, :] = 1.0
    f = lambda a: np.ascontiguousarray(np.asarray(a, np.float32))
    shared = {
        "gbc": f(np.stack([np.broadcast_to(inp["norm_mix"][0], (128, D)), np.broadcast_to(inp["norm_ffn"][0], (128, D)),
                           np.broadcast_to(inp["norm_mix"][1], (128, D)), np.broadcast_to(inp["norm_ffn"][1], (128, D))])),
        "pc": f(pcarr),
        "w_in_even": f(inp["w_in_even"][0]),
        "lora_w": f(np.concatenate([inp["decay_up"][0], inp["iclr_up"][0]], axis=0)),
        "gate_up": f(inp["gate_up"][0]),
        "w_out_even": f(inp["w_out_even"][0]),
        "w_in_odd": f(inp["w_in_odd"][0]),
        "w_rgate": f(inp["w_rgate"][0]),
        "w_igate": f(inp["w_igate"][0]),
        "w_out_odd": f(inp["w_out_odd"][0]),
        "w_router": f(np.concatenate([inp["w_group"], inp["w_erouter"]], axis=2)),
        "b_router": f(np.broadcast_to(np.concatenate([inp["b_group"], inp["b_erouter"]], axis=1)[:, None, :], (2, 128, 20))),
        "exp_w_gate": f(inp["exp_w_gate"]),
        "exp_w_up": f(inp["exp_w_up"]),
        "exp_w_down": f(inp["exp_w_down"]),
        "ident": c["ident"], "blockones": c["blockones"], "ones": c["ones"], "triu": c["triu"],
        "maskrel": f(c["maskrel"]), "m64": f(c["m64"]), "sel": sel,
    }
    return cols, pcarr.shape[1], shared


def kernel(**inputs):
    inp = {k_: np.asarray(v) for k_, v in inputs.items()}
    x = inp["x"]
    B, T, _ = x.shape
    nseq = B // NCORES
    cols, npc, shared = host_inputs(inp)
    nc = build(nseq, T, cols, npc)
    in_maps = []
    for c in range(NCORES):
        m = dict(shared)
        m["x"] = np.ascontiguousarray(x[c * nseq:(c + 1) * nseq].reshape(nseq * T, D), dtype=np.float32)
        in_maps.append(m)
    res = run_bass_kernel_spmd(nc, in_maps, core_ids=list(range(NCORES)))
    out = np.concatenate([np.asarray(r["out"]).reshape(nseq, T, D) for r in res.results], axis=0)
    return out.astype(np.float32)
```
